# Optimizing a Trainium2 kernel written in Bass

```python
import jax, jax.numpy as jnp
from jax import lax
import numpy as np

D_MODEL = 2048
BATCH = 4
SEQ = 8192
DEPTH = 4

GROUP_WIDTH = 128
HEAD_DIM = 128
D_GMLP = D_MODEL // 4
D_SB = D_MODEL // 4
D_MIX = D_GMLP + D_SB
N_GMLP_GROUPS = D_GMLP // GROUP_WIDTH
N_SB_HEADS = D_SB // HEAD_DIM
N_MIX_GROUPS = D_MIX // GROUP_WIDTH
D_IN = 2 * D_GMLP + 3 * D_SB
SPLIT_POINTS = (D_GMLP, 2 * D_GMLP, 2 * D_GMLP + D_SB, 2 * D_GMLP + 2 * D_SB)
CHUNK = 128
Q_BLOCK = 128
N_GROUPS = 4
EXPERTS_PER_GROUP = 8
N_EXPERTS = N_GROUPS * EXPERTS_PER_GROUP
TOP_K = 2
D_EXPERT = D_MODEL // 8
EXPERT_BLOCK = 128
N_MOD = 6
EPS = 1e-6

kernel_name = "hybrid_sgmlp_stickbreak_hmoe_adaln"


def rms_norm(x, g):
    xf = x.astype(jnp.float32)
    y = xf * lax.rsqrt(jnp.mean(xf * xf, axis=-1, keepdims=True) + EPS)
    return (y * g.astype(jnp.float32)).astype(x.dtype)


def chunked_spatial_gating(u, v, norm_g, ws, bs):
    B, S, _ = u.shape
    u = jax.nn.gelu(u, approximate=False)
    v = jax.nn.gelu(v, approximate=False).reshape(B, S // CHUNK, CHUNK, N_GMLP_GROUPS, GROUP_WIDTH)
    v = rms_norm(v, norm_g.reshape(N_GMLP_GROUPS, GROUP_WIDTH))
    w = ws * jnp.tril(jnp.ones((CHUNK, CHUNK), ws.dtype))
    s = jnp.einsum('gtp,bcpgk->bctgk', w, v) + bs.T[:, :, None]
    return u * s.reshape(B, S, D_GMLP)


def stick_breaking_attention(q, k, v):
    B, S, H, dh = q.shape
    nq = S // Q_BLOCK
    scale = HEAD_DIM ** -0.5
    qf = q.astype(jnp.float32).transpose(0, 2, 1, 3)
    kf = k.astype(jnp.float32).transpose(0, 2, 1, 3)
    vt = v.transpose(0, 2, 1, 3)
    idx = jnp.arange(Q_BLOCK, dtype=jnp.int32)
    after_in_block = (idx[:, None] > idx[None, :]).astype(jnp.float32)
    outs = []
    for n in range(nq):
        nk = n + 1
        kl = nk * Q_BLOCK
        qb = qf[:, :, n * Q_BLOCK:kl]
        z = jnp.einsum('bhqd,bhkd->bhqk', qb, kf[:, :, :kl]) * scale
        q_pos = n * Q_BLOCK + idx
        causal = jnp.arange(kl, dtype=jnp.int32)[None, :] < q_pos[:, None]
        log_fail = jnp.where(causal, jax.nn.log_sigmoid(-z), 0.0).reshape(B, H, Q_BLOCK, nk, Q_BLOCK)
        within = jnp.einsum('bhqni,ij->bhqnj', log_fail, after_in_block)
        blk = jnp.sum(log_fail, axis=-1)
        later = lax.cumsum(blk, axis=3, reverse=True) - blk
        log_a = jax.nn.log_sigmoid(z) + (within + later[..., None]).reshape(B, H, Q_BLOCK, kl)
        a = jnp.where(causal, jnp.exp(log_a), 0.0)
        outs.append(jnp.einsum('bhqk,bhkd->bhqd', a.astype(vt.dtype), vt[:, :, :kl]))
    o = jnp.concatenate(outs, axis=2)
    return o.transpose(0, 2, 1, 3).reshape(B, S, H * dh)


def hierarchical_moe(h, w_group, b_group, w_route, b_route, w_gate, w_up, w_down):
    n, d = h.shape
    hf = h.astype(jnp.float32)
    g_logits = hf @ w_group.astype(jnp.float32) + b_group.astype(jnp.float32)
    g_prob = jax.nn.softmax(g_logits, axis=-1)
    g_idx = jnp.argmax(g_logits, axis=-1).astype(jnp.int32)
    p_group = jnp.take_along_axis(g_prob, g_idx[:, None], axis=-1)
    e_logits = (hf @ w_route.astype(jnp.float32) + b_route.astype(jnp.float32)).reshape(n, N_GROUPS, EXPERTS_PER_GROUP)
    e_logits = jnp.take_along_axis(e_logits, g_idx[:, None, None], axis=1)[:, 0]
    top_val, top_idx = lax.top_k(e_logits, TOP_K)
    weights = jax.nn.softmax(top_val, axis=-1) * p_group
    expert_id = g_idx[:, None] * EXPERTS_PER_GROUP + top_idx.astype(jnp.int32)

    a = n * TOP_K
    flat_e = expert_id.reshape(a)
    flat_tok = jnp.repeat(jnp.arange(n, dtype=jnp.int32), TOP_K)
    order = jnp.argsort(flat_e)
    sorted_e = flat_e[order]
    counts = jnp.bincount(flat_e, length=N_EXPERTS).astype(jnp.int32)
    start = jnp.cumsum(counts) - counts
    padded = (counts + EXPERT_BLOCK - 1) // EXPERT_BLOCK * EXPERT_BLOCK
    pend = jnp.cumsum(padded)
    pstart = pend - padded
    rank = jnp.arange(a, dtype=jnp.int32) - start[sorted_e]
    slot = jnp.zeros((a,), jnp.int32).at[order].set((pstart[sorted_e] + rank).astype(jnp.int32))
    n_blocks = (a + EXPERT_BLOCK - 1) // EXPERT_BLOCK + N_EXPERTS
    capacity = n_blocks * EXPERT_BLOCK
    slot_tok = jnp.full((capacity,), n, jnp.int32).at[slot].set(flat_tok)
    block_e = jnp.minimum(
        jnp.searchsorted(pend, jnp.arange(n_blocks, dtype=jnp.int32) * EXPERT_BLOCK, side='right'),
        N_EXPERTS - 1).astype(jnp.int32)
    h_pad = jnp.concatenate([h, jnp.zeros((1, d), h.dtype)], axis=0)
    xs = h_pad[slot_tok].reshape(n_blocks, EXPERT_BLOCK, d)

    def expert_block(args):
        xb, e = args
        act = jax.nn.silu(xb @ w_gate[e]) * (xb @ w_up[e])
        return act @ w_down[e]

    ys = lax.map(expert_block, (xs, block_e)).reshape(capacity, d)
    picked = ys[slot].reshape(n, TOP_K, d)
    return jnp.einsum('nkd,nk->nd', picked, weights.astype(picked.dtype))


def setup_inputs(seed: int = 0) -> dict:
    key = jax.random.key(seed)
    ks = jax.random.split(key, 22)
    f32 = jnp.float32
    L, D = DEPTH, D_MODEL

    def nrm(k, shape, s):
        return jax.random.normal(k, shape, f32) * s

    return {
        "x": nrm(ks[0], (BATCH, SEQ, D), 1.0),
        "c": nrm(ks[1], (BATCH, D), 1.0),
        "w_mod": nrm(ks[2], (D, N_MOD * D), 0.5 * D ** -0.5),
        "b_mod": nrm(ks[3], (N_MOD * D,), 0.02),
        "mod_layer": nrm(ks[4], (L, N_MOD * D), 0.1),
        "norm1_g": 1.0 + nrm(ks[5], (L, D), 0.02),
        "w_in": nrm(ks[6], (L, D, D_IN), D ** -0.5),
        "gm_norm_g": 1.0 + nrm(ks[7], (L, D_GMLP), 0.02),
        "gm_ws": nrm(ks[8], (L, N_GMLP_GROUPS, CHUNK, CHUNK), CHUNK ** -0.5),
        "gm_bs": 1.0 + nrm(ks[9], (L, N_GMLP_GROUPS, CHUNK), 0.1),
        "q_norm_g": 1.0 + nrm(ks[10], (L, HEAD_DIM), 0.02),
        "k_norm_g": 1.0 + nrm(ks[11], (L, HEAD_DIM), 0.02),
        "out_norm_g": 1.0 + nrm(ks[12], (L, D_MIX), 0.02),
        "w_out": nrm(ks[13], (L, D_MIX, D), D_MIX ** -0.5),
        "norm2_g": 1.0 + nrm(ks[14], (L, D), 0.02),
        "w_group": nrm(ks[15], (L, D, N_GROUPS), D ** -0.5),
        "b_group": nrm(ks[16], (L, N_GROUPS), 0.01),
        "w_route": nrm(ks[17], (L, D, N_EXPERTS), D ** -0.5),
        "b_route": nrm(ks[18], (L, N_EXPERTS), 0.01),
        "w_gate": nrm(ks[19], (L, N_EXPERTS, D, D_EXPERT), D ** -0.5),
        "w_up": nrm(ks[20], (L, N_EXPERTS, D, D_EXPERT), D ** -0.5),
        "w_down": nrm(ks[21], (L, N_EXPERTS, D_EXPERT, D), D_EXPERT ** -0.5),
    }


def reference(x, c, w_mod, b_mod, mod_layer, norm1_g, w_in, gm_norm_g, gm_ws, gm_bs,
              q_norm_g, k_norm_g, out_norm_g, w_out, norm2_g, w_group, b_group,
              w_route, b_route, w_gate, w_up, w_down):
    B, S, D = x.shape
    mod_shared = jax.nn.silu(c) @ w_mod + b_mod
    for l in range(DEPTH):
        mod = (mod_shared + mod_layer[l])[:, None, :]
        shift1, scale1, gate1, shift2, scale2, gate2 = jnp.split(mod, N_MOD, axis=-1)

        h = rms_norm(x, norm1_g[l]) * (1.0 + scale1) + shift1
        proj = h @ w_in[l]
        u_a, v_a, q, k, v = jnp.split(proj, SPLIT_POINTS, axis=-1)
        y_a = chunked_spatial_gating(u_a, v_a, gm_norm_g[l], gm_ws[l], gm_bs[l])
        q = rms_norm(q.reshape(B, S, N_SB_HEADS, HEAD_DIM), q_norm_g[l])
        k = rms_norm(k.reshape(B, S, N_SB_HEADS, HEAD_DIM), k_norm_g[l])
        v = v.reshape(B, S, N_SB_HEADS, HEAD_DIM)
        y_b = stick_breaking_attention(q, k, v)
        y = jnp.concatenate([y_a, y_b], axis=-1).reshape(B, S, N_MIX_GROUPS, GROUP_WIDTH)
        y = rms_norm(y, out_norm_g[l].reshape(N_MIX_GROUPS, GROUP_WIDTH)).reshape(B, S, D_MIX)
        x = x + gate1 * (y @ w_out[l])

        h = rms_norm(x, norm2_g[l]) * (1.0 + scale2) + shift2
        m = hierarchical_moe(h.reshape(B * S, D), w_group[l], b_group[l], w_route[l], b_route[l],
                             w_gate[l], w_up[l], w_down[l]).reshape(B, S, D)
        x = x + gate2 * m
    return x
```

```python
import numpy as np
import ml_dtypes
from contextlib import ExitStack
import concourse.bass as bass
import concourse.mybir as mybir
from concourse.bass_utils import run_bass_kernel_spmd

F32 = mybir.dt.float32
BF16 = mybir.dt.bfloat16
I32 = mybir.dt.int32
AF = mybir.ActivationFunctionType
ALU = mybir.AluOpType
AX = mybir.AxisListType

D = 2048
KC = 16
DIN = 2560
NEXP = 32
EPS = 1e-6
DEPTH = 4
NMOD = 6


class Reg:
    __slots__ = ("w", "r")

    def __init__(self):
        self.w = None
        self.r = {}


class Sched:
    NDS = 12

    def __init__(self, nc):
        self.nc = nc
        self.engs = {"pe": nc.tensor, "act": nc.scalar, "dve": nc.vector, "pool": nc.gpsimd, "sp": nc.sync}
        self.sem = {k: nc.semaphore("sem_" + k).__enter__() for k in self.engs}
        self.cnt = {k: 0 for k in self.engs}
        self.waited = {k: {} for k in self.engs}
        self.dsems = {}
        self.dcnt = {}
        self.dnext = {}
        for q in ("sp", "pool", "act"):
            self.dsems[q] = [nc.semaphore("ds_%s%d" % (q, i)).__enter__() for i in range(self.NDS)]
            self.dcnt[q] = [0] * self.NDS
            self.dnext[q] = 0
        self.n_inst = 0

    def _semobj(self, key):
        if isinstance(key, str):
            return self.sem[key]
        return self.dsems[key[0]][key[1]]

    def _wait(self, e, key, val):
        if val <= 0 or self.waited[e].get(key, 0) >= val:
            return
        self.engs[e].wait_ge(self._semobj(key), val)
        self.waited[e][key] = val
        self.n_inst += 1

    def _deps(self, e, reads, writes):
        deps = {}
        for r in reads:
            if r.w is not None:
                k, v = r.w
                deps[k] = max(deps.get(k, 0), v)
        for w in writes:
            if w.w is not None:
                k, v = w.w
                deps[k] = max(deps.get(k, 0), v)
            for k, v in w.r.items():
                deps[k] = max(deps.get(k, 0), v)
        for k, v in deps.items():
            if e == "pe" and k == "pe":
                continue
            self._wait(e, k, v)

    def _mark(self, ev, reads, writes):
        k, v = ev
        for r in reads:
            r.r[k] = max(r.r.get(k, 0), v)
        for w in writes:
            w.w = ev
            w.r = {}

    def op(self, e, fn, reads=(), writes=(), signal=True):
        self._deps(e, reads, writes)
        inst = fn(self.engs[e])
        self.n_inst += 1
        if signal:
            self.cnt[e] += 1
            inst.then_inc(self.sem[e], 1)
            ev = (e, self.cnt[e])
        else:
            ev = (e, self.cnt[e] + 1)
        self._mark(ev, reads, writes)

    def dma(self, q, fn, reads=(), writes=()):
        self._deps(q, reads, writes)
        i = self.dnext[q]
        self.dnext[q] = (i + 1) % self.NDS
        key = (q, i)
        self._wait(q, key, self.dcnt[q][i])
        inst = fn(self.engs[q])
        self.n_inst += 1
        self.dcnt[q][i] += 16
        inst.then_inc(self.dsems[q][i], 16)
        self._mark((key, self.dcnt[q][i]), reads, writes)

    def barrier(self, scratch_ap):
        for k in self.engs:
            if k != "pool":
                self._wait("pool", k, self.cnt[k])
        for q in self.dsems:
            if q == "wc":
                continue
            for i in range(len(self.dsems[q])):
                self._wait("pool", (q, i), self.dcnt[q][i])
        inst = self.nc.gpsimd.memset(scratch_ap, 0.0)
        self.cnt["pool"] += 1
        inst.then_inc(self.sem["pool"], 1)
        self.n_inst += 1
        for k in self.engs:
            if k != "pool":
                self._wait(k, "pool", self.cnt["pool"])

    def final_wait(self):
        for q in self.dsems:
            for i in range(len(self.dsems[q])):
                self._wait("sp", (q, i), self.dcnt[q][i])


def own_blocks(G, NT, rank):
    if G == 1:
        return list(range(NT))
    out = []
    for t in range(NT):
        j = t // 2
        if t % 2 == 0:
            out.append(4 * j + (0 if rank == 0 else 1))
        else:
            out.append(4 * j + (3 if rank == 0 else 2))
    return out


def blk_src(G, n):
    if G == 1:
        return (0, n)
    j, c = n // 4, n % 4
    return {0: (0, 2 * j), 1: (1, 2 * j), 2: (1, 2 * j + 1), 3: (0, 2 * j + 1)}[c]


def build(NT, NBLK, G, depth=DEPTH):
    S = NBLK * 128
    NTOK = NT * 128
    CAPB = (2 * NTOK) // 128 + NEXP
    CAP = CAPB * 128
    nc = bass.Bass("TRN2", target_bir_lowering=False)
    dt_in = lambda n, s, d=F32: nc.dram_tensor(n, s, d, kind="ExternalInput").ap()
    dt_sc = lambda n, s, d=F32: nc.dram_tensor(n, s, d, kind="Internal").ap()
    x_in = dt_in("x", [NTOK, D])
    cT_in = dt_in("cT", [128, KC])
    w_mod = dt_in("w_mod", [D, NMOD * D])
    b_mod = dt_in("b_mod", [1, NMOD * D])
    mod_layer = dt_in("mod_layer", [depth, NMOD * D])
    norm1_g = dt_in("norm1_g", [depth, D])
    w_in = dt_in("w_in", [depth, D, DIN])
    gm_norm_g = dt_in("gm_norm_g", [depth, 512])
    gm_wsT = dt_in("gm_wsT", [depth, 4, 128, 128])
    gm_bsT = dt_in("gm_bsT", [depth, 128, 4])
    q_norm_g = dt_in("q_norm_g", [depth, 128])
    k_norm_g = dt_in("k_norm_g", [depth, 128])
    out_norm_g = dt_in("out_norm_g", [depth, 1024])
    w_out = dt_in("w_out", [depth, 1024, D])
    norm2_g = dt_in("norm2_g", [depth, D])
    w_r = dt_in("w_r", [depth, D, 36])
    b_r = dt_in("b_r", [depth, 36])
    w_gate = dt_in("w_gate", [depth, NEXP, D, 256])
    w_up = dt_in("w_up", [depth, NEXP, D, 256])
    w_down = dt_in("w_down", [depth, NEXP, 256, D])
    consts = dt_in("consts", [128, 1280])
    amask_in = dt_in("amask", [128, 4, 128])
    out_d = nc.dram_tensor("out", [NTOK, D], F32, kind="ExternalOutput").ap()

    xa_d = dt_sc("xa_d", [NTOK, D])
    xm_d = dt_sc("xm_d", [NTOK, D])
    modl_d = dt_sc("modl_d", [depth, NMOD, 128, D])
    kT_d = dt_sc("kT_d", [NT, 128, 512], BF16)
    v_d = dt_sc("v_d", [NT, 128, 512], BF16)
    if G > 1:
        kTall_d = dt_sc("kTall_d", [G * NT, 128, 512], BF16)
        vall_d = dt_sc("vall_d", [G * NT, 128, 512], BF16)
    else:
        kTall_d, vall_d = kT_d, v_d
    qT_d = dt_sc("qT_d", [NT, 128, 512], BF16)
    yT_d = dt_sc("yT_d", [NT, 128, 1024], BF16)
    h2_d = dt_sc("h2_d", [NTOK + 1, D], BF16)
    wgu_d = dt_sc("wgu_d", [NEXP * 128, 8192], BF16)
    wd_d = dt_sc("wd_d", [NEXP * 128, 4096], BF16)
    stok_d = dt_sc("stok_d", [CAP, 1], I32)
    ys_d = dt_sc("ys_d", [CAP, D], BF16)

    sch = Sched(nc)
    op, dma = sch.op, sch.dma
    uid = [0]

    def alloc(st, shape, dt, ps=False):
        uid[0] += 1
        f = nc.psum_tensor if ps else nc.sbuf_tensor
        return st.enter_context(f("t%d" % uid[0], shape, dt))

    class Buf:
        def __init__(self, st, shape, dt, n=1, ps=False):
            self.t = [alloc(st, shape, dt, ps) for _ in range(n)]
            self.g = [Reg() for _ in range(n)]
            self.n = n

        def __getitem__(self, i):
            return self.t[i % self.n], self.g[i % self.n]

    top = ExitStack()
    cst_f = Buf(top, [128, 1280], F32)
    cst_b = Buf(top, [128, 1280], BF16)
    bar_scr = alloc(top, [128, 1], F32)
    cf, cfg_ = cst_f[0]
    cb, cbg = cst_b[0]
    dma("sp", lambda e: e.dma_start(out=cf[:], in_=consts), writes=[cfg_])
    op("dve", lambda e: e.tensor_copy(out=cb[:], in_=cf[:]), reads=[cfg_], writes=[cbg])
    ident_f = cf[:, 0:128]
    ident_b = cb[:, 0:128]
    triIncl_b = cb[:, 128:256]
    triLow_b = cb[:, 256:384]
    ones_b = cb[:, 512:640]
    gmMaskT_f = cf[:, 640:768]
    iota_p = cf[:, 768:769]
    iota_b = cf[:, 1024:1280]
    amask = Buf(top, [128, 4, 128], F32)
    am, amg = amask[0]
    dma("sp", lambda e: e.dma_start(out=am[:], in_=amask_in), writes=[amg])
    zrow = Buf(top, [1, D], BF16)
    zr, zrg = zrow[0]
    op("pool", lambda e: e.memset(zr[:], 0.0), writes=[zrg])
    h2z_g = Reg()
    dma("sp", lambda e: e.dma_start(out=h2_d[NTOK:NTOK + 1, :], in_=zr[:]), reads=[zrg], writes=[h2z_g])

    def rstd_from(ssq_ap, out_ap, n, greads, gwrite):
        op("dve", lambda e: e.tensor_scalar(out=out_ap, in0=ssq_ap, scalar1=1.0 / n, scalar2=EPS, op0=ALU.mult, op1=ALU.add),
           reads=greads, writes=[gwrite])
        op("act", lambda e: e.activation(out=out_ap, in_=out_ap, func=AF.Ln), reads=[gwrite], writes=[gwrite])
        op("act", lambda e: e.activation(out=out_ap, in_=out_ap, func=AF.Exp, scale=-0.5), reads=[gwrite], writes=[gwrite])

    with ExitStack() as st:
        cTt = Buf(st, [128, KC], F32)
        sc_b = Buf(st, [128, KC, 128], F32)
        wm = Buf(st, [128, KC, 512], F32, n=2)
        msh = Buf(st, [128, NMOD * D], F32)
        pm = Buf(st, [128, 512], F32, n=2, ps=True)
        bm = Buf(st, [128, 2048], F32, n=2)
        g_t = Buf(st, [128, 2048], F32, n=2)
        o_t = Buf(st, [128, 2048], F32, n=2)
        c_t, c_g = cTt[0]
        s_t, s_g = sc_b[0]
        m_t, m_g = msh[0]
        dma("sp", lambda e: e.dma_start(out=c_t[:], in_=cT_in), writes=[c_g])
        op("act", lambda e: e.activation(out=c_t[:], in_=c_t[:], func=AF.Silu), reads=[c_g], writes=[c_g])
        for kc in range(KC):
            op("dve", lambda e, kc=kc: e.tensor_copy(out=s_t[:, kc, :], in_=c_t[:, kc:kc + 1].to_broadcast([128, 128])),
               reads=[c_g], writes=[s_g])
        NG = NMOD * D // 512
        for n in range(NG):
            w_t, w_g = wm[n]
            dma("sp", lambda e, n=n, w_t=w_t: e.dma_start(
                out=w_t[:], in_=w_mod[:, n * 512:(n + 1) * 512].rearrange("(kc p) n -> p kc n", p=128)), writes=[w_g])
            p_t, p_g = pm[n]
            for kc in range(KC):
                op("pe", lambda e, kc=kc, p_t=p_t, w_t=w_t: e.matmul(p_t[:], lhsT=s_t[:, kc, :], rhs=w_t[:, kc, :],
                                                                   start=(kc == 0), stop=(kc == KC - 1)),
                   reads=[s_g, w_g], writes=[p_g], signal=(kc == KC - 1))
            op("act", lambda e, n=n, p_t=p_t: e.activation(out=m_t[:, n * 512:(n + 1) * 512], in_=p_t[:], func=AF.Copy),
               reads=[p_g], writes=[m_g])
        for j in range(NMOD):
            b_t, b_g = bm[j]
            dma("sp", lambda e, j=j, b_t=b_t: e.dma_start(out=b_t[:], in_=b_mod[0:1, j * D:(j + 1) * D].partition_broadcast(128)),
                writes=[b_g])
            op("dve", lambda e, j=j, b_t=b_t: e.tensor_tensor(out=m_t[:, j * D:(j + 1) * D], in0=m_t[:, j * D:(j + 1) * D],
                                                             in1=b_t[:], op=ALU.add), reads=[b_g, m_g], writes=[m_g])
        modl_g = Reg()
        k = 0
        for l in range(depth):
            for j in range(NMOD):
                b_t, b_g = bm[k]
                o_tt, o_g = o_t[k]
                dma("sp", lambda e, l=l, j=j, b_t=b_t: e.dma_start(
                    out=b_t[:], in_=mod_layer[l:l + 1, j * D:(j + 1) * D].partition_broadcast(128)), writes=[b_g])
                if j in (1, 4):
                    gg_t, gg_g = g_t[k]
                    gsrc = norm1_g if j == 1 else norm2_g
                    dma("sp", lambda e, l=l, gg_t=gg_t, gsrc=gsrc: e.dma_start(
                        out=gg_t[:], in_=gsrc[l:l + 1, :].partition_broadcast(128)), writes=[gg_g])
                    op("dve", lambda e, j=j, b_t=b_t, o_tt=o_tt: e.scalar_tensor_tensor(
                        out=o_tt[:], in0=m_t[:, j * D:(j + 1) * D], scalar=1.0, in1=b_t[:], op0=ALU.add, op1=ALU.add),
                       reads=[m_g, b_g], writes=[o_g])
                    op("dve", lambda e, o_tt=o_tt, gg_t=gg_t: e.tensor_tensor(out=o_tt[:], in0=o_tt[:], in1=gg_t[:], op=ALU.mult),
                       reads=[o_g, gg_g], writes=[o_g])
                else:
                    op("dve", lambda e, j=j, b_t=b_t, o_tt=o_tt: e.tensor_tensor(
                        out=o_tt[:], in0=m_t[:, j * D:(j + 1) * D], in1=b_t[:], op=ALU.add), reads=[m_g, b_g], writes=[o_g])
                dma("sp", lambda e, l=l, j=j, o_tt=o_tt: e.dma_start(out=modl_d[l, j], in_=o_tt[:]), reads=[o_g], writes=[modl_g])
                k += 1
        sch.barrier(bar_scr[:])

    bc_reg = nc.gpsimd.register("bc_reg").__enter__()
    nc.gpsimd.reg_mov(bc_reg, NEXP * 128 - 1)
    bc_val = nc.gpsimd.snap(bc_reg)
    wcast_g = Reg()
    wc_sem = nc.semaphore("wcast_sem").__enter__()
    sch.dsems["wc"] = [wc_sem]
    sch.dcnt["wc"] = [0]

    def cast_expert_weights(l, ex):
        for (o, i_) in ((wgu_d[ex * 128:(ex + 1) * 128, 0:4096], w_gate[l, ex].rearrange("(p kc) f -> p (kc f)", kc=KC)),
                        (wgu_d[ex * 128:(ex + 1) * 128, 4096:8192], w_up[l, ex].rearrange("(p kc) f -> p (kc f)", kc=KC)),
                        (wd_d[ex * 128:(ex + 1) * 128, :], w_down[l, ex].rearrange("(p c) n -> p (c n)", c=2))):
            inst = nc.gpsimd.dma_start(out=o, in_=i_)
            sch.dcnt["wc"][0] += 16
            inst.then_inc(wc_sem, 16)
            sch.n_inst += 1
        wcast_g.w = (("wc", 0), sch.dcnt["wc"][0])

    def bload(q, t, g, src_row):
        dma(q, lambda e: e.dma_start(out=t, in_=src_row.partition_broadcast(128)), writes=[g])

    for l in range(depth):
        x_src = x_in if l == 0 else xa_d
        x_dst = out_d if l == depth - 1 else xa_d
        lay = ExitStack()
        with ExitStack() as st:
            winb = Buf(st, [128, KC, DIN], BF16)
            wi_t, wi_g = winb[0]
            for kc in range(KC):
                dma("pool", lambda e, kc=kc: e.dma_start(out=wi_t[:, kc, :], in_=w_in[l, kc * 128:(kc + 1) * 128, :]), writes=[wi_g])
            gs1 = Buf(st, [128, D], F32)
            sh1 = Buf(st, [128, D], F32)
            dma("sp", lambda e: e.dma_start(out=gs1[0][0][:], in_=modl_d[l, 1]), writes=[gs1[0][1]])
            dma("sp", lambda e: e.dma_start(out=sh1[0][0][:], in_=modl_d[l, 0]), writes=[sh1[0][1]])
            wmT_f = Buf(st, [128, 4, 128], F32)
            wmT = Buf(st, [128, 4, 128], BF16)
            dma("sp", lambda e: e.dma_start(out=wmT_f[0][0][:], in_=gm_wsT[l].rearrange("g p t -> p g t")), writes=[wmT_f[0][1]])
            op("dve", lambda e: e.tensor_tensor(out=wmT[0][0][:], in0=wmT_f[0][0][:],
                                               in1=gmMaskT_f.unsqueeze(1).to_broadcast([128, 4, 128]), op=ALU.mult),
               reads=[wmT_f[0][1], cfg_], writes=[wmT[0][1]])
            bsT = Buf(st, [128, 4], F32)
            dma("sp", lambda e: e.dma_start(out=bsT[0][0][:], in_=gm_bsT[l]), writes=[bsT[0][1]])
            gmn = Buf(st, [128, 512], F32)
            bload("sp", gmn[0][0][:], gmn[0][1], gm_norm_g[l:l + 1, :])
            oga = Buf(st, [128, 512], F32)
            bload("sp", oga[0][0][:], oga[0][1], out_norm_g[l:l + 1, 0:512])
            gq = Buf(st, [128, 128], F32)
            gk = Buf(st, [128, 128], F32)
            bload("sp", gq[0][0][:], gq[0][1], q_norm_g[l:l + 1, :])
            bload("sp", gk[0][0][:], gk[0][1], k_norm_g[l:l + 1, :])
            op("dve", lambda e: e.tensor_scalar(out=gq[0][0][:], in0=gq[0][0][:], scalar1=128.0 ** -0.5, scalar2=None, op0=ALU.mult),
               reads=[gq[0][1]], writes=[gq[0][1]])
            xb = Buf(st, [128, D], F32, n=2)
            ssb = Buf(st, [128, 2], F32, n=2)
            hf = Buf(st, [128, D], F32, n=1)
            hb = Buf(st, [128, D], BF16, n=2)
            hT = Buf(st, [128, KC, 128], BF16, n=2)
            gu = Buf(st, [128, 512], F32, n=2)
            gv = Buf(st, [128, 512], F32, n=2)
            sq = Buf(st, [128, 512], F32, n=2)
            sqq = Buf(st, [128, 1024], F32, n=1)
            sm = Buf(st, [128, 16], F32, n=2)
            sm2 = Buf(st, [128, 16], F32, n=2)
            vn = Buf(st, [128, 512], BF16, n=2)
            t1 = Buf(st, [128, 512], F32, n=1)
            ynb = Buf(st, [128, 512], BF16, n=2)
            qn = Buf(st, [128, 1024], BF16, n=2)
            qf = Buf(st, [128, 1024], F32, n=1)
            yst = Buf(st, [128, 512], BF16, n=2)
            kst = Buf(st, [128, 512], BF16, n=2)
            qst = Buf(st, [128, 512], BF16, n=2)
            vst = Buf(st, [128, 512], BF16, n=2)
            tp = Buf(st, [128, 1024], BF16, n=2, ps=True)
            pp = [Buf(st, [128, 512], F32, n=1, ps=True) for _ in range(5)]
            sgp = Buf(st, [128, 512], F32, n=1, ps=True)
            yTa_g = Reg()
            kv_g = Reg()
            G4 = lambda ap: ap.rearrange("p (g k) -> p g k", g=4)

            def front_a(i):
                for ex in range(i * NEXP // NT, (i + 1) * NEXP // NT):
                    cast_expert_weights(l, ex)
                xt, xg = xb[i]
                dma("sp", lambda e: e.dma_start(out=xt[:], in_=x_src[i * 128:(i + 1) * 128, :]), writes=[xg])
                s_t, s_g = ssb[i]
                hft, hfg = hf[i]
                hbt, hbg = hb[i]
                op("act", lambda e: e.activation(out=hbt[:], in_=xt[:], func=AF.Square, accum_out=s_t[:, 0:1]),
                   reads=[xg], writes=[hbg, s_g])
                rstd_from(s_t[:, 0:1], s_t[:, 1:2], D, [s_g], s_g)
                op("dve", lambda e: e.scalar_tensor_tensor(out=hft[:], in0=xt[:], scalar=s_t[:, 1:2], in1=gs1[0][0][:],
                                                          op0=ALU.mult, op1=ALU.mult), reads=[xg, s_g, gs1[0][1]], writes=[hfg])
                op("pool", lambda e: e.tensor_tensor(out=hbt[:], in0=hft[:], in1=sh1[0][0][:], op=ALU.add),
                   reads=[hfg, sh1[0][1]], writes=[hbg])

            def front_b(i):
                hbt, hbg = hb[i]
                hTt, hTg = hT[i]
                for half in range(2):
                    tpt, tpg = tp[half]
                    for k8 in range(8):
                        kc = half * 8 + k8
                        op("pe", lambda e, kc=kc, k8=k8: e.transpose(tpt[:, k8 * 128:(k8 + 1) * 128],
                                                                    hbt[:, kc * 128:(kc + 1) * 128], ident_b),
                           reads=[hbg, cbg], writes=[tpg], signal=(k8 == 7))
                    dst = hTt[:, half * 8:(half + 1) * 8, :].rearrange("p a b -> p (a b)")
                    if half == 0:
                        op("act", lambda e: e.activation(out=dst, in_=tpt[:], func=AF.Copy), reads=[tpg], writes=[hTg])
                    else:
                        op("dve", lambda e: e.tensor_copy(out=dst, in_=tpt[:]), reads=[tpg], writes=[hTg])

            def mid(i, nbs=range(5)):
                hTt, hTg = hT[i]
                for nb in nbs:
                    ppt, ppg = pp[nb][0]
                    for kc in range(KC):
                        op("pe", lambda e, kc=kc: e.matmul(ppt[:], lhsT=hTt[:, kc, :], rhs=wi_t[:, kc, nb * 512:(nb + 1) * 512],
                                                          start=(kc == 0), stop=(kc == KC - 1)),
                           reads=[hTg, wi_g], writes=[ppg], signal=(kc == KC - 1))

            def back1(i):
                gut, gug = gu[i]
                gvt, gvg = gv[i]
                op("act", lambda e: e.activation(out=gut[:], in_=pp[0][0][0][:], func=AF.Gelu), reads=[pp[0][0][1]], writes=[gug])
                op("act", lambda e: e.activation(out=gvt[:], in_=pp[1][0][0][:], func=AF.Gelu), reads=[pp[1][0][1]], writes=[gvg])
                sqqt, sqqg = sqq[i]
                sm2t, sm2g = sm2[i]
                for which in range(2):
                    ppt, ppg = pp[2 + which][0]
                    op("act", lambda e: e.activation(out=sqqt[:, which * 512:(which + 1) * 512], in_=ppt[:], func=AF.Square),
                       reads=[ppg], writes=[sqqg])
                vstt, vstg = vst[i]
                op("act", lambda e: e.activation(out=vstt[:], in_=pp[4][0][0][:], func=AF.Copy), reads=[pp[4][0][1]], writes=[vstg])
                dma("act", lambda e: e.dma_start(out=v_d[i], in_=vstt[:]), reads=[vstg], writes=[kv_g])
                op("dve", lambda e: e.tensor_reduce(out=sm2t[:, 0:8], in_=sqqt[:].rearrange("p (g k) -> p g k", g=8), axis=AX.X, op=ALU.add),
                   reads=[sqqg], writes=[sm2g])
                rstd_from(sm2t[:, 0:8], sm2t[:, 8:16], 128, [sm2g], sm2g)
                qft, qfg = qf[i]
                qnt, qng = qn[i]
                for which in range(2):
                    ppt, ppg = pp[2 + which][0]
                    c0 = which * 512
                    op("dve", lambda e: e.tensor_tensor(out=G4(qft[:, c0:c0 + 512]), in0=G4(ppt[:]),
                                                       in1=sm2t[:, 8 + which * 4:12 + which * 4].unsqueeze(2).to_broadcast([128, 4, 128]), op=ALU.mult),
                       reads=[ppg, sm2g], writes=[qfg])
                    gsel = gq if which == 0 else gk
                    op("dve", lambda e: e.tensor_tensor(out=G4(qnt[:, c0:c0 + 512]), in0=G4(qft[:, c0:c0 + 512]),
                                                       in1=gsel[0][0][:].unsqueeze(1).to_broadcast([128, 4, 128]), op=ALU.mult),
                       reads=[qfg, gsel[0][1]], writes=[qng])

            def back1v(i):
                gvt, gvg = gv[i]
                sqt, sqg = sq[i]
                smt, smg = sm[i]
                op("pool", lambda e: e.tensor_tensor(out=sqt[:], in0=gvt[:], in1=gvt[:], op=ALU.mult), reads=[gvg], writes=[sqg])
                op("dve", lambda e: e.tensor_reduce(out=smt[:, 0:4], in_=G4(sqt[:]), axis=AX.X, op=ALU.add), reads=[sqg], writes=[smg])
                rstd_from(smt[:, 0:4], smt[:, 4:8], 128, [smg], smg)
                vnt, vng = vn[i]
                op("dve", lambda e: e.tensor_tensor(out=G4(vnt[:]), in0=G4(gvt[:]),
                                                   in1=smt[:, 4:8].unsqueeze(2).to_broadcast([128, 4, 128]), op=ALU.mult),
                   reads=[gvg, smg], writes=[vng])

            def sgmm(i):
                vnt, vng = vn[i]
                sgt, sgg = sgp[0]
                for g in range(4):
                    op("pe", lambda e, g=g: e.matmul(sgt[:, g * 128:(g + 1) * 128], lhsT=wmT[0][0][:, g, :],
                                                    rhs=vnt[:, g * 128:(g + 1) * 128], start=True, stop=True),
                       reads=[wmT[0][1], vng], writes=[sgg], signal=(g == 3))

            def back2(i):
                gut, gug = gu[i]
                sqt, sqg = sq[i]
                smt, smg = sm[i]
                sgt, sgg = sgp[0]
                t1t, t1g = t1[i]
                op("dve", lambda e: e.tensor_tensor(out=t1t[:], in0=sgt[:], in1=gmn[0][0][:], op=ALU.mult),
                   reads=[sgg, gmn[0][1]], writes=[t1g])
                op("dve", lambda e: e.tensor_tensor(out=G4(t1t[:]), in0=G4(t1t[:]),
                                                   in1=bsT[0][0][:].unsqueeze(2).to_broadcast([128, 4, 128]), op=ALU.add),
                   reads=[t1g, bsT[0][1]], writes=[t1g])
                op("dve", lambda e: e.tensor_tensor(out=t1t[:], in0=t1t[:], in1=gut[:], op=ALU.mult), reads=[t1g, gug], writes=[t1g])
                op("pool", lambda e: e.tensor_tensor(out=sqt[:], in0=t1t[:], in1=t1t[:], op=ALU.mult), reads=[t1g], writes=[sqg])
                op("dve", lambda e: e.tensor_reduce(out=smt[:, 8:12], in_=G4(sqt[:]), axis=AX.X, op=ALU.add), reads=[sqg], writes=[smg])
                rstd_from(smt[:, 8:12], smt[:, 12:16], 128, [smg], smg)
                op("dve", lambda e: e.tensor_tensor(out=G4(t1t[:]), in0=G4(t1t[:]),
                                                   in1=smt[:, 12:16].unsqueeze(2).to_broadcast([128, 4, 128]), op=ALU.mult),
                   reads=[t1g, smg], writes=[t1g])
                ynt, yng = ynb[i]
                op("dve", lambda e: e.tensor_tensor(out=ynt[:], in0=t1t[:], in1=oga[0][0][:], op=ALU.mult),
                   reads=[t1g, oga[0][1]], writes=[yng])
                tpt, tpg = tp[0]
                for g in range(4):
                    op("pe", lambda e, g=g: e.transpose(tpt[:, g * 128:(g + 1) * 128], ynt[:, g * 128:(g + 1) * 128], ident_b),
                       reads=[yng, cbg], writes=[tpg], signal=(g == 3))
                ystt, ystg = yst[i]
                op("act", lambda e: e.activation(out=ystt[:], in_=tpt[:, 0:512], func=AF.Copy), reads=[tpg], writes=[ystg])
                dma("act", lambda e: e.dma_start(out=yT_d[i, :, 0:512], in_=ystt[:]), reads=[ystg], writes=[yTa_g])

            def late_qk(i):
                qnt, qng = qn[i]
                tpt, tpg = tp[1]
                for k8 in range(8):
                    op("pe", lambda e, k8=k8: e.transpose(tpt[:, k8 * 128:(k8 + 1) * 128], qnt[:, k8 * 128:(k8 + 1) * 128], ident_b),
                       reads=[qng, cbg], writes=[tpg], signal=(k8 == 7))
                qstt, qstg = qst[i]
                op("act", lambda e: e.activation(out=qstt[:], in_=tpt[:, 0:512], func=AF.Copy), reads=[tpg], writes=[qstg])
                dma("act", lambda e: e.dma_start(out=qT_d[i], in_=qstt[:]), reads=[qstg], writes=[kv_g])
                kstt, kstg = kst[i]
                op("act", lambda e: e.activation(out=kstt[:], in_=tpt[:, 512:1024], func=AF.Copy), reads=[tpg], writes=[kstg])
                dma("act", lambda e: e.dma_start(out=kT_d[i], in_=kstt[:]), reads=[kstg], writes=[kv_g])

            front_a(0)
            front_b(0)
            mid(0)
            if NT > 1:
                front_a(1)
            for i in range(NT):
                back1(i)
                back1v(i)
                if i + 1 < NT:
                    front_b(i + 1)
                    mid(i + 1, range(0, 2))
                sgmm(i)
                if i + 1 < NT:
                    mid(i + 1, range(2, 5))
                if i + 2 < NT:
                    front_a(i + 2)
                back2(i)
                late_qk(i)
            sch.barrier(bar_scr[:])
        if G > 1:
            groups = [[g * G + r for r in range(G)] for g in range(8 // G)]
            sch._deps("pool", [], [])
            for src, dst in ((kT_d, kTall_d), (v_d, vall_d)):
                dma("pool", lambda e, src=src, dst=dst: e.collective_compute(
                    "AllGather", ALU.bypass, replica_groups=groups,
                    ins=[src.rearrange("t p c -> (t p) c")], outs=[dst.rearrange("t p c -> (t p) c")]), writes=[Reg()])
            sch.barrier(bar_scr[:])
        with ExitStack() as st:
            NCH = 2
            KT = Buf(st, [128, 4, S], BF16)
            VV = Buf(st, [128, NBLK, 512], BF16)
            KTt, _ = KT[0]
            VVt, _ = VV[0]
            kreg = [Reg() for _ in range(NBLK)]
            vreg = [Reg() for _ in range(NBLK)]
            for n in range(NBLK):
                r, t = blk_src(G, n)
                row = r * NT + t
                dma("sp", lambda e, n=n, row=row: e.dma_start(out=KTt[:, :, n * 128:(n + 1) * 128],
                                                             in_=kTall_d[row].rearrange("p (h t) -> p h t", h=4)), writes=[kreg[n]])
                dma("sp", lambda e, n=n, row=row: e.dma_start(out=VVt[:, n, :], in_=vall_d[row]), writes=[vreg[n]])
            ogb = Buf(st, [128, 512], F32)
            bload("sp", ogb[0][0][:], ogb[0][1], out_norm_g[l:l + 1, 512:1024])
            eb = Buf(st, [128, NCH, 512], F32, n=2)
            spb = Buf(st, [128, NCH, 512], BF16, n=3)
            wb = Buf(st, [128, NCH, 512], F32, n=2)
            ab = Buf(st, [128, NCH, 512], BF16, n=2)
            qtb = Buf(st, [128, 4, 128], BF16, n=2 * NCH)
            zps = Buf(st, [128, NCH, 512], F32, n=1, ps=True)
            tpo = Buf(st, [128, 512], F32, n=1, ps=True)
            cps = Buf(st, [128, NCH, 512], F32, n=1, ps=True)
            ops_ = Buf(st, [128, 512], F32, n=NCH, ps=True)
            osq = Buf(st, [128, 512], F32, n=2)
            osm = Buf(st, [128, 8], F32, n=2)
            of = Buf(st, [128, 512], F32, n=2)
            ost = Buf(st, [128, 512], BF16, n=2)
            yTb_g = Reg()
            tiles = [(i, i, i + 1) for i in range(NT)]
            zctr = [0]
            fin_ctr = [0]

            def finish(t, c):
                ot, og_ = ops_[c]
                k = fin_ctr[0]
                fin_ctr[0] += 1
                sqt, sqg = osq[k]
                smt, smg = osm[k]
                op("act", lambda e: e.activation(out=sqt[:], in_=ot[:], func=AF.Square), reads=[og_], writes=[sqg])
                op("dve", lambda e: e.tensor_reduce(out=smt[:, 0:4], in_=sqt[:].rearrange("p (g k) -> p g k", g=4), axis=AX.X, op=ALU.add),
                   reads=[sqg], writes=[smg])
                rstd_from(smt[:, 0:4], smt[:, 4:8], 128, [smg], smg)
                oft, ofg = of[k]
                op("dve", lambda e: e.tensor_tensor(out=oft[:].rearrange("p (g k) -> p g k", g=4),
                                                   in0=ot[:].rearrange("p (g k) -> p g k", g=4),
                                                   in1=smt[:, 4:8].unsqueeze(2).to_broadcast([128, 4, 128]), op=ALU.mult),
                   reads=[og_, smg], writes=[ofg])
                op("dve", lambda e: e.tensor_tensor(out=oft[:], in0=oft[:], in1=ogb[0][0][:], op=ALU.mult),
                   reads=[ofg, ogb[0][1]], writes=[ofg])
                tpt, tpg = tpo[0]
                for g in range(4):
                    op("pe", lambda e, g=g: e.transpose(tpt[:, g * 128:(g + 1) * 128], oft[:, g * 128:(g + 1) * 128], ident_f),
                       reads=[ofg, cfg_], writes=[tpg], signal=(g == 3))
                ostt, ostg = ost[k]
                op("act", lambda e: e.activation(out=ostt[:], in_=tpt[:], func=AF.Copy), reads=[tpg], writes=[ostg])
                dma("act", lambda e: e.dma_start(out=yT_d[t, :, 512:1024], in_=ostt[:]), reads=[ostg], writes=[yTb_g])

            gctr = [0]
            for g0 in range(0, NT, NCH):
                grp = tiles[g0:g0 + NCH]
                Lmax = max(x[2] for x in grp)
                nch = len(grp)
                gi = gctr[0]
                gctr[0] += 1
                qts = []
                for c, (t, nmax, L) in enumerate(grp):
                    QTt, qg_ = qtb[gi * NCH + c]
                    dma("sp", lambda e, t=t, QTt=QTt: e.dma_start(out=QTt[:], in_=qT_d[t].rearrange("p (h t) -> p h t", h=4)), writes=[qg_])
                    qts.append((QTt, qg_))
                spprev = [None]
                zt, zg = zps[0]
                ct, cg = cps[0]

                def E1(r):
                    for c, (t, nmax, L) in enumerate(grp):
                        if r >= L:
                            continue
                        m = nmax - r
                        QTt, qg_ = qts[c]
                        for h in range(4):
                            op("pe", lambda e, h=h: e.matmul(zt[:, c, h * 128:(h + 1) * 128], lhsT=KTt[:, h, m * 128:(m + 1) * 128],
                                                            rhs=QTt[:, h, :], start=True, stop=True),
                               reads=[kreg[m], qg_], writes=[zg], signal=(h == 3))

                def V2(tl, cs):
                    if len(cs) == NCH:
                        return tl[:].rearrange("p c f -> p (c f)")
                    return tl[:, cs[0], :]

                rctr = gi * 64
                E1(0)
                for r in range(Lmax):
                    act_c = [c for c, (t, nmax, L) in enumerate(grp) if r < L]
                    nact = len(act_c)
                    et, eg = eb[rctr + r]
                    spt, spg = spb[rctr + r]
                    wt, wg = wb[rctr + r]
                    at, ag = ab[rctr + r]
                    op("act", lambda e: e.activation(out=V2(et, act_c), in_=V2(zt, act_c), func=AF.Exp), reads=[zg], writes=[eg])
                    op("act", lambda e: e.activation(out=V2(spt, act_c), in_=V2(et, act_c), func=AF.Ln, bias=1.0), reads=[eg], writes=[spg])
                    if r == 0:
                        mk = am[:, 0, :]
                        nh = 4 * nact
                        for (tl, tg) in ((spt, spg), (et, eg)):
                            vv = V2(tl, act_c).rearrange("p (h t) -> p h t", h=nh)
                            op("pool", lambda e, vv=vv: e.tensor_tensor(out=vv, in0=vv, in1=mk.unsqueeze(1).to_broadcast([128, nh, 128]), op=ALU.mult),
                               reads=[tg, amg], writes=[tg])
                    prev = spprev[0]
                    for c in act_c:
                        for h in range(4):
                            hs = slice(h * 128, (h + 1) * 128)
                            if prev is not None:
                                pt_, pg_ = prev
                                op("pe", lambda e, hs=hs: e.matmul(ct[:, c, hs], lhsT=triLow_b, rhs=pt_[:, c, hs], start=False, stop=False,
                                                                  skip_group_check=True),
                                   reads=[pg_, cbg], writes=[cg], signal=False)
                            op("pe", lambda e, hs=hs, h=h: e.matmul(ct[:, c, hs], lhsT=triIncl_b, rhs=spt[:, c, hs],
                                                                   start=(prev is None and h == 0), stop=True, skip_group_check=True),
                               reads=[spg, cbg], writes=[cg], signal=(h == 3 and c == act_c[-1]))
                    spprev[0] = (spt, spg)
                    if r + 1 < Lmax:
                        E1(r + 1)
                    op("act", lambda e: e.activation(out=V2(wt, act_c), in_=V2(ct, act_c), func=AF.Exp, scale=-1.0), reads=[cg], writes=[wg])
                    op("dve", lambda e: e.tensor_tensor(out=V2(at, act_c), in0=V2(et, act_c), in1=V2(wt, act_c), op=ALU.mult),
                       reads=[eg, wg], writes=[ag])
                    for c in act_c:
                        t, nmax, L = grp[c]
                        m = nmax - r
                        ot, og_ = ops_[c]
                        for h in range(4):
                            hs = slice(h * 128, (h + 1) * 128)
                            op("pe", lambda e, hs=hs, h=h: e.matmul(ot[:, hs], lhsT=at[:, c, hs], rhs=VVt[:, m, hs],
                                                                   start=(r == 0 and h == 0), stop=(r == L - 1), skip_group_check=True),
                               reads=[ag, vreg[m]], writes=[og_], signal=(h == 3))
                        if r == L - 1:
                            finish(t, c)
            sch.barrier(bar_scr[:])
        lay.close()
        moe = ExitStack()
        OH0 = Buf(moe, [128, NT, 32], F32)
        OH1 = Buf(moe, [128, NT, 32], F32)
        AA = Buf(moe, [128, NT, 32], BF16)
        WW = Buf(moe, [128, NT, 2], F32)
        SL = Buf(moe, [128, NT, 2], I32)
        widx = Buf(moe, [128, CAPB], I32)
        oh0t, oh0g = OH0[0]
        oh1t, oh1g = OH1[0]
        aat, aag = AA[0]
        wwt, wwg = WW[0]
        slt, slg = SL[0]
        with ExitStack() as st:
            wob = Buf(st, [128, 8, D], BF16)
            wo_t, wo_g = wob[0]
            for c in range(8):
                dma("pool", lambda e, c=c: e.dma_start(out=wo_t[:, c, :], in_=w_out[l, c * 128:(c + 1) * 128, :]), writes=[wo_g])
            gate1 = Buf(st, [128, D], F32)
            gs2 = Buf(st, [128, D], F32)
            sh2 = Buf(st, [128, D], F32)
            dma("sp", lambda e: e.dma_start(out=gate1[0][0][:], in_=modl_d[l, 2]), writes=[gate1[0][1]])
            dma("sp", lambda e: e.dma_start(out=gs2[0][0][:], in_=modl_d[l, 4]), writes=[gs2[0][1]])
            dma("sp", lambda e: e.dma_start(out=sh2[0][0][:], in_=modl_d[l, 3]), writes=[sh2[0][1]])
            wrt = Buf(st, [128, KC, 36], F32)
            dma("sp", lambda e: e.dma_start(out=wrt[0][0][:], in_=w_r[l].rearrange("(kc p) n -> p kc n", p=128)), writes=[wrt[0][1]])
            brt = Buf(st, [128, 36], F32)
            bload("sp", brt[0][0][:], brt[0][1], b_r[l:l + 1, :])
            yTb = Buf(st, [128, 1024], BF16, n=2)
            xb = Buf(st, [128, D], F32, n=2)
            xm = Buf(st, [128, D], F32, n=2)
            junk = Buf(st, [128, D], BF16, n=1)
            ssb = Buf(st, [128, 2], F32, n=2)
            h2f = Buf(st, [128, D], F32, n=2)
            h2b = Buf(st, [128, D], BF16, n=2)
            h2T = Buf(st, [128, KC, 128], F32, n=1)
            lg = Buf(st, [128, 36], F32, n=2)
            rs = Buf(st, [128, 64], F32, n=2)
            po = [Buf(st, [128, 512], F32, n=1, ps=True) for _ in range(4)]
            tpf = Buf(st, [128, 512], F32, n=2, ps=True)
            lgp = Buf(st, [128, 512], F32, n=1, ps=True)
            xm_g = Reg()
            h2_g = Reg()

            def s1(i):
                yt, yg = yTb[i]
                dma("sp", lambda e, i=i: e.dma_start(out=yt[:], in_=yT_d[i]), writes=[yg])
                xt, xg = xb[i]
                dma("sp", lambda e, i=i: e.dma_start(out=xt[:], in_=x_src[i * 128:(i + 1) * 128, :]), writes=[xg])
                for nb in range(4):
                    pt, pg = po[nb][0]
                    for c in range(8):
                        op("pe", lambda e, c=c, nb=nb, pt=pt: e.matmul(pt[:], lhsT=yt[:, c * 128:(c + 1) * 128],
                                                                     rhs=wo_t[:, c, nb * 512:(nb + 1) * 512], start=(c == 0), stop=(c == 7)),
                           reads=[yg, wo_g], writes=[pg], signal=(c == 7))
                xmt, xmg = xm[i]
                for nb in range(4):
                    pt, pg = po[nb][0]
                    cs = slice(nb * 512, (nb + 1) * 512)
                    op("dve", lambda e, cs=cs, pt=pt: e.tensor_tensor(out=xmt[:, cs], in0=pt[:], in1=gate1[0][0][:, cs], op=ALU.mult),
                       reads=[pg, gate1[0][1]], writes=[xmg])
                op("pool", lambda e: e.tensor_tensor(out=xmt[:], in0=xmt[:], in1=xt[:], op=ALU.add), reads=[xmg, xg], writes=[xmg])
                dma("pool", lambda e, i=i: e.dma_start(out=xm_d[i * 128:(i + 1) * 128, :], in_=xmt[:]), reads=[xmg], writes=[xm_g])
                jt, jg = junk[i]
                s_t, s_g = ssb[i]
                op("act", lambda e: e.activation(out=jt[:], in_=xmt[:], func=AF.Square, accum_out=s_t[:, 0:1]),
                   reads=[xmg], writes=[jg, s_g])
                rstd_from(s_t[:, 0:1], s_t[:, 1:2], D, [s_g], s_g)
                hft, hfg = h2f[i]
                op("dve", lambda e: e.scalar_tensor_tensor(out=hft[:], in0=xmt[:], scalar=s_t[:, 1:2], in1=gs2[0][0][:],
                                                          op0=ALU.mult, op1=ALU.mult), reads=[xmg, s_g, gs2[0][1]], writes=[hfg])
                op("pool", lambda e: e.tensor_tensor(out=hft[:], in0=hft[:], in1=sh2[0][0][:], op=ALU.add),
                   reads=[hfg, sh2[0][1]], writes=[hfg])
                hbt, hbg = h2b[i]
                op("act", lambda e: e.activation(out=hbt[:], in_=hft[:], func=AF.Copy), reads=[hfg], writes=[hbg])
                dma("act", lambda e, i=i: e.dma_start(out=h2_d[i * 128:(i + 1) * 128, :], in_=hbt[:]), reads=[hbg], writes=[h2_g])

            def s2(i):
                hft, hfg = h2f[i]
                hTt, hTg = h2T[i]
                for q4 in range(4):
                    tpt, tpg = tpf[q4]
                    for k4 in range(4):
                        kc = q4 * 4 + k4
                        op("pe", lambda e, kc=kc, k4=k4, tpt=tpt: e.transpose(tpt[:, k4 * 128:(k4 + 1) * 128],
                                                                             hft[:, kc * 128:(kc + 1) * 128], ident_f),
                           reads=[hfg, cfg_], writes=[tpg], signal=(k4 == 3))
                    op("act" if q4 % 2 == 0 else "dve",
                       lambda e, q4=q4, tpt=tpt: (e.activation(out=hTt[:, q4 * 4:(q4 + 1) * 4, :].rearrange("p a b -> p (a b)"),
                                                               in_=tpt[:], func=AF.Copy) if q4 % 2 == 0 else
                                                  e.tensor_copy(out=hTt[:, q4 * 4:(q4 + 1) * 4, :].rearrange("p a b -> p (a b)"), in_=tpt[:])),
                       reads=[tpg], writes=[hTg])
                lpt, lpg = lgp[0]
                for kc in range(KC):
                    op("pe", lambda e, kc=kc: e.matmul(lpt[:, 0:36], lhsT=hTt[:, kc, :], rhs=wrt[0][0][:, kc, :],
                                                      start=(kc == 0), stop=(kc == KC - 1)),
                       reads=[hTg, wrt[0][1]], writes=[lpg], signal=(kc == KC - 1))
                lt, lgg = lg[i]
                r, rg = rs[i]
                V = lambda f, **kw: op("dve", f, **kw)
                V(lambda e: e.tensor_tensor(out=lt[:], in0=lpt[:, 0:36], in1=brt[0][0][:], op=ALU.add), reads=[lpg, brt[0][1]], writes=[lgg])
                V(lambda e: e.tensor_reduce(out=r[:, 0:1], in_=lt[:, 0:4], axis=AX.X, op=ALU.max), reads=[lgg], writes=[rg])
                V(lambda e: e.tensor_scalar(out=r[:, 4:8], in0=lt[:, 0:4], scalar1=r[:, 0:1], scalar2=None, op0=ALU.is_equal),
                  reads=[lgg, rg], writes=[rg])
                V(lambda e: e.tensor_scalar(out=r[:, 1:2], in0=r[:, 0:1], scalar1=-1.0, scalar2=None, op0=ALU.mult), reads=[rg], writes=[rg])
                op("act", lambda e: e.activation(out=r[:, 8:12], in_=lt[:, 0:4], func=AF.Exp, bias=r[:, 1:2], accum_out=r[:, 2:3]),
                   reads=[lgg, rg], writes=[rg])
                V(lambda e: e.reciprocal(out=r[:, 3:4], in_=r[:, 2:3]), reads=[rg], writes=[rg])
                V(lambda e: e.tensor_tensor(out=r[:, 16:48].rearrange("p (g j) -> p g j", g=4),
                                            in0=lt[:, 4:36].rearrange("p (g j) -> p g j", g=4),
                                            in1=r[:, 4:8].unsqueeze(2).to_broadcast([128, 4, 8]), op=ALU.mult),
                  reads=[lgg, rg], writes=[rg])
                V(lambda e: e.tensor_reduce(out=r[:, 48:56], in_=r[:, 16:48].rearrange("p (g j) -> p j g", g=4), axis=AX.X, op=ALU.add),
                  reads=[rg], writes=[rg])
                V(lambda e: e.max(out=r[:, 56:64], in_=r[:, 48:56]), reads=[rg], writes=[rg])
                V(lambda e: e.tensor_scalar(out=r[:, 16:24], in0=r[:, 48:56], scalar1=r[:, 56:57], scalar2=None, op0=ALU.is_equal),
                  reads=[rg], writes=[rg])
                V(lambda e: e.tensor_scalar(out=r[:, 24:32], in0=r[:, 48:56], scalar1=r[:, 57:58], scalar2=None, op0=ALU.is_equal),
                  reads=[rg], writes=[rg])
                V(lambda e: e.tensor_tensor(out=r[:, 8:9], in0=r[:, 57:58], in1=r[:, 56:57], op=ALU.subtract), reads=[rg], writes=[rg])
                op("act", lambda e: e.activation(out=r[:, 9:10], in_=r[:, 8:9], func=AF.Exp), reads=[rg], writes=[rg])
                V(lambda e: e.tensor_scalar(out=r[:, 9:10], in0=r[:, 9:10], scalar1=1.0, scalar2=None, op0=ALU.add), reads=[rg], writes=[rg])
                V(lambda e: e.reciprocal(out=r[:, 10:11], in_=r[:, 9:10]), reads=[rg], writes=[rg])
                V(lambda e, i=i: e.tensor_tensor(out=wwt[:, i, 0:1], in0=r[:, 10:11], in1=r[:, 3:4], op=ALU.mult), reads=[rg], writes=[wwg])
                V(lambda e, i=i: e.tensor_tensor(out=wwt[:, i, 1:2], in0=r[:, 3:4], in1=wwt[:, i, 0:1], op=ALU.subtract),
                  reads=[rg, wwg], writes=[wwg])
                for kk, (oht, ohg) in enumerate(((oh0t, oh0g), (oh1t, oh1g))):
                    V(lambda e, i=i, kk=kk, oht=oht: e.tensor_tensor(
                        out=oht[:, i, :].rearrange("p (g j) -> p g j", g=4),
                        in0=r[:, 4:8].unsqueeze(2).to_broadcast([128, 4, 8]),
                        in1=r[:, 16 + 8 * kk:24 + 8 * kk].unsqueeze(1).to_broadcast([128, 4, 8]), op=ALU.mult),
                      reads=[rg], writes=[ohg])
                V(lambda e, i=i: e.tensor_tensor(out=aat[:, i, :], in0=oh0t[:, i, :], in1=oh1t[:, i, :], op=ALU.add),
                  reads=[oh0g, oh1g], writes=[aag])

            s1(0)
            for i in range(NT):
                if i + 1 < NT:
                    s1(i + 1)
                s2(i)
            sch.barrier(bar_scr[:])
        with ExitStack() as st:
            NE = NT * 32
            Tt = Buf(st, [128, NT, 32], F32)
            Rt = Buf(st, [128, NT, 32], F32)
            Pt = Buf(st, [128, NT, 32], F32)
            cn = Buf(st, [128, 4, 32], F32)
            tmp = Buf(st, [128, NT, 32], F32)
            slf = Buf(st, [128, NT, 2], F32)
            posf = Buf(st, [128, NT, 4], F32)
            posi = Buf(st, [128, NT, 2], I32)
            qint = Buf(st, [128, 32], I32)
            cmp_ = Buf(st, [128, CAPB, 32], F32)
            bef = Buf(st, [128, CAPB], F32)
            skipf = Buf(st, [128, CAPB], F32)
            tok = Buf(st, [128, NT], F32)
            toki = Buf(st, [128, NT], I32)
            fill = Buf(st, [128, CAPB], I32)
            pq = Buf(st, [128, 512], F32, n=2, ps=True)
            T_, Tg = Tt[0]
            R_, Rg = Rt[0]
            P_, Pg = Pt[0]
            c_, cg_ = cn[0]
            V = lambda f, **kw: op("dve", f, **kw)
            aflat = aat[:].rearrange("p t e -> p (t e)")
            for (dst, dg, lhs) in ((T_, Tg, ones_b), (R_, Rg, triLow_b)):
                dflat = dst[:].rearrange("p t e -> p (t e)")
                for c0 in range(0, NE, 512):
                    c1 = min(NE, c0 + 512)
                    pt, pg = pq[c0 // 512]
                    op("pe", lambda e, c0=c0, c1=c1, pt=pt, lhs=lhs: e.matmul(pt[:, 0:c1 - c0], lhsT=lhs, rhs=aflat[:, c0:c1], start=True, stop=True),
                       reads=[aag, cbg], writes=[pg])
                    V(lambda e, c0=c0, c1=c1, pt=pt, dflat=dflat: e.tensor_copy(out=dflat[:, c0:c1], in_=pt[:, 0:c1 - c0]), reads=[pg], writes=[dg])
            V(lambda e: e.memset(P_[:, 0, :], 0.0), writes=[Pg])
            for i in range(1, NT):
                V(lambda e, i=i: e.tensor_tensor(out=P_[:, i, :], in0=P_[:, i - 1, :], in1=T_[:, i - 1, :], op=ALU.add), reads=[Pg, Tg], writes=[Pg])
            V(lambda e: e.tensor_tensor(out=c_[:, 0, :], in0=P_[:, NT - 1, :], in1=T_[:, NT - 1, :], op=ALU.add), reads=[Pg, Tg], writes=[cg_])
            qi, qig = qint[0]
            V(lambda e: e.tensor_scalar(out=c_[:, 1, :], in0=c_[:, 0, :], scalar1=127.0 - 63.5, scalar2=1.0 / 128.0, op0=ALU.add, op1=ALU.mult),
              reads=[cg_], writes=[cg_])
            V(lambda e: e.tensor_copy(out=qi[:, 0:32], in_=c_[:, 1, :]), reads=[cg_], writes=[qig])
            V(lambda e: e.tensor_copy(out=c_[:, 1, :], in_=qi[:, 0:32]), reads=[qig], writes=[cg_])
            V(lambda e: e.tensor_scalar(out=c_[:, 1, :], in0=c_[:, 1, :], scalar1=128.0, scalar2=None, op0=ALU.mult), reads=[cg_], writes=[cg_])
            V(lambda e: e.tensor_copy(out=c_[:, 2, 0:1], in_=c_[:, 1, 0:1]), reads=[cg_], writes=[cg_])
            for ex in range(1, 32):
                V(lambda e, ex=ex: e.tensor_tensor(out=c_[:, 2, ex:ex + 1], in0=c_[:, 2, ex - 1:ex], in1=c_[:, 1, ex:ex + 1], op=ALU.add),
                  reads=[cg_], writes=[cg_])
            V(lambda e: e.tensor_tensor(out=c_[:, 3, :], in0=c_[:, 2, :], in1=c_[:, 1, :], op=ALU.subtract), reads=[cg_], writes=[cg_])
            V(lambda e: e.tensor_tensor(out=P_[:], in0=P_[:], in1=R_[:], op=ALU.add), reads=[Pg, Rg], writes=[Pg])
            V(lambda e: e.tensor_tensor(out=P_[:], in0=P_[:], in1=c_[:, 3, :].unsqueeze(1).to_broadcast([128, NT, 32]), op=ALU.add),
              reads=[Pg, cg_], writes=[Pg])
            tm, tmg = tmp[0]
            sf, sfg = slf[0]
            for kk, (oht, ohg) in enumerate(((oh0t, oh0g), (oh1t, oh1g))):
                V(lambda e, oht=oht: e.tensor_tensor(out=tm[:], in0=oht[:], in1=P_[:], op=ALU.mult), reads=[ohg, Pg], writes=[tmg])
                V(lambda e, kk=kk: e.tensor_reduce(out=sf[:, :, kk], in_=tm[:], axis=AX.X, op=ALU.add), reads=[tmg], writes=[sfg])
            V(lambda e: e.tensor_copy(out=slt[:], in_=sf[:]), reads=[sfg], writes=[slg])
            pf, pfg = posf[0]
            pi_, pig = posi[0]
            V(lambda e: e.tensor_scalar(out=pf[:, :, 2:4], in0=sf[:], scalar1=-63.5, scalar2=1.0 / 128.0, op0=ALU.add, op1=ALU.mult), reads=[sfg], writes=[pfg])
            V(lambda e: e.tensor_copy(out=pi_[:], in_=pf[:, :, 2:4]), reads=[pfg], writes=[pig])
            V(lambda e: e.tensor_copy(out=pf[:, :, 2:4], in_=pi_[:]), reads=[pig], writes=[pfg])
            V(lambda e: e.scalar_tensor_tensor(out=pf[:, :, 0:2], in0=pf[:, :, 2:4], scalar=-128.0, in1=sf[:], op0=ALU.mult, op1=ALU.add),
              reads=[pfg, sfg], writes=[pfg])
            V(lambda e: e.scalar_tensor_tensor(out=pf[:, :, 0:2], in0=pf[:, :, 0:2], scalar=float(CAPB), in1=pf[:, :, 2:4], op0=ALU.mult, op1=ALU.add),
              reads=[pfg], writes=[pfg])
            V(lambda e: e.tensor_copy(out=pi_[:], in_=pf[:, :, 0:2]), reads=[pfg], writes=[pig])
            cm, cmg = cmp_[0]
            be, beg = bef[0]
            V(lambda e: e.tensor_scalar(out=be[:], in0=iota_b[:, 0:CAPB], scalar1=128.0, scalar2=None, op0=ALU.mult), reads=[cfg_], writes=[beg])
            V(lambda e: e.tensor_tensor(out=cm[:], in0=c_[:, 2, :].unsqueeze(1).to_broadcast([128, CAPB, 32]),
                                        in1=be[:].unsqueeze(2).to_broadcast([128, CAPB, 32]), op=ALU.is_le), reads=[cg_, beg], writes=[cmg])
            V(lambda e: e.tensor_reduce(out=be[:], in_=cm[:], axis=AX.X, op=ALU.add), reads=[cmg], writes=[beg])
            V(lambda e: e.tensor_scalar(out=be[:], in0=be[:], scalar1=31.0, scalar2=None, op0=ALU.min), reads=[beg], writes=[beg])
            sk, skg = skipf[0]
            V(lambda e: e.memset(sk[:], 0.0), writes=[skg])
            V(lambda e: e.tensor_tensor(out=sk[:, 2:CAPB], in0=be[:, 2:CAPB], in1=be[:, 0:CAPB - 2], op=ALU.is_equal), reads=[beg], writes=[skg])
            V(lambda e: e.tensor_scalar(out=be[:], in0=be[:], scalar1=128.0, scalar2=iota_p, op0=ALU.mult, op1=ALU.add), reads=[beg, cfg_], writes=[beg])
            V(lambda e: e.scalar_tensor_tensor(out=be[:], in0=sk[:], scalar=8192.0, in1=be[:], op0=ALU.mult, op1=ALU.add), reads=[skg, beg], writes=[beg])
            wi, wig = widx[0]
            V(lambda e: e.tensor_copy(out=wi[:], in_=be[:]), reads=[beg], writes=[wig])
            tk, tkg = tok[0]
            tki, tkig = toki[0]
            V(lambda e: e.tensor_scalar(out=tk[:], in0=iota_b[:, 0:NT], scalar1=128.0, scalar2=iota_p, op0=ALU.mult, op1=ALU.add),
              reads=[cfg_], writes=[tkg])
            V(lambda e: e.tensor_copy(out=tki[:], in_=tk[:]), reads=[tkg], writes=[tkig])
            fl, flg = fill[0]
            V(lambda e: e.memset(fl[:], NTOK), writes=[flg])
            stok_g = Reg()
            dma("sp", lambda e: e.dma_start(out=stok_d.rearrange("(p b) o -> p (b o)", b=CAPB), in_=fl[:]), reads=[flg], writes=[stok_g])
            for i in range(NT):
                for kk in range(2):
                    dma("pool", lambda e, i=i, kk=kk: e.indirect_dma_start(
                        out=stok_d, out_offset=bass.IndirectOffsetOnAxis(ap=pi_[:, i, kk:kk + 1], axis=0),
                        in_=tki[:, i:i + 1], in_offset=None), reads=[pig, tkig, stok_g], writes=[Reg()])
            sch.barrier(bar_scr[:])
        with ExitStack() as st:
            wi, wig = widx[0]
            sidx = Buf(st, [128, CAPB], I32)
            si, sig = sidx[0]
            dma("sp", lambda e: e.dma_start(out=si[:], in_=stok_d.rearrange("(p b) o -> p (b o)", b=CAPB)), writes=[sig])
            xs = Buf(st, [128, D], BF16, n=2)
            wgu = Buf(st, [128, 8192], BF16, n=2)
            wdn = Buf(st, [128, 4096], BF16, n=2)
            xsT = Buf(st, [128, KC, 128], BF16, n=2)
            sg_ = Buf(st, [128, 256], F32, n=2)
            act_ = Buf(st, [128, 256], BF16, n=2)
            actT = Buf(st, [128, 2, 128], BF16, n=2)
            yo = Buf(st, [128, D], BF16, n=2)
            tpb = Buf(st, [128, 1024], BF16, n=2, ps=True)
            hid = Buf(st, [128, 512], F32, n=2, ps=True)
            yop = Buf(st, [128, 512], F32, n=4, ps=True)
            ys_g = Reg()
            for b in range(CAPB):
                xst, xsg = xs[b]
                dma("pool", lambda e, b=b: e.indirect_dma_start(
                    out=xst[:], out_offset=None, in_=h2_d, in_offset=bass.IndirectOffsetOnAxis(ap=si[:, b:b + 1], axis=0)),
                    reads=[sig], writes=[xsg])
                wgt, wgg = wgu[b]
                wdt, wdg = wdn[b]
                dma("pool", lambda e, b=b: e.indirect_dma_start(
                    out=wgt[:], out_offset=None, in_=wgu_d, in_offset=bass.IndirectOffsetOnAxis(ap=wi[:, b:b + 1], axis=0),
                    bounds_check=bc_val, oob_is_err=False),
                    reads=[wig, wcast_g], writes=[wgg])
                dma("pool", lambda e, b=b: e.indirect_dma_start(
                    out=wdt[:], out_offset=None, in_=wd_d, in_offset=bass.IndirectOffsetOnAxis(ap=wi[:, b:b + 1], axis=0),
                    bounds_check=bc_val, oob_is_err=False),
                    reads=[wig, wcast_g], writes=[wdg])
                xTt, xTg = xsT[b]
                xv = xst[:].rearrange("p (q kc) -> p kc q", kc=KC)
                for half in range(2):
                    tpt, tpg = tpb[half]
                    for k8 in range(8):
                        kc = half * 8 + k8
                        op("pe", lambda e, kc=kc, k8=k8, tpt=tpt: e.transpose(tpt[:, k8 * 128:(k8 + 1) * 128], xv[:, kc, :], ident_b),
                           reads=[xsg, cbg], writes=[tpg], signal=(k8 == 7))
                    op("act" if half == 0 else "dve",
                       lambda e, half=half, tpt=tpt: (e.activation(out=xTt[:, half * 8:(half + 1) * 8, :].rearrange("p a b -> p (a b)"),
                                                                   in_=tpt[:], func=AF.Copy) if half == 0 else
                                                      e.tensor_copy(out=xTt[:, half * 8:(half + 1) * 8, :].rearrange("p a b -> p (a b)"), in_=tpt[:])),
                       reads=[tpg], writes=[xTg])
                ht, hg = hid[b]
                for gu_ in range(2):
                    for kc in range(KC):
                        op("pe", lambda e, kc=kc, gu_=gu_: e.matmul(ht[:, gu_ * 256:(gu_ + 1) * 256], lhsT=xTt[:, kc, :],
                                                                   rhs=wgt[:, gu_ * 4096 + kc * 256:gu_ * 4096 + (kc + 1) * 256],
                                                                   start=(kc == 0), stop=(kc == KC - 1)),
                           reads=[xTg, wgg], writes=[hg], signal=(kc == KC - 1 and gu_ == 1))
                sgt, sgg = sg_[b]
                op("act", lambda e: e.activation(out=sgt[:], in_=ht[:, 0:256], func=AF.Silu), reads=[hg], writes=[sgg])
                att, atg = act_[b]
                op("dve", lambda e: e.tensor_tensor(out=att[:], in0=sgt[:], in1=ht[:, 256:512], op=ALU.mult), reads=[sgg, hg], writes=[atg])
                aTt, aTg = actT[b]
                tpt, tpg = tpb[0]
                av = att[:].rearrange("p (q c) -> p c q", c=2)
                for c in range(2):
                    op("pe", lambda e, c=c: e.transpose(tpt[:, c * 128:(c + 1) * 128], av[:, c, :], ident_b),
                       reads=[atg, cbg], writes=[tpg], signal=(c == 1))
                op("act", lambda e: e.activation(out=aTt[:].rearrange("p a b -> p (a b)"), in_=tpt[:, 0:256], func=AF.Copy), reads=[tpg], writes=[aTg])
                yot, yog = yo[b]
                for nb in range(4):
                    ypt, ypg = yop[nb]
                    for c in range(2):
                        op("pe", lambda e, c=c, nb=nb, ypt=ypt: e.matmul(ypt[:], lhsT=aTt[:, c, :],
                                                                       rhs=wdt[:, c * 2048 + nb * 512:c * 2048 + (nb + 1) * 512],
                                                                       start=(c == 0), stop=(c == 1)),
                           reads=[aTg, wdg], writes=[ypg], signal=(c == 1))
                    op("act" if nb % 2 == 0 else "dve",
                       lambda e, nb=nb, ypt=ypt: (e.activation(out=yot[:, nb * 512:(nb + 1) * 512], in_=ypt[:], func=AF.Copy) if nb % 2 == 0
                                                  else e.tensor_copy(out=yot[:, nb * 512:(nb + 1) * 512], in_=ypt[:])),
                       reads=[ypg], writes=[yog])
                dma("act", lambda e, b=b: e.dma_start(out=ys_d[b * 128:(b + 1) * 128, :], in_=yot[:]), reads=[yog], writes=[ys_g])
            sch.barrier(bar_scr[:])
        with ExitStack() as st:
            gate2 = Buf(st, [128, D], F32)
            dma("sp", lambda e: e.dma_start(out=gate2[0][0][:], in_=modl_d[l, 5]), writes=[gate2[0][1]])
            p0 = Buf(st, [128, D], BF16, n=2)
            p1 = Buf(st, [128, D], BF16, n=2)
            xmb = Buf(st, [128, D], F32, n=2)
            mm = Buf(st, [128, D], F32, n=2)
            out_g = Reg()
            for i in range(NT):
                a0, a0g = p0[i]
                a1, a1g = p1[i]
                dma("pool", lambda e, i=i: e.indirect_dma_start(
                    out=a0[:], out_offset=None, in_=ys_d, in_offset=bass.IndirectOffsetOnAxis(ap=slt[:, i, 0:1], axis=0)),
                    reads=[slg], writes=[a0g])
                dma("pool", lambda e, i=i: e.indirect_dma_start(
                    out=a1[:], out_offset=None, in_=ys_d, in_offset=bass.IndirectOffsetOnAxis(ap=slt[:, i, 1:2], axis=0)),
                    reads=[slg], writes=[a1g])
                xt, xg = xmb[i]
                dma("sp", lambda e, i=i: e.dma_start(out=xt[:], in_=xm_d[i * 128:(i + 1) * 128, :]), writes=[xg])
                mt, mg = mm[i]
                op("dve", lambda e, i=i: e.tensor_scalar(out=mt[:], in0=a0[:], scalar1=wwt[:, i, 0:1], scalar2=None, op0=ALU.mult),
                   reads=[a0g, wwg], writes=[mg])
                op("dve", lambda e, i=i: e.scalar_tensor_tensor(out=mt[:], in0=a1[:], scalar=wwt[:, i, 1:2], in1=mt[:], op0=ALU.mult, op1=ALU.add),
                   reads=[a1g, wwg, mg], writes=[mg])
                op("dve", lambda e: e.tensor_tensor(out=mt[:], in0=mt[:], in1=gate2[0][0][:], op=ALU.mult), reads=[mg, gate2[0][1]], writes=[mg])
                op("pool", lambda e: e.tensor_tensor(out=mt[:], in0=mt[:], in1=xt[:], op=ALU.add), reads=[mg, xg], writes=[mg])
                dma("pool", lambda e, i=i: e.dma_start(out=x_dst[i * 128:(i + 1) * 128, :], in_=mt[:]), reads=[mg], writes=[out_g])
            sch.barrier(bar_scr[:])
        moe.close()
    sch.final_wait()
    return nc, sch


def make_consts():
    c = np.zeros((128, 1280), np.float32)
    i = np.arange(128)
    c[:, 0:128] = np.eye(128)
    c[:, 128:256] = (i[:, None] >= i[None, :])
    c[:, 256:384] = (i[:, None] < i[None, :])
    c[:, 384:512] = (i[:, None] < i[None, :])
    c[:, 512:640] = 1.0
    c[:, 640:768] = (i[:, None] <= i[None, :])
    c[:, 768] = i
    c[:, 1024:1280] = np.arange(256)[None, :]
    return c


_CACHE = {}


def run(inputs, trace=False):
    x = np.asarray(inputs["x"], np.float32)
    B, S, _ = x.shape
    G = 1
    NCORES = B
    NBLK = S // 128
    NT = NBLK // G
    depth = inputs["w_in"].shape[0]
    key = (NT, NBLK, G, depth)
    if key not in _CACHE:
        _CACHE[key] = build(NT, NBLK, G, depth)
    nc, sch = _CACHE[key]
    f = lambda a: np.ascontiguousarray(np.asarray(a, np.float32))
    cst = make_consts()
    tri = cst[:, 384:512]
    ones = np.ones((128, 128), np.float32)
    zeros = np.zeros((128, 128), np.float32)
    shared = {
        "w_mod": f(inputs["w_mod"]), "b_mod": f(inputs["b_mod"]).reshape(1, -1), "mod_layer": f(inputs["mod_layer"]),
        "norm1_g": f(inputs["norm1_g"]), "w_in": f(inputs["w_in"]), "gm_norm_g": f(inputs["gm_norm_g"]),
        "gm_wsT": f(np.asarray(inputs["gm_ws"]).transpose(0, 1, 3, 2)), "gm_bsT": f(np.asarray(inputs["gm_bs"]).transpose(0, 2, 1)),
        "q_norm_g": f(inputs["q_norm_g"]), "k_norm_g": f(inputs["k_norm_g"]), "out_norm_g": f(inputs["out_norm_g"]),
        "w_out": f(inputs["w_out"]), "norm2_g": f(inputs["norm2_g"]),
        "w_r": f(np.concatenate([np.asarray(inputs["w_group"]), np.asarray(inputs["w_route"])], axis=-1)),
        "b_r": f(np.concatenate([np.asarray(inputs["b_group"]), np.asarray(inputs["b_route"])], axis=-1)),
        "w_gate": f(inputs["w_gate"]), "w_up": f(inputs["w_up"]), "w_down": f(inputs["w_down"]),
        "consts": cst,
    }
    in_maps = []
    owns = []
    for c in range(NCORES):
        b, r = c // G, c % G
        ob = own_blocks(G, NT, r)
        owns.append((b, ob))
        xs = np.concatenate([x[b, n * 128:(n + 1) * 128] for n in ob], axis=0)
        if G == 1:
            am = np.stack([tri, ones, tri, ones], axis=1)
        elif r == 0:
            am = np.stack([zeros, tri, tri, ones], axis=1)
        else:
            am = np.stack([tri, ones, zeros, tri], axis=1)
        m = dict(shared)
        m["x"] = np.ascontiguousarray(xs)
        m["cT"] = f(np.asarray(inputs["c"])[b].reshape(KC, 128).T)
        m["amask"] = np.ascontiguousarray(am.astype(np.float32))
        in_maps.append(m)
    res = run_bass_kernel_spmd(nc, in_maps, core_ids=list(range(NCORES)), **({"trace": True} if trace else {}))
    out = np.empty_like(x)
    for c in range(NCORES):
        b, ob = owns[c]
        o = res.results[c]["out"]
        for t, n in enumerate(ob):
            out[b, n * 128:(n + 1) * 128] = o[t * 128:(t + 1) * 128]
    return out, res


def kernel(**inputs):
    out, _ = run(inputs)
    return out
```

```python
import numpy as np
import ml_dtypes
from contextlib import ExitStack
import concourse.bass as bass
import concourse.mybir as mybir
from concourse.bass_utils import run_bass_kernel_spmd

F32 = mybir.dt.float32
BF16 = mybir.dt.bfloat16
I32 = mybir.dt.int32
AF = mybir.ActivationFunctionType
ALU = mybir.AluOpType
AX = mybir.AxisListType

D = 2048
KC = 16
DIN = 2560
NEXP = 32
EPS = 1e-6
DEPTH = 4
NMOD = 6


class Reg:
    __slots__ = ("w", "r")

    def __init__(self):
        self.w = None
        self.r = {}


class Sched:
    NDS = 12

    def __init__(self, nc):
        self.nc = nc
        self.engs = {"pe": nc.tensor, "act": nc.scalar, "dve": nc.vector, "pool": nc.gpsimd, "sp": nc.sync}
        self.sem = {k: nc.semaphore("sem_" + k).__enter__() for k in self.engs}
        self.cnt = {k: 0 for k in self.engs}
        self.waited = {k: {} for k in self.engs}
        self.dsems = {}
        self.dcnt = {}
        self.dnext = {}
        for q in ("sp", "pool", "act"):
            self.dsems[q] = [nc.semaphore("ds_%s%d" % (q, i)).__enter__() for i in range(self.NDS)]
            self.dcnt[q] = [0] * self.NDS
            self.dnext[q] = 0
        self.n_inst = 0

    def _semobj(self, key):
        if isinstance(key, str):
            return self.sem[key]
        return self.dsems[key[0]][key[1]]

    def _wait(self, e, key, val):
        if val <= 0 or self.waited[e].get(key, 0) >= val:
            return
        self.engs[e].wait_ge(self._semobj(key), val)
        self.waited[e][key] = val
        self.n_inst += 1

    def _deps(self, e, reads, writes):
        deps = {}
        for r in reads:
            if r.w is not None:
                k, v = r.w
                deps[k] = max(deps.get(k, 0), v)
        for w in writes:
            if w.w is not None:
                k, v = w.w
                deps[k] = max(deps.get(k, 0), v)
            for k, v in w.r.items():
                deps[k] = max(deps.get(k, 0), v)
        for k, v in deps.items():
            if e == "pe" and k == "pe":
                continue
            self._wait(e, k, v)

    def _mark(self, ev, reads, writes):
        k, v = ev
        for r in reads:
            r.r[k] = max(r.r.get(k, 0), v)
        for w in writes:
            w.w = ev
            w.r = {}

    def op(self, e, fn, reads=(), writes=(), signal=True):
        self._deps(e, reads, writes)
        inst = fn(self.engs[e])
        self.n_inst += 1
        if signal:
            self.cnt[e] += 1
            inst.then_inc(self.sem[e], 1)
            ev = (e, self.cnt[e])
        else:
            ev = (e, self.cnt[e] + 1)
        self._mark(ev, reads, writes)

    def dma(self, q, fn, reads=(), writes=()):
        self._deps(q, reads, writes)
        i = self.dnext[q]
        self.dnext[q] = (i + 1) % self.NDS
        key = (q, i)
        self._wait(q, key, self.dcnt[q][i])
        inst = fn(self.engs[q])
        self.n_inst += 1
        self.dcnt[q][i] += 16
        inst.then_inc(self.dsems[q][i], 16)
        self._mark((key, self.dcnt[q][i]), reads, writes)

    def barrier(self, scratch_ap):
        for k in self.engs:
            if k != "pool":
                self._wait("pool", k, self.cnt[k])
        for q in self.dsems:
            if q == "wc":
                continue
            for i in range(len(self.dsems[q])):
                self._wait("pool", (q, i), self.dcnt[q][i])
        inst = self.nc.gpsimd.memset(scratch_ap, 0.0)
        self.cnt["pool"] += 1
        inst.then_inc(self.sem["pool"], 1)
        self.n_inst += 1
        for k in self.engs:
            if k != "pool":
                self._wait(k, "pool", self.cnt["pool"])

    def final_wait(self):
        for q in self.dsems:
            for i in range(len(self.dsems[q])):
                self._wait("sp", (q, i), self.dcnt[q][i])


def own_blocks(G, NT, rank):
    if G == 1:
        return list(range(NT))
    out = []
    for t in range(NT):
        j = t // 2
        if t % 2 == 0:
            out.append(4 * j + (0 if rank == 0 else 1))
        else:
            out.append(4 * j + (3 if rank == 0 else 2))
    return out


def blk_src(G, n):
    if G == 1:
        return (0, n)
    j, c = n // 4, n % 4
    return {0: (0, 2 * j), 1: (1, 2 * j), 2: (1, 2 * j + 1), 3: (0, 2 * j + 1)}[c]


def build(NT, NBLK, G, depth=DEPTH):
    S = NBLK * 128
    NTOK = NT * 128
    CAPB = (2 * NTOK) // 128 + NEXP
    CAP = CAPB * 128
    nc = bass.Bass("TRN2", target_bir_lowering=False)
    dt_in = lambda n, s, d=F32: nc.dram_tensor(n, s, d, kind="ExternalInput").ap()
    dt_sc = lambda n, s, d=F32: nc.dram_tensor(n, s, d, kind="Internal").ap()
    x_in = dt_in("x", [NTOK, D])
    cT_in = dt_in("cT", [128, KC])
    w_mod = dt_in("w_mod", [D, NMOD * D])
    b_mod = dt_in("b_mod", [1, NMOD * D])
    mod_layer = dt_in("mod_layer", [depth, NMOD * D])
    norm1_g = dt_in("norm1_g", [depth, D])
    w_in = dt_in("w_in", [depth, D, DIN])
    gm_norm_g = dt_in("gm_norm_g", [depth, 512])
    gm_wsT = dt_in("gm_wsT", [depth, 4, 128, 128])
    gm_bsT = dt_in("gm_bsT", [depth, 128, 4])
    q_norm_g = dt_in("q_norm_g", [depth, 128])
    k_norm_g = dt_in("k_norm_g", [depth, 128])
    out_norm_g = dt_in("out_norm_g", [depth, 1024])
    w_out = dt_in("w_out", [depth, 1024, D])
    norm2_g = dt_in("norm2_g", [depth, D])
    w_r = dt_in("w_r", [depth, D, 36])
    b_r = dt_in("b_r", [depth, 36])
    w_gate = dt_in("w_gate", [depth, NEXP, D, 256])
    w_up = dt_in("w_up", [depth, NEXP, D, 256])
    w_down = dt_in("w_down", [depth, NEXP, 256, D])
    consts = dt_in("consts", [128, 1280])
    amask_in = dt_in("amask", [128, 4, 128])
    out_d = nc.dram_tensor("out", [NTOK, D], F32, kind="ExternalOutput").ap()

    xa_d = dt_sc("xa_d", [NTOK, D])
    xm_d = dt_sc("xm_d", [NTOK, D])
    modl_d = dt_sc("modl_d", [depth, NMOD, 128, D])
    kT_d = dt_sc("kT_d", [NT, 128, 512], BF16)
    v_d = dt_sc("v_d", [NT, 128, 512], BF16)
    if G > 1:
        kTall_d = dt_sc("kTall_d", [G * NT, 128, 512], BF16)
        vall_d = dt_sc("vall_d", [G * NT, 128, 512], BF16)
    else:
        kTall_d, vall_d = kT_d, v_d
    qT_d = dt_sc("qT_d", [NT, 128, 512], BF16)
    yT_d = dt_sc("yT_d", [NT, 128, 1024], BF16)
    h2_d = dt_sc("h2_d", [NTOK + 1, D], BF16)
    wgu_d = dt_sc("wgu_d", [NEXP * 128, 8192], BF16)
    wd_d = dt_sc("wd_d", [NEXP * 128, 4096], BF16)
    stok_d = dt_sc("stok_d", [CAP, 1], I32)
    ys_d = dt_sc("ys_d", [CAP, D], BF16)

    sch = Sched(nc)
    op, dma = sch.op, sch.dma
    uid = [0]

    def alloc(st, shape, dt, ps=False):
        uid[0] += 1
        f = nc.psum_tensor if ps else nc.sbuf_tensor
        return st.enter_context(f("t%d" % uid[0], shape, dt))

    class Buf:
        def __init__(self, st, shape, dt, n=1, ps=False):
            self.t = [alloc(st, shape, dt, ps) for _ in range(n)]
            self.g = [Reg() for _ in range(n)]
            self.n = n

        def __getitem__(self, i):
            return self.t[i % self.n], self.g[i % self.n]

    top = ExitStack()
    cst_f = Buf(top, [128, 1280], F32)
    cst_b = Buf(top, [128, 1280], BF16)
    bar_scr = alloc(top, [128, 1], F32)
    cf, cfg_ = cst_f[0]
    cb, cbg = cst_b[0]
    dma("sp", lambda e: e.dma_start(out=cf[:], in_=consts), writes=[cfg_])
    op("dve", lambda e: e.tensor_copy(out=cb[:], in_=cf[:]), reads=[cfg_], writes=[cbg])
    ident_f = cf[:, 0:128]
    ident_b = cb[:, 0:128]
    triIncl_b = cb[:, 128:256]
    triLow_b = cb[:, 256:384]
    ones_b = cb[:, 512:640]
    gmMaskT_f = cf[:, 640:768]
    iota_p = cf[:, 768:769]
    iota_b = cf[:, 1024:1280]
    amask = Buf(top, [128, 4, 128], F32)
    am, amg = amask[0]
    dma("sp", lambda e: e.dma_start(out=am[:], in_=amask_in), writes=[amg])
    zrow = Buf(top, [1, D], BF16)
    zr, zrg = zrow[0]
    op("pool", lambda e: e.memset(zr[:], 0.0), writes=[zrg])
    h2z_g = Reg()
    dma("sp", lambda e: e.dma_start(out=h2_d[NTOK:NTOK + 1, :], in_=zr[:]), reads=[zrg], writes=[h2z_g])

    def rstd_from(ssq_ap, out_ap, n, greads, gwrite):
        op("dve", lambda e: e.tensor_scalar(out=out_ap, in0=ssq_ap, scalar1=1.0 / n, scalar2=EPS, op0=ALU.mult, op1=ALU.add),
           reads=greads, writes=[gwrite])
        op("act", lambda e: e.activation(out=out_ap, in_=out_ap, func=AF.Ln), reads=[gwrite], writes=[gwrite])
        op("act", lambda e: e.activation(out=out_ap, in_=out_ap, func=AF.Exp, scale=-0.5), reads=[gwrite], writes=[gwrite])

    with ExitStack() as st:
        cTt = Buf(st, [128, KC], F32)
        sc_b = Buf(st, [128, KC, 128], F32)
        wm = Buf(st, [128, KC, 512], F32, n=2)
        msh = Buf(st, [128, NMOD * D], F32)
        pm = Buf(st, [128, 512], F32, n=2, ps=True)
        bm = Buf(st, [128, 2048], F32, n=2)
        g_t = Buf(st, [128, 2048], F32, n=2)
        o_t = Buf(st, [128, 2048], F32, n=2)
        c_t, c_g = cTt[0]
        s_t, s_g = sc_b[0]
        m_t, m_g = msh[0]
        dma("sp", lambda e: e.dma_start(out=c_t[:], in_=cT_in), writes=[c_g])
        op("act", lambda e: e.activation(out=c_t[:], in_=c_t[:], func=AF.Silu), reads=[c_g], writes=[c_g])
        for kc in range(KC):
            op("dve", lambda e, kc=kc: e.tensor_copy(out=s_t[:, kc, :], in_=c_t[:, kc:kc + 1].to_broadcast([128, 128])),
               reads=[c_g], writes=[s_g])
        NG = NMOD * D // 512
        for n in range(NG):
            w_t, w_g = wm[n]
            dma("sp", lambda e, n=n, w_t=w_t: e.dma_start(
                out=w_t[:], in_=w_mod[:, n * 512:(n + 1) * 512].rearrange("(kc p) n -> p kc n", p=128)), writes=[w_g])
            p_t, p_g = pm[n]
            for kc in range(KC):
                op("pe", lambda e, kc=kc, p_t=p_t, w_t=w_t: e.matmul(p_t[:], lhsT=s_t[:, kc, :], rhs=w_t[:, kc, :],
                                                                   start=(kc == 0), stop=(kc == KC - 1)),
                   reads=[s_g, w_g], writes=[p_g], signal=(kc == KC - 1))
            op("act", lambda e, n=n, p_t=p_t: e.activation(out=m_t[:, n * 512:(n + 1) * 512], in_=p_t[:], func=AF.Copy),
               reads=[p_g], writes=[m_g])
        for j in range(NMOD):
            b_t, b_g = bm[j]
            dma("sp", lambda e, j=j, b_t=b_t: e.dma_start(out=b_t[:], in_=b_mod[0:1, j * D:(j + 1) * D].partition_broadcast(128)),
                writes=[b_g])
            op("dve", lambda e, j=j, b_t=b_t: e.tensor_tensor(out=m_t[:, j * D:(j + 1) * D], in0=m_t[:, j * D:(j + 1) * D],
                                                             in1=b_t[:], op=ALU.add), reads=[b_g, m_g], writes=[m_g])
        modl_g = Reg()
        k = 0
        for l in range(depth):
            for j in range(NMOD):
                b_t, b_g = bm[k]
                o_tt, o_g = o_t[k]
                dma("sp", lambda e, l=l, j=j, b_t=b_t: e.dma_start(
                    out=b_t[:], in_=mod_layer[l:l + 1, j * D:(j + 1) * D].partition_broadcast(128)), writes=[b_g])
                if j in (1, 4):
                    gg_t, gg_g = g_t[k]
                    gsrc = norm1_g if j == 1 else norm2_g
                    dma("sp", lambda e, l=l, gg_t=gg_t, gsrc=gsrc: e.dma_start(
                        out=gg_t[:], in_=gsrc[l:l + 1, :].partition_broadcast(128)), writes=[gg_g])
                    op("dve", lambda e, j=j, b_t=b_t, o_tt=o_tt: e.scalar_tensor_tensor(
                        out=o_tt[:], in0=m_t[:, j * D:(j + 1) * D], scalar=1.0, in1=b_t[:], op0=ALU.add, op1=ALU.add),
                       reads=[m_g, b_g], writes=[o_g])
                    op("dve", lambda e, o_tt=o_tt, gg_t=gg_t: e.tensor_tensor(out=o_tt[:], in0=o_tt[:], in1=gg_t[:], op=ALU.mult),
                       reads=[o_g, gg_g], writes=[o_g])
                else:
                    op("dve", lambda e, j=j, b_t=b_t, o_tt=o_tt: e.tensor_tensor(
                        out=o_tt[:], in0=m_t[:, j * D:(j + 1) * D], in1=b_t[:], op=ALU.add), reads=[m_g, b_g], writes=[o_g])
                dma("sp", lambda e, l=l, j=j, o_tt=o_tt: e.dma_start(out=modl_d[l, j], in_=o_tt[:]), reads=[o_g], writes=[modl_g])
                k += 1
        sch.barrier(bar_scr[:])

    bc_reg = nc.gpsimd.register("bc_reg").__enter__()
    nc.gpsimd.reg_mov(bc_reg, NEXP * 128 - 1)
    bc_val = nc.gpsimd.snap(bc_reg)
    wcast_g = Reg()
    wc_sem = nc.semaphore("wcast_sem").__enter__()
    sch.dsems["wc"] = [wc_sem]
    sch.dcnt["wc"] = [0]

    def cast_expert_weights(l, ex):
        for (o, i_) in ((wgu_d[ex * 128:(ex + 1) * 128, 0:4096], w_gate[l, ex].rearrange("(p kc) f -> p (kc f)", kc=KC)),
                        (wgu_d[ex * 128:(ex + 1) * 128, 4096:8192], w_up[l, ex].rearrange("(p kc) f -> p (kc f)", kc=KC)),
                        (wd_d[ex * 128:(ex + 1) * 128, :], w_down[l, ex].rearrange("(p c) n -> p (c n)", c=2))):
            inst = nc.gpsimd.dma_start(out=o, in_=i_)
            sch.dcnt["wc"][0] += 16
            inst.then_inc(wc_sem, 16)
            sch.n_inst += 1
        wcast_g.w = (("wc", 0), sch.dcnt["wc"][0])

    def bload(q, t, g, src_row):
        dma(q, lambda e: e.dma_start(out=t, in_=src_row.partition_broadcast(128)), writes=[g])

    for l in range(depth):
        x_src = x_in if l == 0 else xa_d
        x_dst = out_d if l == depth - 1 else xa_d
        lay = ExitStack()
        with ExitStack() as st:
            winb = Buf(st, [128, KC, DIN], BF16)
            wi_t, wi_g = winb[0]
            for kc in range(KC):
                dma("pool", lambda e, kc=kc: e.dma_start(out=wi_t[:, kc, :], in_=w_in[l, kc * 128:(kc + 1) * 128, :]), writes=[wi_g])
            gs1 = Buf(st, [128, D], F32)
            sh1 = Buf(st, [128, D], F32)
            dma("sp", lambda e: e.dma_start(out=gs1[0][0][:], in_=modl_d[l, 1]), writes=[gs1[0][1]])
            dma("sp", lambda e: e.dma_start(out=sh1[0][0][:], in_=modl_d[l, 0]), writes=[sh1[0][1]])
            wmT_f = Buf(st, [128, 4, 128], F32)
            wmT = Buf(st, [128, 4, 128], BF16)
            dma("sp", lambda e: e.dma_start(out=wmT_f[0][0][:], in_=gm_wsT[l].rearrange("g p t -> p g t")), writes=[wmT_f[0][1]])
            op("dve", lambda e: e.tensor_tensor(out=wmT[0][0][:], in0=wmT_f[0][0][:],
                                               in1=gmMaskT_f.unsqueeze(1).to_broadcast([128, 4, 128]), op=ALU.mult),
               reads=[wmT_f[0][1], cfg_], writes=[wmT[0][1]])
            bsT = Buf(st, [128, 4], F32)
            dma("sp", lambda e: e.dma_start(out=bsT[0][0][:], in_=gm_bsT[l]), writes=[bsT[0][1]])
            gmn = Buf(st, [128, 512], F32)
            bload("sp", gmn[0][0][:], gmn[0][1], gm_norm_g[l:l + 1, :])
            oga = Buf(st, [128, 512], F32)
            bload("sp", oga[0][0][:], oga[0][1], out_norm_g[l:l + 1, 0:512])
            gq = Buf(st, [128, 128], F32)
            gk = Buf(st, [128, 128], F32)
            bload("sp", gq[0][0][:], gq[0][1], q_norm_g[l:l + 1, :])
            bload("sp", gk[0][0][:], gk[0][1], k_norm_g[l:l + 1, :])
            op("dve", lambda e: e.tensor_scalar(out=gq[0][0][:], in0=gq[0][0][:], scalar1=128.0 ** -0.5, scalar2=None, op0=ALU.mult),
               reads=[gq[0][1]], writes=[gq[0][1]])
            xb = Buf(st, [128, D], F32, n=2)
            ssb = Buf(st, [128, 2], F32, n=2)
            hf = Buf(st, [128, D], F32, n=1)
            hb = Buf(st, [128, D], BF16, n=2)
            hT = Buf(st, [128, KC, 128], BF16, n=2)
            gu = Buf(st, [128, 512], F32, n=2)
            gv = Buf(st, [128, 512], F32, n=2)
            sq = Buf(st, [128, 512], F32, n=2)
            sqq = Buf(st, [128, 1024], F32, n=1)
            sm = Buf(st, [128, 16], F32, n=2)
            sm2 = Buf(st, [128, 16], F32, n=2)
            vn = Buf(st, [128, 512], BF16, n=2)
            t1 = Buf(st, [128, 512], F32, n=1)
            ynb = Buf(st, [128, 512], BF16, n=2)
            qn = Buf(st, [128, 1024], BF16, n=2)
            qf = Buf(st, [128, 1024], F32, n=1)
            yst = Buf(st, [128, 512], BF16, n=2)
            kst = Buf(st, [128, 512], BF16, n=2)
            qst = Buf(st, [128, 512], BF16, n=2)
            vst = Buf(st, [128, 512], BF16, n=2)
            tp = Buf(st, [128, 1024], BF16, n=2, ps=True)
            pp = [Buf(st, [128, 512], F32, n=1, ps=True) for _ in range(5)]
            sgp = Buf(st, [128, 512], F32, n=1, ps=True)
            yTa_g = Reg()
            kv_g = Reg()
            G4 = lambda ap: ap.rearrange("p (g k) -> p g k", g=4)

            def front_a(i):
                for ex in range(i * NEXP // NT, (i + 1) * NEXP // NT):
                    cast_expert_weights(l, ex)
                xt, xg = xb[i]
                dma("sp", lambda e: e.dma_start(out=xt[:], in_=x_src[i * 128:(i + 1) * 128, :]), writes=[xg])
                s_t, s_g = ssb[i]
                hft, hfg = hf[i]
                hbt, hbg = hb[i]
                op("act", lambda e: e.activation(out=hbt[:], in_=xt[:], func=AF.Square, accum_out=s_t[:, 0:1]),
                   reads=[xg], writes=[hbg, s_g])
                rstd_from(s_t[:, 0:1], s_t[:, 1:2], D, [s_g], s_g)
                op("dve", lambda e: e.scalar_tensor_tensor(out=hft[:], in0=xt[:], scalar=s_t[:, 1:2], in1=gs1[0][0][:],
                                                          op0=ALU.mult, op1=ALU.mult), reads=[xg, s_g, gs1[0][1]], writes=[hfg])
                op("pool", lambda e: e.tensor_tensor(out=hbt[:], in0=hft[:], in1=sh1[0][0][:], op=ALU.add),
                   reads=[hfg, sh1[0][1]], writes=[hbg])

            def front_b(i):
                hbt, hbg = hb[i]
                hTt, hTg = hT[i]
                for half in range(2):
                    tpt, tpg = tp[half]
                    for k8 in range(8):
                        kc = half * 8 + k8
                        op("pe", lambda e, kc=kc, k8=k8: e.transpose(tpt[:, k8 * 128:(k8 + 1) * 128],
                                                                    hbt[:, kc * 128:(kc + 1) * 128], ident_b),
                           reads=[hbg, cbg], writes=[tpg], signal=(k8 == 7))
                    dst = hTt[:, half * 8:(half + 1) * 8, :].rearrange("p a b -> p (a b)")
                    if half == 0:
                        op("act", lambda e: e.activation(out=dst, in_=tpt[:], func=AF.Copy), reads=[tpg], writes=[hTg])
                    else:
                        op("dve", lambda e: e.tensor_copy(out=dst, in_=tpt[:]), reads=[tpg], writes=[hTg])

            def mid(i, nbs=range(5)):
                hTt, hTg = hT[i]
                for nb in nbs:
                    ppt, ppg = pp[nb][0]
                    for kc in range(KC):
                        op("pe", lambda e, kc=kc: e.matmul(ppt[:], lhsT=hTt[:, kc, :], rhs=wi_t[:, kc, nb * 512:(nb + 1) * 512],
                                                          start=(kc == 0), stop=(kc == KC - 1)),
                           reads=[hTg, wi_g], writes=[ppg], signal=(kc == KC - 1))

            def back1(i):
                gut, gug = gu[i]
                gvt, gvg = gv[i]
                op("act", lambda e: e.activation(out=gut[:], in_=pp[0][0][0][:], func=AF.Gelu), reads=[pp[0][0][1]], writes=[gug])
                op("act", lambda e: e.activation(out=gvt[:], in_=pp[1][0][0][:], func=AF.Gelu), reads=[pp[1][0][1]], writes=[gvg])
                sqqt, sqqg = sqq[i]
                sm2t, sm2g = sm2[i]
                for which in range(2):
                    ppt, ppg = pp[2 + which][0]
                    op("act", lambda e: e.activation(out=sqqt[:, which * 512:(which + 1) * 512], in_=ppt[:], func=AF.Square),
                       reads=[ppg], writes=[sqqg])
                vstt, vstg = vst[i]
                op("act", lambda e: e.activation(out=vstt[:], in_=pp[4][0][0][:], func=AF.Copy), reads=[pp[4][0][1]], writes=[vstg])
                dma("act", lambda e: e.dma_start(out=v_d[i], in_=vstt[:]), reads=[vstg], writes=[kv_g])
                op("dve", lambda e: e.tensor_reduce(out=sm2t[:, 0:8], in_=sqqt[:].rearrange("p (g k) -> p g k", g=8), axis=AX.X, op=ALU.add),
                   reads=[sqqg], writes=[sm2g])
                rstd_from(sm2t[:, 0:8], sm2t[:, 8:16], 128, [sm2g], sm2g)
                qft, qfg = qf[i]
                qnt, qng = qn[i]
                for which in range(2):
                    ppt, ppg = pp[2 + which][0]
                    c0 = which * 512
                    op("dve", lambda e: e.tensor_tensor(out=G4(qft[:, c0:c0 + 512]), in0=G4(ppt[:]),
                                                       in1=sm2t[:, 8 + which * 4:12 + which * 4].unsqueeze(2).to_broadcast([128, 4, 128]), op=ALU.mult),
                       reads=[ppg, sm2g], writes=[qfg])
                    gsel = gq if which == 0 else gk
                    op("dve", lambda e: e.tensor_tensor(out=G4(qnt[:, c0:c0 + 512]), in0=G4(qft[:, c0:c0 + 512]),
                                                       in1=gsel[0][0][:].unsqueeze(1).to_broadcast([128, 4, 128]), op=ALU.mult),
                       reads=[qfg, gsel[0][1]], writes=[qng])

            def back1v(i):
                gvt, gvg = gv[i]
                sqt, sqg = sq[i]
                smt, smg = sm[i]
                op("pool", lambda e: e.tensor_tensor(out=sqt[:], in0=gvt[:], in1=gvt[:], op=ALU.mult), reads=[gvg], writes=[sqg])
                op("dve", lambda e: e.tensor_reduce(out=smt[:, 0:4], in_=G4(sqt[:]), axis=AX.X, op=ALU.add), reads=[sqg], writes=[smg])
                rstd_from(smt[:, 0:4], smt[:, 4:8], 128, [smg], smg)
                vnt, vng = vn[i]
                op("dve", lambda e: e.tensor_tensor(out=G4(vnt[:]), in0=G4(gvt[:]),
                                                   in1=smt[:, 4:8].unsqueeze(2).to_broadcast([128, 4, 128]), op=ALU.mult),
                   reads=[gvg, smg], writes=[vng])

            def sgmm(i):
                vnt, vng = vn[i]
                sgt, sgg = sgp[0]
                for g in range(4):
                    op("pe", lambda e, g=g: e.matmul(sgt[:, g * 128:(g + 1) * 128], lhsT=wmT[0][0][:, g, :],
                                                    rhs=vnt[:, g * 128:(g + 1) * 128], start=True, stop=True),
                       reads=[wmT[0][1], vng], writes=[sgg], signal=(g == 3))

            def back2(i):
                gut, gug = gu[i]
                sqt, sqg = sq[i]
                smt, smg = sm[i]
                sgt, sgg = sgp[0]
                t1t, t1g = t1[i]
                op("dve", lambda e: e.tensor_tensor(out=t1t[:], in0=sgt[:], in1=gmn[0][0][:], op=ALU.mult),
                   reads=[sgg, gmn[0][1]], writes=[t1g])
                op("dve", lambda e: e.tensor_tensor(out=G4(t1t[:]), in0=G4(t1t[:]),
                                                   in1=bsT[0][0][:].unsqueeze(2).to_broadcast([128, 4, 128]), op=ALU.add),
                   reads=[t1g, bsT[0][1]], writes=[t1g])
                op("dve", lambda e: e.tensor_tensor(out=t1t[:], in0=t1t[:], in1=gut[:], op=ALU.mult), reads=[t1g, gug], writes=[t1g])
                op("pool", lambda e: e.tensor_tensor(out=sqt[:], in0=t1t[:], in1=t1t[:], op=ALU.mult), reads=[t1g], writes=[sqg])
                op("dve", lambda e: e.tensor_reduce(out=smt[:, 8:12], in_=G4(sqt[:]), axis=AX.X, op=ALU.add), reads=[sqg], writes=[smg])
                rstd_from(smt[:, 8:12], smt[:, 12:16], 128, [smg], smg)
                op("dve", lambda e: e.tensor_tensor(out=G4(t1t[:]), in0=G4(t1t[:]),
                                                   in1=smt[:, 12:16].unsqueeze(2).to_broadcast([128, 4, 128]), op=ALU.mult),
                   reads=[t1g, smg], writes=[t1g])
                ynt, yng = ynb[i]
                op("dve", lambda e: e.tensor_tensor(out=ynt[:], in0=t1t[:], in1=oga[0][0][:], op=ALU.mult),
                   reads=[t1g, oga[0][1]], writes=[yng])
                tpt, tpg = tp[0]
                for g in range(4):
                    op("pe", lambda e, g=g: e.transpose(tpt[:, g * 128:(g + 1) * 128], ynt[:, g * 128:(g + 1) * 128], ident_b),
                       reads=[yng, cbg], writes=[tpg], signal=(g == 3))
                ystt, ystg = yst[i]
                op("act", lambda e: e.activation(out=ystt[:], in_=tpt[:, 0:512], func=AF.Copy), reads=[tpg], writes=[ystg])
                dma("act", lambda e: e.dma_start(out=yT_d[i, :, 0:512], in_=ystt[:]), reads=[ystg], writes=[yTa_g])

            def late_qk(i):
                qnt, qng = qn[i]
                tpt, tpg = tp[1]
                for k8 in range(8):
                    op("pe", lambda e, k8=k8: e.transpose(tpt[:, k8 * 128:(k8 + 1) * 128], qnt[:, k8 * 128:(k8 + 1) * 128], ident_b),
                       reads=[qng, cbg], writes=[tpg], signal=(k8 == 7))
                qstt, qstg = qst[i]
                op("act", lambda e: e.activation(out=qstt[:], in_=tpt[:, 0:512], func=AF.Copy), reads=[tpg], writes=[qstg])
                dma("act", lambda e: e.dma_start(out=qT_d[i], in_=qstt[:]), reads=[qstg], writes=[kv_g])
                kstt, kstg = kst[i]
                op("act", lambda e: e.activation(out=kstt[:], in_=tpt[:, 512:1024], func=AF.Copy), reads=[tpg], writes=[kstg])
                dma("act", lambda e: e.dma_start(out=kT_d[i], in_=kstt[:]), reads=[kstg], writes=[kv_g])

            front_a(0)
            front_b(0)
            mid(0)
            if NT > 1:
                front_a(1)
            for i in range(NT):
                if i + 1 < NT:
                    front_b(i + 1)
                back1(i)
                back1v(i)
                if i + 1 < NT:
                    mid(i + 1, range(0, 2))
                sgmm(i)
                if i + 1 < NT:
                    mid(i + 1, range(2, 5))
                if i + 2 < NT:
                    front_a(i + 2)
                back2(i)
                late_qk(i)
            sch.barrier(bar_scr[:])
        if G > 1:
            groups = [[g * G + r for r in range(G)] for g in range(8 // G)]
            sch._deps("pool", [], [])
            for src, dst in ((kT_d, kTall_d), (v_d, vall_d)):
                dma("pool", lambda e, src=src, dst=dst: e.collective_compute(
                    "AllGather", ALU.bypass, replica_groups=groups,
                    ins=[src.rearrange("t p c -> (t p) c")], outs=[dst.rearrange("t p c -> (t p) c")]), writes=[Reg()])
            sch.barrier(bar_scr[:])
        with ExitStack() as st:
            NCH = 2
            KT = Buf(st, [128, 4, S], BF16)
            VV = Buf(st, [128, NBLK, 512], BF16)
            KTt, _ = KT[0]
            VVt, _ = VV[0]
            kreg = [Reg() for _ in range(NBLK)]
            vreg = [Reg() for _ in range(NBLK)]
            for n in range(NBLK):
                r, t = blk_src(G, n)
                row = r * NT + t
                dma("sp", lambda e, n=n, row=row: e.dma_start(out=KTt[:, :, n * 128:(n + 1) * 128],
                                                             in_=kTall_d[row].rearrange("p (h t) -> p h t", h=4)), writes=[kreg[n]])
                dma("sp", lambda e, n=n, row=row: e.dma_start(out=VVt[:, n, :], in_=vall_d[row]), writes=[vreg[n]])
            ogb = Buf(st, [128, 512], F32)
            bload("sp", ogb[0][0][:], ogb[0][1], out_norm_g[l:l + 1, 512:1024])
            eb = Buf(st, [128, 512], F32, n=2 * NCH)
            spb = Buf(st, [128, 512], BF16, n=3 * NCH)
            wb = Buf(st, [128, 512], F32, n=NCH + 1)
            ab = Buf(st, [128, 512], BF16, n=2 * NCH)
            qtb = Buf(st, [128, 4, 128], BF16, n=2 * NCH)
            zps = Buf(st, [128, 512], F32, n=NCH, ps=True)
            tpo = Buf(st, [128, 512], F32, n=1, ps=True)
            cps = Buf(st, [128, 512], F32, n=NCH, ps=True)
            ops_ = Buf(st, [128, 512], F32, n=NCH, ps=True)
            osq = Buf(st, [128, 512], F32, n=2)
            osm = Buf(st, [128, 8], F32, n=2)
            of = Buf(st, [128, 512], F32, n=2)
            ost = Buf(st, [128, 512], BF16, n=2)
            yTb_g = Reg()
            tiles = [(i, i, i + 1) for i in range(NT)]
            zctr = [0]
            fin_ctr = [0]

            def finish(t, c):
                ot, og_ = ops_[c]
                k = fin_ctr[0]
                fin_ctr[0] += 1
                sqt, sqg = osq[k]
                smt, smg = osm[k]
                op("act", lambda e: e.activation(out=sqt[:], in_=ot[:], func=AF.Square), reads=[og_], writes=[sqg])
                op("dve", lambda e: e.tensor_reduce(out=smt[:, 0:4], in_=sqt[:].rearrange("p (g k) -> p g k", g=4), axis=AX.X, op=ALU.add),
                   reads=[sqg], writes=[smg])
                rstd_from(smt[:, 0:4], smt[:, 4:8], 128, [smg], smg)
                oft, ofg = of[k]
                op("dve", lambda e: e.tensor_tensor(out=oft[:].rearrange("p (g k) -> p g k", g=4),
                                                   in0=ot[:].rearrange("p (g k) -> p g k", g=4),
                                                   in1=smt[:, 4:8].unsqueeze(2).to_broadcast([128, 4, 128]), op=ALU.mult),
                   reads=[og_, smg], writes=[ofg])
                op("dve", lambda e: e.tensor_tensor(out=oft[:], in0=oft[:], in1=ogb[0][0][:], op=ALU.mult),
                   reads=[ofg, ogb[0][1]], writes=[ofg])
                tpt, tpg = tpo[0]
                for g in range(4):
                    op("pe", lambda e, g=g: e.transpose(tpt[:, g * 128:(g + 1) * 128], oft[:, g * 128:(g + 1) * 128], ident_f),
                       reads=[ofg, cfg_], writes=[tpg], signal=(g == 3))
                ostt, ostg = ost[k]
                op("act", lambda e: e.activation(out=ostt[:], in_=tpt[:], func=AF.Copy), reads=[tpg], writes=[ostg])
                dma("act", lambda e: e.dma_start(out=yT_d[t, :, 512:1024], in_=ostt[:]), reads=[ostg], writes=[yTb_g])

            gctr = [0]
            for g0 in range(0, NT, NCH):
                grp = tiles[g0:g0 + NCH]
                Lmax = max(x[2] for x in grp)
                nch = len(grp)
                gi = gctr[0]
                gctr[0] += 1
                qts = []
                for c, (t, nmax, L) in enumerate(grp):
                    QTt, qg_ = qtb[gi * NCH + c]
                    dma("sp", lambda e, t=t, QTt=QTt: e.dma_start(out=QTt[:], in_=qT_d[t].rearrange("p (h t) -> p h t", h=4)), writes=[qg_])
                    qts.append((QTt, qg_))
                zb = {}
                spprev = {}

                def E1(r):
                    for c, (t, nmax, L) in enumerate(grp):
                        if r >= L:
                            continue
                        m = nmax - r
                        zt, zg = zps[c]
                        QTt, qg_ = qts[c]
                        for h in range(4):
                            op("pe", lambda e, h=h: e.matmul(zt[:, h * 128:(h + 1) * 128], lhsT=KTt[:, h, m * 128:(m + 1) * 128],
                                                            rhs=QTt[:, h, :], start=True, stop=True),
                               reads=[kreg[m], qg_], writes=[zg], signal=(h == 3))
                        zb[(r, c)] = (zt, zg)

                cur = {}
                E1(0)
                for r in range(Lmax):
                    act_c = [c for c, (t, nmax, L) in enumerate(grp) if r < L]
                    for c in act_c:
                        t, nmax, L = grp[c]
                        zt, zg = zb.pop((r, c))
                        idx = (gi * 64 + r) * NCH + c
                        et, eg = eb[idx]
                        spt, spg = spb[idx]
                        op("act", lambda e: e.activation(out=et[:], in_=zt[:], func=AF.Exp), reads=[zg], writes=[eg])
                        op("act", lambda e: e.activation(out=spt[:], in_=et[:], func=AF.Ln, bias=1.0), reads=[eg], writes=[spg])
                        if r == 0:
                            mk = am[:, 0, :]
                            op("pool", lambda e: e.tensor_tensor(out=spt[:].rearrange("p (h t) -> p h t", h=4),
                                                                in0=spt[:].rearrange("p (h t) -> p h t", h=4),
                                                                in1=mk.unsqueeze(1).to_broadcast([128, 4, 128]), op=ALU.mult),
                               reads=[spg, amg], writes=[spg])
                            op("pool", lambda e: e.tensor_tensor(out=et[:].rearrange("p (h t) -> p h t", h=4),
                                                                in0=et[:].rearrange("p (h t) -> p h t", h=4),
                                                                in1=mk.unsqueeze(1).to_broadcast([128, 4, 128]), op=ALU.mult),
                               reads=[eg, amg], writes=[eg])
                        cur[c] = (et, eg, spt, spg)
                    for c in act_c:
                        et, eg, spt, spg = cur[c]
                        ct, cg = cps[c]
                        prev = spprev.get(c)
                        for h in range(4):
                            hs = slice(h * 128, (h + 1) * 128)
                            if prev is not None:
                                pt_, pg_ = prev
                                op("pe", lambda e, hs=hs: e.matmul(ct[:, hs], lhsT=triLow_b, rhs=pt_[:, hs], start=False, stop=False,
                                                                  skip_group_check=True),
                                   reads=[pg_, cbg], writes=[cg], signal=False)
                            op("pe", lambda e, hs=hs, h=h: e.matmul(ct[:, hs], lhsT=triIncl_b, rhs=spt[:, hs],
                                                                   start=(prev is None and h == 0), stop=True, skip_group_check=True),
                               reads=[spg, cbg], writes=[cg], signal=(h == 3))
                        spprev[c] = (spt, spg)
                    if r + 1 < Lmax:
                        E1(r + 1)
                    ws = {}
                    for c in act_c:
                        ct, cg = cps[c]
                        wt, wg = wb[(gi * 64 + r) * NCH + c]
                        op("act", lambda e: e.activation(out=wt[:], in_=ct[:], func=AF.Exp, scale=-1.0), reads=[cg], writes=[wg])
                        ws[c] = (wt, wg)
                    as_ = {}
                    for c in act_c:
                        et, eg, spt, spg = cur[c]
                        wt, wg = ws[c]
                        at, ag = ab[(gi * 64 + r) * NCH + c]
                        op("dve", lambda e: e.tensor_tensor(out=at[:], in0=et[:], in1=wt[:], op=ALU.mult), reads=[eg, wg], writes=[ag])
                        as_[c] = (at, ag)
                    for c in act_c:
                        t, nmax, L = grp[c]
                        m = nmax - r
                        at, ag = as_[c]
                        ot, og_ = ops_[c]
                        for h in range(4):
                            hs = slice(h * 128, (h + 1) * 128)
                            op("pe", lambda e, hs=hs, h=h: e.matmul(ot[:, hs], lhsT=at[:, hs], rhs=VVt[:, m, hs],
                                                                   start=(r == 0 and h == 0), stop=(r == L - 1), skip_group_check=True),
                               reads=[ag, vreg[m]], writes=[og_], signal=(h == 3))
                        if r == L - 1:
                            finish(t, c)
            sch.barrier(bar_scr[:])
        lay.close()
        moe = ExitStack()
        OH0 = Buf(moe, [128, NT, 32], F32)
        OH1 = Buf(moe, [128, NT, 32], F32)
        AA = Buf(moe, [128, NT, 32], BF16)
        WW = Buf(moe, [128, NT, 2], F32)
        SL = Buf(moe, [128, NT, 2], I32)
        widx = Buf(moe, [128, CAPB], I32)
        oh0t, oh0g = OH0[0]
        oh1t, oh1g = OH1[0]
        aat, aag = AA[0]
        wwt, wwg = WW[0]
        slt, slg = SL[0]
        with ExitStack() as st:
            wob = Buf(st, [128, 8, D], BF16)
            wo_t, wo_g = wob[0]
            for c in range(8):
                dma("pool", lambda e, c=c: e.dma_start(out=wo_t[:, c, :], in_=w_out[l, c * 128:(c + 1) * 128, :]), writes=[wo_g])
            gate1 = Buf(st, [128, D], F32)
            gs2 = Buf(st, [128, D], F32)
            sh2 = Buf(st, [128, D], F32)
            dma("sp", lambda e: e.dma_start(out=gate1[0][0][:], in_=modl_d[l, 2]), writes=[gate1[0][1]])
            dma("sp", lambda e: e.dma_start(out=gs2[0][0][:], in_=modl_d[l, 4]), writes=[gs2[0][1]])
            dma("sp", lambda e: e.dma_start(out=sh2[0][0][:], in_=modl_d[l, 3]), writes=[sh2[0][1]])
            wrt = Buf(st, [128, KC, 36], F32)
            dma("sp", lambda e: e.dma_start(out=wrt[0][0][:], in_=w_r[l].rearrange("(kc p) n -> p kc n", p=128)), writes=[wrt[0][1]])
            brt = Buf(st, [128, 36], F32)
            bload("sp", brt[0][0][:], brt[0][1], b_r[l:l + 1, :])
            yTb = Buf(st, [128, 1024], BF16, n=2)
            xb = Buf(st, [128, D], F32, n=2)
            xm = Buf(st, [128, D], F32, n=2)
            junk = Buf(st, [128, D], BF16, n=1)
            ssb = Buf(st, [128, 2], F32, n=2)
            h2f = Buf(st, [128, D], F32, n=2)
            h2b = Buf(st, [128, D], BF16, n=2)
            h2T = Buf(st, [128, KC, 128], F32, n=1)
            lg = Buf(st, [128, 36], F32, n=2)
            rs = Buf(st, [128, 64], F32, n=2)
            po = [Buf(st, [128, 512], F32, n=1, ps=True) for _ in range(4)]
            tpf = Buf(st, [128, 512], F32, n=2, ps=True)
            lgp = Buf(st, [128, 512], F32, n=1, ps=True)
            xm_g = Reg()
            h2_g = Reg()

            def s1(i):
                yt, yg = yTb[i]
                dma("sp", lambda e, i=i: e.dma_start(out=yt[:], in_=yT_d[i]), writes=[yg])
                xt, xg = xb[i]
                dma("sp", lambda e, i=i: e.dma_start(out=xt[:], in_=x_src[i * 128:(i + 1) * 128, :]), writes=[xg])
                for nb in range(4):
                    pt, pg = po[nb][0]
                    for c in range(8):
                        op("pe", lambda e, c=c, nb=nb, pt=pt: e.matmul(pt[:], lhsT=yt[:, c * 128:(c + 1) * 128],
                                                                     rhs=wo_t[:, c, nb * 512:(nb + 1) * 512], start=(c == 0), stop=(c == 7)),
                           reads=[yg, wo_g], writes=[pg], signal=(c == 7))
                xmt, xmg = xm[i]
                for nb in range(4):
                    pt, pg = po[nb][0]
                    cs = slice(nb * 512, (nb + 1) * 512)
                    op("dve", lambda e, cs=cs, pt=pt: e.tensor_tensor(out=xmt[:, cs], in0=pt[:], in1=gate1[0][0][:, cs], op=ALU.mult),
                       reads=[pg, gate1[0][1]], writes=[xmg])
                op("dve", lambda e: e.tensor_tensor(out=xmt[:], in0=xmt[:], in1=xt[:], op=ALU.add), reads=[xmg, xg], writes=[xmg])
                dma("pool", lambda e, i=i: e.dma_start(out=xm_d[i * 128:(i + 1) * 128, :], in_=xmt[:]), reads=[xmg], writes=[xm_g])
                jt, jg = junk[i]
                s_t, s_g = ssb[i]
                op("act", lambda e: e.activation(out=jt[:], in_=xmt[:], func=AF.Square, accum_out=s_t[:, 0:1]),
                   reads=[xmg], writes=[jg, s_g])
                rstd_from(s_t[:, 0:1], s_t[:, 1:2], D, [s_g], s_g)
                hft, hfg = h2f[i]
                op("dve", lambda e: e.scalar_tensor_tensor(out=hft[:], in0=xmt[:], scalar=s_t[:, 1:2], in1=gs2[0][0][:],
                                                          op0=ALU.mult, op1=ALU.mult), reads=[xmg, s_g, gs2[0][1]], writes=[hfg])
                op("dve", lambda e: e.tensor_tensor(out=hft[:], in0=hft[:], in1=sh2[0][0][:], op=ALU.add),
                   reads=[hfg, sh2[0][1]], writes=[hfg])
                hbt, hbg = h2b[i]
                op("act", lambda e: e.activation(out=hbt[:], in_=hft[:], func=AF.Copy), reads=[hfg], writes=[hbg])
                dma("act", lambda e, i=i: e.dma_start(out=h2_d[i * 128:(i + 1) * 128, :], in_=hbt[:]), reads=[hbg], writes=[h2_g])

            def s2(i):
                hft, hfg = h2f[i]
                hTt, hTg = h2T[i]
                for q4 in range(4):
                    tpt, tpg = tpf[q4]
                    for k4 in range(4):
                        kc = q4 * 4 + k4
                        op("pe", lambda e, kc=kc, k4=k4, tpt=tpt: e.transpose(tpt[:, k4 * 128:(k4 + 1) * 128],
                                                                             hft[:, kc * 128:(kc + 1) * 128], ident_f),
                           reads=[hfg, cfg_], writes=[tpg], signal=(k4 == 3))
                    op("act" if q4 % 2 == 0 else "dve",
                       lambda e, q4=q4, tpt=tpt: (e.activation(out=hTt[:, q4 * 4:(q4 + 1) * 4, :].rearrange("p a b -> p (a b)"),
                                                               in_=tpt[:], func=AF.Copy) if q4 % 2 == 0 else
                                                  e.tensor_copy(out=hTt[:, q4 * 4:(q4 + 1) * 4, :].rearrange("p a b -> p (a b)"), in_=tpt[:])),
                       reads=[tpg], writes=[hTg])
                lpt, lpg = lgp[0]
                for kc in range(KC):
                    op("pe", lambda e, kc=kc: e.matmul(lpt[:, 0:36], lhsT=hTt[:, kc, :], rhs=wrt[0][0][:, kc, :],
                                                      start=(kc == 0), stop=(kc == KC - 1)),
                       reads=[hTg, wrt[0][1]], writes=[lpg], signal=(kc == KC - 1))
                lt, lgg = lg[i]
                r, rg = rs[i]
                V = lambda f, **kw: op("dve", f, **kw)
                V(lambda e: e.tensor_tensor(out=lt[:], in0=lpt[:, 0:36], in1=brt[0][0][:], op=ALU.add), reads=[lpg, brt[0][1]], writes=[lgg])
                V(lambda e: e.tensor_reduce(out=r[:, 0:1], in_=lt[:, 0:4], axis=AX.X, op=ALU.max), reads=[lgg], writes=[rg])
                V(lambda e: e.tensor_scalar(out=r[:, 4:8], in0=lt[:, 0:4], scalar1=r[:, 0:1], scalar2=None, op0=ALU.is_equal),
                  reads=[lgg, rg], writes=[rg])
                V(lambda e: e.tensor_scalar(out=r[:, 1:2], in0=r[:, 0:1], scalar1=-1.0, scalar2=None, op0=ALU.mult), reads=[rg], writes=[rg])
                op("act", lambda e: e.activation(out=r[:, 8:12], in_=lt[:, 0:4], func=AF.Exp, bias=r[:, 1:2], accum_out=r[:, 2:3]),
                   reads=[lgg, rg], writes=[rg])
                V(lambda e: e.reciprocal(out=r[:, 3:4], in_=r[:, 2:3]), reads=[rg], writes=[rg])
                V(lambda e: e.tensor_tensor(out=r[:, 16:48].rearrange("p (g j) -> p g j", g=4),
                                            in0=lt[:, 4:36].rearrange("p (g j) -> p g j", g=4),
                                            in1=r[:, 4:8].unsqueeze(2).to_broadcast([128, 4, 8]), op=ALU.mult),
                  reads=[lgg, rg], writes=[rg])
                V(lambda e: e.tensor_reduce(out=r[:, 48:56], in_=r[:, 16:48].rearrange("p (g j) -> p j g", g=4), axis=AX.X, op=ALU.add),
                  reads=[rg], writes=[rg])
                V(lambda e: e.max(out=r[:, 56:64], in_=r[:, 48:56]), reads=[rg], writes=[rg])
                V(lambda e: e.tensor_scalar(out=r[:, 16:24], in0=r[:, 48:56], scalar1=r[:, 56:57], scalar2=None, op0=ALU.is_equal),
                  reads=[rg], writes=[rg])
                V(lambda e: e.tensor_scalar(out=r[:, 24:32], in0=r[:, 48:56], scalar1=r[:, 57:58], scalar2=None, op0=ALU.is_equal),
                  reads=[rg], writes=[rg])
                V(lambda e: e.tensor_tensor(out=r[:, 8:9], in0=r[:, 57:58], in1=r[:, 56:57], op=ALU.subtract), reads=[rg], writes=[rg])
                op("act", lambda e: e.activation(out=r[:, 9:10], in_=r[:, 8:9], func=AF.Exp), reads=[rg], writes=[rg])
                V(lambda e: e.tensor_scalar(out=r[:, 9:10], in0=r[:, 9:10], scalar1=1.0, scalar2=None, op0=ALU.add), reads=[rg], writes=[rg])
                V(lambda e: e.reciprocal(out=r[:, 10:11], in_=r[:, 9:10]), reads=[rg], writes=[rg])
                V(lambda e, i=i: e.tensor_tensor(out=wwt[:, i, 0:1], in0=r[:, 10:11], in1=r[:, 3:4], op=ALU.mult), reads=[rg], writes=[wwg])
                V(lambda e, i=i: e.tensor_tensor(out=wwt[:, i, 1:2], in0=r[:, 3:4], in1=wwt[:, i, 0:1], op=ALU.subtract),
                  reads=[rg, wwg], writes=[wwg])
                for kk, (oht, ohg) in enumerate(((oh0t, oh0g), (oh1t, oh1g))):
                    V(lambda e, i=i, kk=kk, oht=oht: e.tensor_tensor(
                        out=oht[:, i, :].rearrange("p (g j) -> p g j", g=4),
                        in0=r[:, 4:8].unsqueeze(2).to_broadcast([128, 4, 8]),
                        in1=r[:, 16 + 8 * kk:24 + 8 * kk].unsqueeze(1).to_broadcast([128, 4, 8]), op=ALU.mult),
                      reads=[rg], writes=[ohg])
                V(lambda e, i=i: e.tensor_tensor(out=aat[:, i, :], in0=oh0t[:, i, :], in1=oh1t[:, i, :], op=ALU.add),
                  reads=[oh0g, oh1g], writes=[aag])

            s1(0)
            for i in range(NT):
                if i + 1 < NT:
                    s1(i + 1)
                s2(i)
            sch.barrier(bar_scr[:])
        with ExitStack() as st:
            NE = NT * 32
            Tt = Buf(st, [128, NT, 32], F32)
            Rt = Buf(st, [128, NT, 32], F32)
            Pt = Buf(st, [128, NT, 32], F32)
            cn = Buf(st, [128, 4, 32], F32)
            tmp = Buf(st, [128, NT, 32], F32)
            slf = Buf(st, [128, NT, 2], F32)
            posf = Buf(st, [128, NT, 4], F32)
            posi = Buf(st, [128, NT, 2], I32)
            qint = Buf(st, [128, 32], I32)
            cmp_ = Buf(st, [128, CAPB, 32], F32)
            bef = Buf(st, [128, CAPB], F32)
            skipf = Buf(st, [128, CAPB], F32)
            tok = Buf(st, [128, NT], F32)
            toki = Buf(st, [128, NT], I32)
            fill = Buf(st, [128, CAPB], I32)
            pq = Buf(st, [128, 512], F32, n=2, ps=True)
            T_, Tg = Tt[0]
            R_, Rg = Rt[0]
            P_, Pg = Pt[0]
            c_, cg_ = cn[0]
            V = lambda f, **kw: op("dve", f, **kw)
            aflat = aat[:].rearrange("p t e -> p (t e)")
            for (dst, dg, lhs) in ((T_, Tg, ones_b), (R_, Rg, triLow_b)):
                dflat = dst[:].rearrange("p t e -> p (t e)")
                for c0 in range(0, NE, 512):
                    c1 = min(NE, c0 + 512)
                    pt, pg = pq[c0 // 512]
                    op("pe", lambda e, c0=c0, c1=c1, pt=pt, lhs=lhs: e.matmul(pt[:, 0:c1 - c0], lhsT=lhs, rhs=aflat[:, c0:c1], start=True, stop=True),
                       reads=[aag, cbg], writes=[pg])
                    V(lambda e, c0=c0, c1=c1, pt=pt, dflat=dflat: e.tensor_copy(out=dflat[:, c0:c1], in_=pt[:, 0:c1 - c0]), reads=[pg], writes=[dg])
            V(lambda e: e.memset(P_[:, 0, :], 0.0), writes=[Pg])
            for i in range(1, NT):
                V(lambda e, i=i: e.tensor_tensor(out=P_[:, i, :], in0=P_[:, i - 1, :], in1=T_[:, i - 1, :], op=ALU.add), reads=[Pg, Tg], writes=[Pg])
            V(lambda e: e.tensor_tensor(out=c_[:, 0, :], in0=P_[:, NT - 1, :], in1=T_[:, NT - 1, :], op=ALU.add), reads=[Pg, Tg], writes=[cg_])
            qi, qig = qint[0]
            V(lambda e: e.tensor_scalar(out=c_[:, 1, :], in0=c_[:, 0, :], scalar1=127.0 - 63.5, scalar2=1.0 / 128.0, op0=ALU.add, op1=ALU.mult),
              reads=[cg_], writes=[cg_])
            V(lambda e: e.tensor_copy(out=qi[:, 0:32], in_=c_[:, 1, :]), reads=[cg_], writes=[qig])
            V(lambda e: e.tensor_copy(out=c_[:, 1, :], in_=qi[:, 0:32]), reads=[qig], writes=[cg_])
            V(lambda e: e.tensor_scalar(out=c_[:, 1, :], in0=c_[:, 1, :], scalar1=128.0, scalar2=None, op0=ALU.mult), reads=[cg_], writes=[cg_])
            V(lambda e: e.tensor_copy(out=c_[:, 2, 0:1], in_=c_[:, 1, 0:1]), reads=[cg_], writes=[cg_])
            for ex in range(1, 32):
                V(lambda e, ex=ex: e.tensor_tensor(out=c_[:, 2, ex:ex + 1], in0=c_[:, 2, ex - 1:ex], in1=c_[:, 1, ex:ex + 1], op=ALU.add),
                  reads=[cg_], writes=[cg_])
            V(lambda e: e.tensor_tensor(out=c_[:, 3, :], in0=c_[:, 2, :], in1=c_[:, 1, :], op=ALU.subtract), reads=[cg_], writes=[cg_])
            V(lambda e: e.tensor_tensor(out=P_[:], in0=P_[:], in1=R_[:], op=ALU.add), reads=[Pg, Rg], writes=[Pg])
            V(lambda e: e.tensor_tensor(out=P_[:], in0=P_[:], in1=c_[:, 3, :].unsqueeze(1).to_broadcast([128, NT, 32]), op=ALU.add),
              reads=[Pg, cg_], writes=[Pg])
            tm, tmg = tmp[0]
            sf, sfg = slf[0]
            for kk, (oht, ohg) in enumerate(((oh0t, oh0g), (oh1t, oh1g))):
                V(lambda e, oht=oht: e.tensor_tensor(out=tm[:], in0=oht[:], in1=P_[:], op=ALU.mult), reads=[ohg, Pg], writes=[tmg])
                V(lambda e, kk=kk: e.tensor_reduce(out=sf[:, :, kk], in_=tm[:], axis=AX.X, op=ALU.add), reads=[tmg], writes=[sfg])
            V(lambda e: e.tensor_copy(out=slt[:], in_=sf[:]), reads=[sfg], writes=[slg])
            pf, pfg = posf[0]
            pi_, pig = posi[0]
            V(lambda e: e.tensor_scalar(out=pf[:, :, 2:4], in0=sf[:], scalar1=-63.5, scalar2=1.0 / 128.0, op0=ALU.add, op1=ALU.mult), reads=[sfg], writes=[pfg])
            V(lambda e: e.tensor_copy(out=pi_[:], in_=pf[:, :, 2:4]), reads=[pfg], writes=[pig])
            V(lambda e: e.tensor_copy(out=pf[:, :, 2:4], in_=pi_[:]), reads=[pig], writes=[pfg])
            V(lambda e: e.scalar_tensor_tensor(out=pf[:, :, 0:2], in0=pf[:, :, 2:4], scalar=-128.0, in1=sf[:], op0=ALU.mult, op1=ALU.add),
              reads=[pfg, sfg], writes=[pfg])
            V(lambda e: e.scalar_tensor_tensor(out=pf[:, :, 0:2], in0=pf[:, :, 0:2], scalar=float(CAPB), in1=pf[:, :, 2:4], op0=ALU.mult, op1=ALU.add),
              reads=[pfg], writes=[pfg])
            V(lambda e: e.tensor_copy(out=pi_[:], in_=pf[:, :, 0:2]), reads=[pfg], writes=[pig])
            cm, cmg = cmp_[0]
            be, beg = bef[0]
            V(lambda e: e.tensor_scalar(out=be[:], in0=iota_b[:, 0:CAPB], scalar1=128.0, scalar2=None, op0=ALU.mult), reads=[cfg_], writes=[beg])
            V(lambda e: e.tensor_tensor(out=cm[:], in0=c_[:, 2, :].unsqueeze(1).to_broadcast([128, CAPB, 32]),
                                        in1=be[:].unsqueeze(2).to_broadcast([128, CAPB, 32]), op=ALU.is_le), reads=[cg_, beg], writes=[cmg])
            V(lambda e: e.tensor_reduce(out=be[:], in_=cm[:], axis=AX.X, op=ALU.add), reads=[cmg], writes=[beg])
            V(lambda e: e.tensor_scalar(out=be[:], in0=be[:], scalar1=31.0, scalar2=None, op0=ALU.min), reads=[beg], writes=[beg])
            sk, skg = skipf[0]
            V(lambda e: e.memset(sk[:], 0.0), writes=[skg])
            V(lambda e: e.tensor_tensor(out=sk[:, 2:CAPB], in0=be[:, 2:CAPB], in1=be[:, 0:CAPB - 2], op=ALU.is_equal), reads=[beg], writes=[skg])
            V(lambda e: e.tensor_scalar(out=be[:], in0=be[:], scalar1=128.0, scalar2=iota_p, op0=ALU.mult, op1=ALU.add), reads=[beg, cfg_], writes=[beg])
            V(lambda e: e.scalar_tensor_tensor(out=be[:], in0=sk[:], scalar=8192.0, in1=be[:], op0=ALU.mult, op1=ALU.add), reads=[skg, beg], writes=[beg])
            wi, wig = widx[0]
            V(lambda e: e.tensor_copy(out=wi[:], in_=be[:]), reads=[beg], writes=[wig])
            tk, tkg = tok[0]
            tki, tkig = toki[0]
            V(lambda e: e.tensor_scalar(out=tk[:], in0=iota_b[:, 0:NT], scalar1=128.0, scalar2=iota_p, op0=ALU.mult, op1=ALU.add),
              reads=[cfg_], writes=[tkg])
            V(lambda e: e.tensor_copy(out=tki[:], in_=tk[:]), reads=[tkg], writes=[tkig])
            fl, flg = fill[0]
            V(lambda e: e.memset(fl[:], NTOK), writes=[flg])
            stok_g = Reg()
            dma("sp", lambda e: e.dma_start(out=stok_d.rearrange("(p b) o -> p (b o)", b=CAPB), in_=fl[:]), reads=[flg], writes=[stok_g])
            for i in range(NT):
                for kk in range(2):
                    dma("pool", lambda e, i=i, kk=kk: e.indirect_dma_start(
                        out=stok_d, out_offset=bass.IndirectOffsetOnAxis(ap=pi_[:, i, kk:kk + 1], axis=0),
                        in_=tki[:, i:i + 1], in_offset=None), reads=[pig, tkig, stok_g], writes=[Reg()])
            sch.barrier(bar_scr[:])
        with ExitStack() as st:
            wi, wig = widx[0]
            sidx = Buf(st, [128, CAPB], I32)
            si, sig = sidx[0]
            dma("sp", lambda e: e.dma_start(out=si[:], in_=stok_d.rearrange("(p b) o -> p (b o)", b=CAPB)), writes=[sig])
            xs = Buf(st, [128, D], BF16, n=2)
            wgu = Buf(st, [128, 8192], BF16, n=2)
            wdn = Buf(st, [128, 4096], BF16, n=2)
            xsT = Buf(st, [128, KC, 128], BF16, n=2)
            sg_ = Buf(st, [128, 256], F32, n=2)
            act_ = Buf(st, [128, 256], BF16, n=2)
            actT = Buf(st, [128, 2, 128], BF16, n=2)
            yo = Buf(st, [128, D], BF16, n=2)
            tpb = Buf(st, [128, 1024], BF16, n=2, ps=True)
            hid = Buf(st, [128, 512], F32, n=2, ps=True)
            yop = Buf(st, [128, 512], F32, n=4, ps=True)
            ys_g = Reg()

            def stA(b):
                xst, xsg = xs[b]
                dma("pool", lambda e, b=b: e.indirect_dma_start(
                    out=xst[:], out_offset=None, in_=h2_d, in_offset=bass.IndirectOffsetOnAxis(ap=si[:, b:b + 1], axis=0)),
                    reads=[sig], writes=[xsg])
                wgt, wgg = wgu[b]
                wdt, wdg = wdn[b]
                dma("pool", lambda e, b=b: e.indirect_dma_start(
                    out=wgt[:], out_offset=None, in_=wgu_d, in_offset=bass.IndirectOffsetOnAxis(ap=wi[:, b:b + 1], axis=0),
                    bounds_check=bc_val, oob_is_err=False),
                    reads=[wig, wcast_g], writes=[wgg])
                dma("pool", lambda e, b=b: e.indirect_dma_start(
                    out=wdt[:], out_offset=None, in_=wd_d, in_offset=bass.IndirectOffsetOnAxis(ap=wi[:, b:b + 1], axis=0),
                    bounds_check=bc_val, oob_is_err=False),
                    reads=[wig, wcast_g], writes=[wdg])
                xTt, xTg = xsT[b]
                xv = xst[:].rearrange("p (q kc) -> p kc q", kc=KC)
                for half in range(2):
                    tpt, tpg = tpb[half]
                    for k8 in range(8):
                        kc = half * 8 + k8
                        op("pe", lambda e, kc=kc, k8=k8, tpt=tpt: e.transpose(tpt[:, k8 * 128:(k8 + 1) * 128], xv[:, kc, :], ident_b),
                           reads=[xsg, cbg], writes=[tpg], signal=(k8 == 7))
                    op("act" if half == 0 else "dve",
                       lambda e, half=half, tpt=tpt: (e.activation(out=xTt[:, half * 8:(half + 1) * 8, :].rearrange("p a b -> p (a b)"),
                                                                   in_=tpt[:], func=AF.Copy) if half == 0 else
                                                      e.tensor_copy(out=xTt[:, half * 8:(half + 1) * 8, :].rearrange("p a b -> p (a b)"), in_=tpt[:])),
                       reads=[tpg], writes=[xTg])
                ht, hg = hid[b]
                for gu_ in range(2):
                    for kc in range(KC):
                        op("pe", lambda e, kc=kc, gu_=gu_: e.matmul(ht[:, gu_ * 256:(gu_ + 1) * 256], lhsT=xTt[:, kc, :],
                                                                   rhs=wgt[:, gu_ * 4096 + kc * 256:gu_ * 4096 + (kc + 1) * 256],
                                                                   start=(kc == 0), stop=(kc == KC - 1)),
                           reads=[xTg, wgg], writes=[hg], signal=(kc == KC - 1 and gu_ == 1))

            def stB(b):
                ht, hg = hid[b]
                wdt, wdg = wdn[b]
                sgt, sgg = sg_[b]
                op("act", lambda e: e.activation(out=sgt[:], in_=ht[:, 0:256], func=AF.Silu), reads=[hg], writes=[sgg])
                att, atg = act_[b]
                op("dve", lambda e: e.tensor_tensor(out=att[:], in0=sgt[:], in1=ht[:, 256:512], op=ALU.mult), reads=[sgg, hg], writes=[atg])
                aTt, aTg = actT[b]
                tpt, tpg = tpb[0]
                av = att[:].rearrange("p (q c) -> p c q", c=2)
                for c in range(2):
                    op("pe", lambda e, c=c: e.transpose(tpt[:, c * 128:(c + 1) * 128], av[:, c, :], ident_b),
                       reads=[atg, cbg], writes=[tpg], signal=(c == 1))
                op("act", lambda e: e.activation(out=aTt[:].rearrange("p a b -> p (a b)"), in_=tpt[:, 0:256], func=AF.Copy), reads=[tpg], writes=[aTg])
                yot, yog = yo[b]
                for nb in range(4):
                    ypt, ypg = yop[nb]
                    for c in range(2):
                        op("pe", lambda e, c=c, nb=nb, ypt=ypt: e.matmul(ypt[:], lhsT=aTt[:, c, :],
                                                                       rhs=wdt[:, c * 2048 + nb * 512:c * 2048 + (nb + 1) * 512],
                                                                       start=(c == 0), stop=(c == 1)),
                           reads=[aTg, wdg], writes=[ypg], signal=(c == 1))
                    op("act" if nb % 2 == 0 else "dve",
                       lambda e, nb=nb, ypt=ypt: (e.activation(out=yot[:, nb * 512:(nb + 1) * 512], in_=ypt[:], func=AF.Copy) if nb % 2 == 0
                                                  else e.tensor_copy(out=yot[:, nb * 512:(nb + 1) * 512], in_=ypt[:])),
                       reads=[ypg], writes=[yog])
                dma("act", lambda e, b=b: e.dma_start(out=ys_d[b * 128:(b + 1) * 128, :], in_=yot[:]), reads=[yog], writes=[ys_g])

            stA(0)
            for b in range(CAPB):
                if b + 1 < CAPB:
                    stA(b + 1)
                stB(b)
            sch.barrier(bar_scr[:])
        with ExitStack() as st:
            gate2 = Buf(st, [128, D], F32)
            dma("sp", lambda e: e.dma_start(out=gate2[0][0][:], in_=modl_d[l, 5]), writes=[gate2[0][1]])
            p0 = Buf(st, [128, D], BF16, n=2)
            p1 = Buf(st, [128, D], BF16, n=2)
            xmb = Buf(st, [128, D], F32, n=2)
            mm = Buf(st, [128, D], F32, n=2)
            out_g = Reg()
            for i in range(NT):
                a0, a0g = p0[i]
                a1, a1g = p1[i]
                dma("pool", lambda e, i=i: e.indirect_dma_start(
                    out=a0[:], out_offset=None, in_=ys_d, in_offset=bass.IndirectOffsetOnAxis(ap=slt[:, i, 0:1], axis=0)),
                    reads=[slg], writes=[a0g])
                dma("pool", lambda e, i=i: e.indirect_dma_start(
                    out=a1[:], out_offset=None, in_=ys_d, in_offset=bass.IndirectOffsetOnAxis(ap=slt[:, i, 1:2], axis=0)),
                    reads=[slg], writes=[a1g])
                xt, xg = xmb[i]
                dma("sp", lambda e, i=i: e.dma_start(out=xt[:], in_=xm_d[i * 128:(i + 1) * 128, :]), writes=[xg])
                mt, mg = mm[i]
                op("act", lambda e, i=i: e.activation(out=mt[:], in_=a0[:], func=AF.Copy, scale=wwt[:, i, 0:1]),
                   reads=[a0g, wwg], writes=[mg])
                op("dve", lambda e, i=i: e.scalar_tensor_tensor(out=mt[:], in0=a1[:], scalar=wwt[:, i, 1:2], in1=mt[:], op0=ALU.mult, op1=ALU.add),
                   reads=[a1g, wwg, mg], writes=[mg])
                op("dve", lambda e: e.tensor_tensor(out=mt[:], in0=mt[:], in1=gate2[0][0][:], op=ALU.mult), reads=[mg, gate2[0][1]], writes=[mg])
                op("dve", lambda e: e.tensor_tensor(out=mt[:], in0=mt[:], in1=xt[:], op=ALU.add), reads=[mg, xg], writes=[mg])
                dma("act", lambda e, i=i: e.dma_start(out=x_dst[i * 128:(i + 1) * 128, :], in_=mt[:]), reads=[mg], writes=[out_g])
            sch.barrier(bar_scr[:])
        moe.close()
    sch.final_wait()
    return nc, sch


def make_consts():
    c = np.zeros((128, 1280), np.float32)
    i = np.arange(128)
    c[:, 0:128] = np.eye(128)
    c[:, 128:256] = (i[:, None] >= i[None, :])
    c[:, 256:384] = (i[:, None] < i[None, :])
    c[:, 384:512] = (i[:, None] < i[None, :])
    c[:, 512:640] = 1.0
    c[:, 640:768] = (i[:, None] <= i[None, :])
    c[:, 768] = i
    c[:, 1024:1280] = np.arange(256)[None, :]
    return c


_CACHE = {}


def run(inputs, trace=False):
    x = np.asarray(inputs["x"], np.float32)
    B, S, _ = x.shape
    G = 1
    NCORES = B
    NBLK = S // 128
    NT = NBLK // G
    depth = inputs["w_in"].shape[0]
    key = (NT, NBLK, G, depth)
    if key not in _CACHE:
        _CACHE[key] = build(NT, NBLK, G, depth)
    nc, sch = _CACHE[key]
    f = lambda a: np.ascontiguousarray(np.asarray(a, np.float32))
    cst = make_consts()
    tri = cst[:, 384:512]
    ones = np.ones((128, 128), np.float32)
    zeros = np.zeros((128, 128), np.float32)
    shared = {
        "w_mod": f(inputs["w_mod"]), "b_mod": f(inputs["b_mod"]).reshape(1, -1), "mod_layer": f(inputs["mod_layer"]),
        "norm1_g": f(inputs["norm1_g"]), "w_in": f(inputs["w_in"]), "gm_norm_g": f(inputs["gm_norm_g"]),
        "gm_wsT": f(np.asarray(inputs["gm_ws"]).transpose(0, 1, 3, 2)), "gm_bsT": f(np.asarray(inputs["gm_bs"]).transpose(0, 2, 1)),
        "q_norm_g": f(inputs["q_norm_g"]), "k_norm_g": f(inputs["k_norm_g"]), "out_norm_g": f(inputs["out_norm_g"]),
        "w_out": f(inputs["w_out"]), "norm2_g": f(inputs["norm2_g"]),
        "w_r": f(np.concatenate([np.asarray(inputs["w_group"]), np.asarray(inputs["w_route"])], axis=-1)),
        "b_r": f(np.concatenate([np.asarray(inputs["b_group"]), np.asarray(inputs["b_route"])], axis=-1)),
        "w_gate": f(inputs["w_gate"]), "w_up": f(inputs["w_up"]), "w_down": f(inputs["w_down"]),
        "consts": cst,
    }
    in_maps = []
    owns = []
    for c in range(NCORES):
        b, r = c // G, c % G
        ob = own_blocks(G, NT, r)
        owns.append((b, ob))
        xs = np.concatenate([x[b, n * 128:(n + 1) * 128] for n in ob], axis=0)
        if G == 1:
            am = np.stack([tri, ones, tri, ones], axis=1)
        elif r == 0:
            am = np.stack([zeros, tri, tri, ones], axis=1)
        else:
            am = np.stack([tri, ones, zeros, tri], axis=1)
        m = dict(shared)
        m["x"] = np.ascontiguousarray(xs)
        m["cT"] = f(np.asarray(inputs["c"])[b].reshape(KC, 128).T)
        m["amask"] = np.ascontiguousarray(am.astype(np.float32))
        in_maps.append(m)
    res = run_bass_kernel_spmd(nc, in_maps, core_ids=list(range(NCORES)), **({"trace": True} if trace else {}))
    out = np.empty_like(x)
    for c in range(NCORES):
        b, ob = owns[c]
        o = res.results[c]["out"]
        for t, n in enumerate(ob):
            out[b, n * 128:(n + 1) * 128] = o[t * 128:(t + 1) * 128]
    return out, res


def kernel(**inputs):
    out, _ = run(inputs)
    return out
```

```python
import numpy as np
import ml_dtypes
from contextlib import ExitStack
import concourse.bass as bass
import concourse.mybir as mybir
from concourse.bass_utils import run_bass_kernel_spmd

F32 = mybir.dt.float32
BF16 = mybir.dt.bfloat16
I32 = mybir.dt.int32
AF = mybir.ActivationFunctionType
ALU = mybir.AluOpType
AX = mybir.AxisListType

D = 2048
KC = 16
DIN = 2560
NEXP = 32
EPS = 1e-6
DEPTH = 4
NMOD = 6


class Reg:
    __slots__ = ("w", "r")

    def __init__(self):
        self.w = None
        self.r = {}


class Sched:
    NDS = 12

    def __init__(self, nc):
        self.nc = nc
        self.engs = {"pe": nc.tensor, "act": nc.scalar, "dve": nc.vector, "pool": nc.gpsimd, "sp": nc.sync}
        self.sem = {k: nc.semaphore("sem_" + k).__enter__() for k in self.engs}
        self.cnt = {k: 0 for k in self.engs}
        self.waited = {k: {} for k in self.engs}
        self.dsems = {}
        self.dcnt = {}
        self.dnext = {}
        for q in ("sp", "pool", "act"):
            self.dsems[q] = [nc.semaphore("ds_%s%d" % (q, i)).__enter__() for i in range(self.NDS)]
            self.dcnt[q] = [0] * self.NDS
            self.dnext[q] = 0
        self.n_inst = 0

    def _semobj(self, key):
        if isinstance(key, str):
            return self.sem[key]
        return self.dsems[key[0]][key[1]]

    def _wait(self, e, key, val):
        if val <= 0 or self.waited[e].get(key, 0) >= val:
            return
        self.engs[e].wait_ge(self._semobj(key), val)
        self.waited[e][key] = val
        self.n_inst += 1

    def _deps(self, e, reads, writes):
        deps = {}
        for r in reads:
            if r.w is not None:
                k, v = r.w
                deps[k] = max(deps.get(k, 0), v)
        for w in writes:
            if w.w is not None:
                k, v = w.w
                deps[k] = max(deps.get(k, 0), v)
            for k, v in w.r.items():
                deps[k] = max(deps.get(k, 0), v)
        for k, v in deps.items():
            if e == "pe" and k == "pe":
                continue
            self._wait(e, k, v)

    def _mark(self, ev, reads, writes):
        k, v = ev
        for r in reads:
            r.r[k] = max(r.r.get(k, 0), v)
        for w in writes:
            w.w = ev
            w.r = {}

    def op(self, e, fn, reads=(), writes=(), signal=True):
        self._deps(e, reads, writes)
        inst = fn(self.engs[e])
        self.n_inst += 1
        if signal:
            self.cnt[e] += 1
            inst.then_inc(self.sem[e], 1)
            ev = (e, self.cnt[e])
        else:
            ev = (e, self.cnt[e] + 1)
        self._mark(ev, reads, writes)

    def dma(self, q, fn, reads=(), writes=()):
        self._deps(q, reads, writes)
        i = self.dnext[q]
        self.dnext[q] = (i + 1) % self.NDS
        key = (q, i)
        self._wait(q, key, self.dcnt[q][i])
        inst = fn(self.engs[q])
        self.n_inst += 1
        self.dcnt[q][i] += 16
        inst.then_inc(self.dsems[q][i], 16)
        self._mark((key, self.dcnt[q][i]), reads, writes)

    def barrier(self, scratch_ap):
        for k in self.engs:
            if k != "pool":
                self._wait("pool", k, self.cnt[k])
        for q in self.dsems:
            if q == "wc":
                continue
            for i in range(len(self.dsems[q])):
                self._wait("pool", (q, i), self.dcnt[q][i])
        inst = self.nc.gpsimd.memset(scratch_ap, 0.0)
        self.cnt["pool"] += 1
        inst.then_inc(self.sem["pool"], 1)
        self.n_inst += 1
        for k in self.engs:
            if k != "pool":
                self._wait(k, "pool", self.cnt["pool"])

    def final_wait(self):
        for q in self.dsems:
            for i in range(len(self.dsems[q])):
                self._wait("sp", (q, i), self.dcnt[q][i])


def own_blocks(G, NT, rank):
    if G == 1:
        return list(range(NT))
    out = []
    for t in range(NT):
        j = t // 2
        if t % 2 == 0:
            out.append(4 * j + (0 if rank == 0 else 1))
        else:
            out.append(4 * j + (3 if rank == 0 else 2))
    return out


def blk_src(G, n):
    if G == 1:
        return (0, n)
    j, c = n // 4, n % 4
    return {0: (0, 2 * j), 1: (1, 2 * j), 2: (1, 2 * j + 1), 3: (0, 2 * j + 1)}[c]


def build(NT, NBLK, G, depth=DEPTH):
    S = NBLK * 128
    NTOK = NT * 128
    CAPB = (2 * NTOK) // 128 + NEXP
    CAP = CAPB * 128
    nc = bass.Bass("TRN2", target_bir_lowering=False)
    dt_in = lambda n, s, d=F32: nc.dram_tensor(n, s, d, kind="ExternalInput").ap()
    dt_sc = lambda n, s, d=F32: nc.dram_tensor(n, s, d, kind="Internal").ap()
    x_in = dt_in("x", [NTOK, D])
    cT_in = dt_in("cT", [128, KC])
    w_mod = dt_in("w_mod", [D, NMOD * D])
    b_mod = dt_in("b_mod", [1, NMOD * D])
    mod_layer = dt_in("mod_layer", [depth, NMOD * D])
    norm1_g = dt_in("norm1_g", [depth, D])
    w_in = dt_in("w_in", [depth, D, DIN])
    gm_norm_g = dt_in("gm_norm_g", [depth, 512])
    gm_wsT = dt_in("gm_wsT", [depth, 4, 128, 128])
    gm_bsT = dt_in("gm_bsT", [depth, 128, 4])
    q_norm_g = dt_in("q_norm_g", [depth, 128])
    k_norm_g = dt_in("k_norm_g", [depth, 128])
    out_norm_g = dt_in("out_norm_g", [depth, 1024])
    w_out = dt_in("w_out", [depth, 1024, D])
    norm2_g = dt_in("norm2_g", [depth, D])
    w_r = dt_in("w_r", [depth, D, 36])
    b_r = dt_in("b_r", [depth, 36])
    w_gate = dt_in("w_gate", [depth, NEXP, D, 256])
    w_up = dt_in("w_up", [depth, NEXP, D, 256])
    w_down = dt_in("w_down", [depth, NEXP, 256, D])
    consts = dt_in("consts", [128, 1280])
    amask_in = dt_in("amask", [128, 4, 128])
    out_d = nc.dram_tensor("out", [NTOK, D], F32, kind="ExternalOutput").ap()

    xa_d = dt_sc("xa_d", [NTOK, D])
    xm_d = dt_sc("xm_d", [NTOK, D])
    modl_d = dt_sc("modl_d", [depth, NMOD, 128, D])
    kT_d = dt_sc("kT_d", [NT, 128, 512], BF16)
    v_d = dt_sc("v_d", [NT, 128, 512], BF16)
    if G > 1:
        kTall_d = dt_sc("kTall_d", [G * NT, 128, 512], BF16)
        vall_d = dt_sc("vall_d", [G * NT, 128, 512], BF16)
    else:
        kTall_d, vall_d = kT_d, v_d
    qT_d = dt_sc("qT_d", [NT, 128, 512], BF16)
    yT_d = dt_sc("yT_d", [NT, 128, 1024], BF16)
    h2_d = dt_sc("h2_d", [NTOK + 1, D], BF16)
    wgu_d = dt_sc("wgu_d", [NEXP * 128, 8192], BF16)
    wd_d = dt_sc("wd_d", [NEXP * 128, 4096], BF16)
    stok_d = dt_sc("stok_d", [CAP, 1], I32)
    ys_d = dt_sc("ys_d", [CAP, D], BF16)

    sch = Sched(nc)
    op, dma = sch.op, sch.dma
    uid = [0]

    def alloc(st, shape, dt, ps=False):
        uid[0] += 1
        f = nc.psum_tensor if ps else nc.sbuf_tensor
        return st.enter_context(f("t%d" % uid[0], shape, dt))

    class Buf:
        def __init__(self, st, shape, dt, n=1, ps=False):
            self.t = [alloc(st, shape, dt, ps) for _ in range(n)]
            self.g = [Reg() for _ in range(n)]
            self.n = n

        def __getitem__(self, i):
            return self.t[i % self.n], self.g[i % self.n]

    top = ExitStack()
    cst_f = Buf(top, [128, 1280], F32)
    cst_b = Buf(top, [128, 1280], BF16)
    bar_scr = alloc(top, [128, 1], F32)
    cf, cfg_ = cst_f[0]
    cb, cbg = cst_b[0]
    dma("sp", lambda e: e.dma_start(out=cf[:], in_=consts), writes=[cfg_])
    op("dve", lambda e: e.tensor_copy(out=cb[:], in_=cf[:]), reads=[cfg_], writes=[cbg])
    ident_f = cf[:, 0:128]
    ident_b = cb[:, 0:128]
    triIncl_b = cb[:, 128:256]
    triLow_b = cb[:, 256:384]
    ones_b = cb[:, 512:640]
    gmMaskT_f = cf[:, 640:768]
    iota_p = cf[:, 768:769]
    iota_b = cf[:, 1024:1280]
    amask = Buf(top, [128, 4, 128], F32)
    am, amg = amask[0]
    dma("sp", lambda e: e.dma_start(out=am[:], in_=amask_in), writes=[amg])
    zrow = Buf(top, [1, D], BF16)
    zr, zrg = zrow[0]
    op("pool", lambda e: e.memset(zr[:], 0.0), writes=[zrg])
    h2z_g = Reg()
    dma("sp", lambda e: e.dma_start(out=h2_d[NTOK:NTOK + 1, :], in_=zr[:]), reads=[zrg], writes=[h2z_g])

    def rstd_from(ssq_ap, out_ap, n, greads, gwrite):
        op("dve", lambda e: e.tensor_scalar(out=out_ap, in0=ssq_ap, scalar1=1.0 / n, scalar2=EPS, op0=ALU.mult, op1=ALU.add),
           reads=greads, writes=[gwrite])
        op("act", lambda e: e.activation(out=out_ap, in_=out_ap, func=AF.Ln), reads=[gwrite], writes=[gwrite])
        op("act", lambda e: e.activation(out=out_ap, in_=out_ap, func=AF.Exp, scale=-0.5), reads=[gwrite], writes=[gwrite])

    with ExitStack() as st:
        cTt = Buf(st, [128, KC], F32)
        sc_b = Buf(st, [128, KC, 128], F32)
        wm = Buf(st, [128, KC, 512], F32, n=2)
        msh = Buf(st, [128, NMOD * D], F32)
        pm = Buf(st, [128, 512], F32, n=2, ps=True)
        bm = Buf(st, [128, 2048], F32, n=2)
        g_t = Buf(st, [128, 2048], F32, n=2)
        o_t = Buf(st, [128, 2048], F32, n=2)
        c_t, c_g = cTt[0]
        s_t, s_g = sc_b[0]
        m_t, m_g = msh[0]
        dma("sp", lambda e: e.dma_start(out=c_t[:], in_=cT_in), writes=[c_g])
        op("act", lambda e: e.activation(out=c_t[:], in_=c_t[:], func=AF.Silu), reads=[c_g], writes=[c_g])
        for kc in range(KC):
            op("dve", lambda e, kc=kc: e.tensor_copy(out=s_t[:, kc, :], in_=c_t[:, kc:kc + 1].to_broadcast([128, 128])),
               reads=[c_g], writes=[s_g])
        NG = NMOD * D // 512
        for n in range(NG):
            w_t, w_g = wm[n]
            dma("sp", lambda e, n=n, w_t=w_t: e.dma_start(
                out=w_t[:], in_=w_mod[:, n * 512:(n + 1) * 512].rearrange("(kc p) n -> p kc n", p=128)), writes=[w_g])
            p_t, p_g = pm[n]
            for kc in range(KC):
                op("pe", lambda e, kc=kc, p_t=p_t, w_t=w_t: e.matmul(p_t[:], lhsT=s_t[:, kc, :], rhs=w_t[:, kc, :],
                                                                   start=(kc == 0), stop=(kc == KC - 1)),
                   reads=[s_g, w_g], writes=[p_g], signal=(kc == KC - 1))
            op("act", lambda e, n=n, p_t=p_t: e.activation(out=m_t[:, n * 512:(n + 1) * 512], in_=p_t[:], func=AF.Copy),
               reads=[p_g], writes=[m_g])
        for j in range(NMOD):
            b_t, b_g = bm[j]
            dma("sp", lambda e, j=j, b_t=b_t: e.dma_start(out=b_t[:], in_=b_mod[0:1, j * D:(j + 1) * D].partition_broadcast(128)),
                writes=[b_g])
            op("dve", lambda e, j=j, b_t=b_t: e.tensor_tensor(out=m_t[:, j * D:(j + 1) * D], in0=m_t[:, j * D:(j + 1) * D],
                                                             in1=b_t[:], op=ALU.add), reads=[b_g, m_g], writes=[m_g])
        modl_g = Reg()
        k = 0
        for l in range(depth):
            for j in range(NMOD):
                b_t, b_g = bm[k]
                o_tt, o_g = o_t[k]
                dma("sp", lambda e, l=l, j=j, b_t=b_t: e.dma_start(
                    out=b_t[:], in_=mod_layer[l:l + 1, j * D:(j + 1) * D].partition_broadcast(128)), writes=[b_g])
                if j in (1, 4):
                    gg_t, gg_g = g_t[k]
                    gsrc = norm1_g if j == 1 else norm2_g
                    dma("sp", lambda e, l=l, gg_t=gg_t, gsrc=gsrc: e.dma_start(
                        out=gg_t[:], in_=gsrc[l:l + 1, :].partition_broadcast(128)), writes=[gg_g])
                    op("dve", lambda e, j=j, b_t=b_t, o_tt=o_tt: e.scalar_tensor_tensor(
                        out=o_tt[:], in0=m_t[:, j * D:(j + 1) * D], scalar=1.0, in1=b_t[:], op0=ALU.add, op1=ALU.add),
                       reads=[m_g, b_g], writes=[o_g])
                    op("dve", lambda e, o_tt=o_tt, gg_t=gg_t: e.tensor_tensor(out=o_tt[:], in0=o_tt[:], in1=gg_t[:], op=ALU.mult),
                       reads=[o_g, gg_g], writes=[o_g])
                else:
                    op("dve", lambda e, j=j, b_t=b_t, o_tt=o_tt: e.tensor_tensor(
                        out=o_tt[:], in0=m_t[:, j * D:(j + 1) * D], in1=b_t[:], op=ALU.add), reads=[m_g, b_g], writes=[o_g])
                dma("sp", lambda e, l=l, j=j, o_tt=o_tt: e.dma_start(out=modl_d[l, j], in_=o_tt[:]), reads=[o_g], writes=[modl_g])
                k += 1
        sch.barrier(bar_scr[:])

    bc_reg = nc.gpsimd.register("bc_reg").__enter__()
    nc.gpsimd.reg_mov(bc_reg, NEXP * 128 - 1)
    bc_val = nc.gpsimd.snap(bc_reg)
    wcast_g = Reg()
    wc_sem = nc.semaphore("wcast_sem").__enter__()
    sch.dsems["wc"] = [wc_sem]
    sch.dcnt["wc"] = [0]

    def cast_expert_weights(l, ex):
        for (o, i_) in ((wgu_d[ex * 128:(ex + 1) * 128, 0:4096], w_gate[l, ex].rearrange("(p kc) f -> p (kc f)", kc=KC)),
                        (wgu_d[ex * 128:(ex + 1) * 128, 4096:8192], w_up[l, ex].rearrange("(p kc) f -> p (kc f)", kc=KC)),
                        (wd_d[ex * 128:(ex + 1) * 128, :], w_down[l, ex].rearrange("(p c) n -> p (c n)", c=2))):
            inst = nc.gpsimd.dma_start(out=o, in_=i_)
            sch.dcnt["wc"][0] += 16
            inst.then_inc(wc_sem, 16)
            sch.n_inst += 1
        wcast_g.w = (("wc", 0), sch.dcnt["wc"][0])

    def bload(q, t, g, src_row):
        dma(q, lambda e: e.dma_start(out=t, in_=src_row.partition_broadcast(128)), writes=[g])

    for l in range(depth):
        x_src = x_in if l == 0 else xa_d
        x_dst = out_d if l == depth - 1 else xa_d
        lay = ExitStack()
        with ExitStack() as st:
            winb = Buf(st, [128, KC, DIN], BF16)
            wi_t, wi_g = winb[0]
            for kc in range(KC):
                dma("pool", lambda e, kc=kc: e.dma_start(out=wi_t[:, kc, :], in_=w_in[l, kc * 128:(kc + 1) * 128, :]), writes=[wi_g])
            gs1 = Buf(st, [128, D], F32)
            sh1 = Buf(st, [128, D], F32)
            dma("sp", lambda e: e.dma_start(out=gs1[0][0][:], in_=modl_d[l, 1]), writes=[gs1[0][1]])
            dma("sp", lambda e: e.dma_start(out=sh1[0][0][:], in_=modl_d[l, 0]), writes=[sh1[0][1]])
            wmT_f = Buf(st, [128, 4, 128], F32)
            wmT = Buf(st, [128, 4, 128], BF16)
            dma("sp", lambda e: e.dma_start(out=wmT_f[0][0][:], in_=gm_wsT[l].rearrange("g p t -> p g t")), writes=[wmT_f[0][1]])
            op("dve", lambda e: e.tensor_tensor(out=wmT[0][0][:], in0=wmT_f[0][0][:],
                                               in1=gmMaskT_f.unsqueeze(1).to_broadcast([128, 4, 128]), op=ALU.mult),
               reads=[wmT_f[0][1], cfg_], writes=[wmT[0][1]])
            bsT = Buf(st, [128, 4], F32)
            dma("sp", lambda e: e.dma_start(out=bsT[0][0][:], in_=gm_bsT[l]), writes=[bsT[0][1]])
            gmn = Buf(st, [128, 512], F32)
            bload("sp", gmn[0][0][:], gmn[0][1], gm_norm_g[l:l + 1, :])
            oga = Buf(st, [128, 512], F32)
            bload("sp", oga[0][0][:], oga[0][1], out_norm_g[l:l + 1, 0:512])
            gq = Buf(st, [128, 128], F32)
            gk = Buf(st, [128, 128], F32)
            bload("sp", gq[0][0][:], gq[0][1], q_norm_g[l:l + 1, :])
            bload("sp", gk[0][0][:], gk[0][1], k_norm_g[l:l + 1, :])
            op("dve", lambda e: e.tensor_scalar(out=gq[0][0][:], in0=gq[0][0][:], scalar1=128.0 ** -0.5, scalar2=None, op0=ALU.mult),
               reads=[gq[0][1]], writes=[gq[0][1]])
            xb = Buf(st, [128, D], F32, n=2)
            ssb = Buf(st, [128, 2], F32, n=2)
            hf = Buf(st, [128, D], F32, n=1)
            hb = Buf(st, [128, D], BF16, n=2)
            hT = Buf(st, [128, KC, 128], BF16, n=2)
            gu = Buf(st, [128, 512], F32, n=2)
            gv = Buf(st, [128, 512], F32, n=2)
            sq = Buf(st, [128, 512], F32, n=2)
            sqq = Buf(st, [128, 1024], F32, n=1)
            sm = Buf(st, [128, 16], F32, n=2)
            sm2 = Buf(st, [128, 16], F32, n=2)
            vn = Buf(st, [128, 512], BF16, n=2)
            t1 = Buf(st, [128, 512], F32, n=1)
            ynb = Buf(st, [128, 512], BF16, n=2)
            qn = Buf(st, [128, 1024], BF16, n=2)
            qf = Buf(st, [128, 1024], F32, n=1)
            yst = Buf(st, [128, 512], BF16, n=2)
            kst = Buf(st, [128, 512], BF16, n=2)
            qst = Buf(st, [128, 512], BF16, n=2)
            vst = Buf(st, [128, 512], BF16, n=2)
            tp = Buf(st, [128, 1024], BF16, n=2, ps=True)
            pp = [Buf(st, [128, 512], F32, n=1, ps=True) for _ in range(5)]
            sgp = Buf(st, [128, 512], F32, n=1, ps=True)
            yTa_g = Reg()
            kv_g = Reg()
            G4 = lambda ap: ap.rearrange("p (g k) -> p g k", g=4)

            def front_a(i):
                for ex in range(i * NEXP // NT, (i + 1) * NEXP // NT):
                    cast_expert_weights(l, ex)
                xt, xg = xb[i]
                dma("sp", lambda e: e.dma_start(out=xt[:], in_=x_src[i * 128:(i + 1) * 128, :]), writes=[xg])
                s_t, s_g = ssb[i]
                hft, hfg = hf[i]
                hbt, hbg = hb[i]
                op("act", lambda e: e.activation(out=hbt[:], in_=xt[:], func=AF.Square, accum_out=s_t[:, 0:1]),
                   reads=[xg], writes=[hbg, s_g])
                rstd_from(s_t[:, 0:1], s_t[:, 1:2], D, [s_g], s_g)
                op("dve", lambda e: e.scalar_tensor_tensor(out=hft[:], in0=xt[:], scalar=s_t[:, 1:2], in1=gs1[0][0][:],
                                                          op0=ALU.mult, op1=ALU.mult), reads=[xg, s_g, gs1[0][1]], writes=[hfg])
                op("pool", lambda e: e.tensor_tensor(out=hbt[:], in0=hft[:], in1=sh1[0][0][:], op=ALU.add),
                   reads=[hfg, sh1[0][1]], writes=[hbg])

            def front_b(i):
                hbt, hbg = hb[i]
                hTt, hTg = hT[i]
                for half in range(2):
                    tpt, tpg = tp[half]
                    for k8 in range(8):
                        kc = half * 8 + k8
                        op("pe", lambda e, kc=kc, k8=k8: e.transpose(tpt[:, k8 * 128:(k8 + 1) * 128],
                                                                    hbt[:, kc * 128:(kc + 1) * 128], ident_b),
                           reads=[hbg, cbg], writes=[tpg], signal=(k8 == 7))
                    dst = hTt[:, half * 8:(half + 1) * 8, :].rearrange("p a b -> p (a b)")
                    if half == 0:
                        op("act", lambda e: e.activation(out=dst, in_=tpt[:], func=AF.Copy), reads=[tpg], writes=[hTg])
                    else:
                        op("dve", lambda e: e.tensor_copy(out=dst, in_=tpt[:]), reads=[tpg], writes=[hTg])

            def mid(i, nbs=range(5)):
                hTt, hTg = hT[i]
                for nb in nbs:
                    ppt, ppg = pp[nb][0]
                    for kc in range(KC):
                        op("pe", lambda e, kc=kc: e.matmul(ppt[:], lhsT=hTt[:, kc, :], rhs=wi_t[:, kc, nb * 512:(nb + 1) * 512],
                                                          start=(kc == 0), stop=(kc == KC - 1)),
                           reads=[hTg, wi_g], writes=[ppg], signal=(kc == KC - 1))

            def back1(i):
                gut, gug = gu[i]
                gvt, gvg = gv[i]
                op("act", lambda e: e.activation(out=gut[:], in_=pp[0][0][0][:], func=AF.Gelu), reads=[pp[0][0][1]], writes=[gug])
                op("act", lambda e: e.activation(out=gvt[:], in_=pp[1][0][0][:], func=AF.Gelu), reads=[pp[1][0][1]], writes=[gvg])
                sqqt, sqqg = sqq[i]
                sm2t, sm2g = sm2[i]
                for which in range(2):
                    ppt, ppg = pp[2 + which][0]
                    op("act", lambda e: e.activation(out=sqqt[:, which * 512:(which + 1) * 512], in_=ppt[:], func=AF.Square),
                       reads=[ppg], writes=[sqqg])
                vstt, vstg = vst[i]
                op("act", lambda e: e.activation(out=vstt[:], in_=pp[4][0][0][:], func=AF.Copy), reads=[pp[4][0][1]], writes=[vstg])
                dma("act", lambda e: e.dma_start(out=v_d[i], in_=vstt[:]), reads=[vstg], writes=[kv_g])
                op("dve", lambda e: e.tensor_reduce(out=sm2t[:, 0:8], in_=sqqt[:].rearrange("p (g k) -> p g k", g=8), axis=AX.X, op=ALU.add),
                   reads=[sqqg], writes=[sm2g])
                rstd_from(sm2t[:, 0:8], sm2t[:, 8:16], 128, [sm2g], sm2g)
                qft, qfg = qf[i]
                qnt, qng = qn[i]
                for which in range(2):
                    ppt, ppg = pp[2 + which][0]
                    c0 = which * 512
                    op("dve", lambda e: e.tensor_tensor(out=G4(qft[:, c0:c0 + 512]), in0=G4(ppt[:]),
                                                       in1=sm2t[:, 8 + which * 4:12 + which * 4].unsqueeze(2).to_broadcast([128, 4, 128]), op=ALU.mult),
                       reads=[ppg, sm2g], writes=[qfg])
                    gsel = gq if which == 0 else gk
                    op("dve", lambda e: e.tensor_tensor(out=G4(qnt[:, c0:c0 + 512]), in0=G4(qft[:, c0:c0 + 512]),
                                                       in1=gsel[0][0][:].unsqueeze(1).to_broadcast([128, 4, 128]), op=ALU.mult),
                       reads=[qfg, gsel[0][1]], writes=[qng])

            def back1v(i):
                gvt, gvg = gv[i]
                sqt, sqg = sq[i]
                smt, smg = sm[i]
                op("pool", lambda e: e.tensor_tensor(out=sqt[:], in0=gvt[:], in1=gvt[:], op=ALU.mult), reads=[gvg], writes=[sqg])
                op("dve", lambda e: e.tensor_reduce(out=smt[:, 0:4], in_=G4(sqt[:]), axis=AX.X, op=ALU.add), reads=[sqg], writes=[smg])
                rstd_from(smt[:, 0:4], smt[:, 4:8], 128, [smg], smg)
                vnt, vng = vn[i]
                op("dve", lambda e: e.tensor_tensor(out=G4(vnt[:]), in0=G4(gvt[:]),
                                                   in1=smt[:, 4:8].unsqueeze(2).to_broadcast([128, 4, 128]), op=ALU.mult),
                   reads=[gvg, smg], writes=[vng])

            def sgmm(i):
                vnt, vng = vn[i]
                sgt, sgg = sgp[0]
                for g in range(4):
                    op("pe", lambda e, g=g: e.matmul(sgt[:, g * 128:(g + 1) * 128], lhsT=wmT[0][0][:, g, :],
                                                    rhs=vnt[:, g * 128:(g + 1) * 128], start=True, stop=True),
                       reads=[wmT[0][1], vng], writes=[sgg], signal=(g == 3))

            def back2(i):
                gut, gug = gu[i]
                sqt, sqg = sq[i]
                smt, smg = sm[i]
                sgt, sgg = sgp[0]
                t1t, t1g = t1[i]
                op("dve", lambda e: e.tensor_tensor(out=t1t[:], in0=sgt[:], in1=gmn[0][0][:], op=ALU.mult),
                   reads=[sgg, gmn[0][1]], writes=[t1g])
                op("dve", lambda e: e.tensor_tensor(out=G4(t1t[:]), in0=G4(t1t[:]),
                                                   in1=bsT[0][0][:].unsqueeze(2).to_broadcast([128, 4, 128]), op=ALU.add),
                   reads=[t1g, bsT[0][1]], writes=[t1g])
                op("dve", lambda e: e.tensor_tensor(out=t1t[:], in0=t1t[:], in1=gut[:], op=ALU.mult), reads=[t1g, gug], writes=[t1g])
                op("pool", lambda e: e.tensor_tensor(out=sqt[:], in0=t1t[:], in1=t1t[:], op=ALU.mult), reads=[t1g], writes=[sqg])
                op("dve", lambda e: e.tensor_reduce(out=smt[:, 8:12], in_=G4(sqt[:]), axis=AX.X, op=ALU.add), reads=[sqg], writes=[smg])
                rstd_from(smt[:, 8:12], smt[:, 12:16], 128, [smg], smg)
                op("dve", lambda e: e.tensor_tensor(out=G4(t1t[:]), in0=G4(t1t[:]),
                                                   in1=smt[:, 12:16].unsqueeze(2).to_broadcast([128, 4, 128]), op=ALU.mult),
                   reads=[t1g, smg], writes=[t1g])
                ynt, yng = ynb[i]
                op("dve", lambda e: e.tensor_tensor(out=ynt[:], in0=t1t[:], in1=oga[0][0][:], op=ALU.mult),
                   reads=[t1g, oga[0][1]], writes=[yng])
                tpt, tpg = tp[0]
                for g in range(4):
                    op("pe", lambda e, g=g: e.transpose(tpt[:, g * 128:(g + 1) * 128], ynt[:, g * 128:(g + 1) * 128], ident_b),
                       reads=[yng, cbg], writes=[tpg], signal=(g == 3))
                ystt, ystg = yst[i]
                op("act", lambda e: e.activation(out=ystt[:], in_=tpt[:, 0:512], func=AF.Copy), reads=[tpg], writes=[ystg])
                dma("act", lambda e: e.dma_start(out=yT_d[i, :, 0:512], in_=ystt[:]), reads=[ystg], writes=[yTa_g])

            def late_qk(i):
                qnt, qng = qn[i]
                tpt, tpg = tp[1]
                for k8 in range(8):
                    op("pe", lambda e, k8=k8: e.transpose(tpt[:, k8 * 128:(k8 + 1) * 128], qnt[:, k8 * 128:(k8 + 1) * 128], ident_b),
                       reads=[qng, cbg], writes=[tpg], signal=(k8 == 7))
                qstt, qstg = qst[i]
                op("act", lambda e: e.activation(out=qstt[:], in_=tpt[:, 0:512], func=AF.Copy), reads=[tpg], writes=[qstg])
                dma("act", lambda e: e.dma_start(out=qT_d[i], in_=qstt[:]), reads=[qstg], writes=[kv_g])
                kstt, kstg = kst[i]
                op("act", lambda e: e.activation(out=kstt[:], in_=tpt[:, 512:1024], func=AF.Copy), reads=[tpg], writes=[kstg])
                dma("act", lambda e: e.dma_start(out=kT_d[i], in_=kstt[:]), reads=[kstg], writes=[kv_g])

            front_a(0)
            front_b(0)
            mid(0)
            if NT > 1:
                front_a(1)
            for i in range(NT):
                if i + 1 < NT:
                    front_b(i + 1)
                back1(i)
                back1v(i)
                if i + 1 < NT:
                    mid(i + 1, range(0, 2))
                sgmm(i)
                if i + 1 < NT:
                    mid(i + 1, range(2, 5))
                if i + 2 < NT:
                    front_a(i + 2)
                back2(i)
                late_qk(i)
            sch.barrier(bar_scr[:])
        if G > 1:
            groups = [[g * G + r for r in range(G)] for g in range(8 // G)]
            sch._deps("pool", [], [])
            for src, dst in ((kT_d, kTall_d), (v_d, vall_d)):
                dma("pool", lambda e, src=src, dst=dst: e.collective_compute(
                    "AllGather", ALU.bypass, replica_groups=groups,
                    ins=[src.rearrange("t p c -> (t p) c")], outs=[dst.rearrange("t p c -> (t p) c")]), writes=[Reg()])
            sch.barrier(bar_scr[:])
        with ExitStack() as st:
            NCH = 2
            KT = Buf(st, [128, 4, S], BF16)
            VV = Buf(st, [128, NBLK, 512], BF16)
            KTt, _ = KT[0]
            VVt, _ = VV[0]
            kreg = [Reg() for _ in range(NBLK)]
            vreg = [Reg() for _ in range(NBLK)]
            for n in range(NBLK):
                r, t = blk_src(G, n)
                row = r * NT + t
                dma("sp", lambda e, n=n, row=row: e.dma_start(out=KTt[:, :, n * 128:(n + 1) * 128],
                                                             in_=kTall_d[row].rearrange("p (h t) -> p h t", h=4)), writes=[kreg[n]])
                dma("sp", lambda e, n=n, row=row: e.dma_start(out=VVt[:, n, :], in_=vall_d[row]), writes=[vreg[n]])
            ogb = Buf(st, [128, 512], F32)
            bload("sp", ogb[0][0][:], ogb[0][1], out_norm_g[l:l + 1, 512:1024])
            eb = Buf(st, [128, 512], F32, n=2 * NCH)
            spb = Buf(st, [128, 512], BF16, n=3 * NCH)
            wb = Buf(st, [128, 512], F32, n=NCH + 1)
            ab = Buf(st, [128, 512], BF16, n=2 * NCH)
            qtb = Buf(st, [128, 4, 128], BF16, n=2 * NCH)
            zps = Buf(st, [128, 512], F32, n=NCH, ps=True)
            tpo = Buf(st, [128, 512], F32, n=1, ps=True)
            cps = Buf(st, [128, 512], F32, n=NCH, ps=True)
            ops_ = Buf(st, [128, 512], F32, n=NCH, ps=True)
            osq = Buf(st, [128, 512], F32, n=2)
            osm = Buf(st, [128, 8], F32, n=2)
            of = Buf(st, [128, 512], F32, n=2)
            ost = Buf(st, [128, 512], BF16, n=2)
            yTb_g = Reg()
            tiles = [(i, i, i + 1) for i in range(NT)]
            zctr = [0]
            fin_ctr = [0]

            def finish(t, c):
                ot, og_ = ops_[c]
                k = fin_ctr[0]
                fin_ctr[0] += 1
                sqt, sqg = osq[k]
                smt, smg = osm[k]
                op("act", lambda e: e.activation(out=sqt[:], in_=ot[:], func=AF.Square), reads=[og_], writes=[sqg])
                op("dve", lambda e: e.tensor_reduce(out=smt[:, 0:4], in_=sqt[:].rearrange("p (g k) -> p g k", g=4), axis=AX.X, op=ALU.add),
                   reads=[sqg], writes=[smg])
                rstd_from(smt[:, 0:4], smt[:, 4:8], 128, [smg], smg)
                oft, ofg = of[k]
                op("dve", lambda e: e.tensor_tensor(out=oft[:].rearrange("p (g k) -> p g k", g=4),
                                                   in0=ot[:].rearrange("p (g k) -> p g k", g=4),
                                                   in1=smt[:, 4:8].unsqueeze(2).to_broadcast([128, 4, 128]), op=ALU.mult),
                   reads=[og_, smg], writes=[ofg])
                op("dve", lambda e: e.tensor_tensor(out=oft[:], in0=oft[:], in1=ogb[0][0][:], op=ALU.mult),
                   reads=[ofg, ogb[0][1]], writes=[ofg])
                tpt, tpg = tpo[0]
                for g in range(4):
                    op("pe", lambda e, g=g: e.transpose(tpt[:, g * 128:(g + 1) * 128], oft[:, g * 128:(g + 1) * 128], ident_f),
                       reads=[ofg, cfg_], writes=[tpg], signal=(g == 3))
                ostt, ostg = ost[k]
                op("act", lambda e: e.activation(out=ostt[:], in_=tpt[:], func=AF.Copy), reads=[tpg], writes=[ostg])
                dma("act", lambda e: e.dma_start(out=yT_d[t, :, 512:1024], in_=ostt[:]), reads=[ostg], writes=[yTb_g])

            gctr = [0]
            for g0 in range(0, NT, NCH):
                grp = tiles[g0:g0 + NCH]
                Lmax = max(x[2] for x in grp)
                nch = len(grp)
                gi = gctr[0]
                gctr[0] += 1
                qts = []
                for c, (t, nmax, L) in enumerate(grp):
                    QTt, qg_ = qtb[gi * NCH + c]
                    dma("sp", lambda e, t=t, QTt=QTt: e.dma_start(out=QTt[:], in_=qT_d[t].rearrange("p (h t) -> p h t", h=4)), writes=[qg_])
                    qts.append((QTt, qg_))
                zb = {}
                spprev = {}

                def E1(r):
                    for c, (t, nmax, L) in enumerate(grp):
                        if r >= L:
                            continue
                        m = nmax - r
                        zt, zg = zps[c]
                        QTt, qg_ = qts[c]
                        for h in range(4):
                            op("pe", lambda e, h=h: e.matmul(zt[:, h * 128:(h + 1) * 128], lhsT=KTt[:, h, m * 128:(m + 1) * 128],
                                                            rhs=QTt[:, h, :], start=True, stop=True),
                               reads=[kreg[m], qg_], writes=[zg], signal=(h == 3))
                        zb[(r, c)] = (zt, zg)

                cur = {}
                E1(0)
                for r in range(Lmax):
                    act_c = [c for c, (t, nmax, L) in enumerate(grp) if r < L]
                    for c in act_c:
                        t, nmax, L = grp[c]
                        zt, zg = zb.pop((r, c))
                        idx = (gi * 64 + r) * NCH + c
                        et, eg = eb[idx]
                        spt, spg = spb[idx]
                        op("act", lambda e: e.activation(out=et[:], in_=zt[:], func=AF.Exp), reads=[zg], writes=[eg])
                        op("act", lambda e: e.activation(out=spt[:], in_=et[:], func=AF.Ln, bias=1.0), reads=[eg], writes=[spg])
                        if r == 0:
                            mk = am[:, 0, :]
                            op("pool", lambda e: e.tensor_tensor(out=spt[:].rearrange("p (h t) -> p h t", h=4),
                                                                in0=spt[:].rearrange("p (h t) -> p h t", h=4),
                                                                in1=mk.unsqueeze(1).to_broadcast([128, 4, 128]), op=ALU.mult),
                               reads=[spg, amg], writes=[spg])
                            op("pool", lambda e: e.tensor_tensor(out=et[:].rearrange("p (h t) -> p h t", h=4),
                                                                in0=et[:].rearrange("p (h t) -> p h t", h=4),
                                                                in1=mk.unsqueeze(1).to_broadcast([128, 4, 128]), op=ALU.mult),
                               reads=[eg, amg], writes=[eg])
                        cur[c] = (et, eg, spt, spg)
                    for c in act_c:
                        et, eg, spt, spg = cur[c]
                        ct, cg = cps[c]
                        prev = spprev.get(c)
                        for h in range(4):
                            hs = slice(h * 128, (h + 1) * 128)
                            if prev is not None:
                                pt_, pg_ = prev
                                op("pe", lambda e, hs=hs: e.matmul(ct[:, hs], lhsT=triLow_b, rhs=pt_[:, hs], start=False, stop=False,
                                                                  skip_group_check=True),
                                   reads=[pg_, cbg], writes=[cg], signal=False)
                            op("pe", lambda e, hs=hs, h=h: e.matmul(ct[:, hs], lhsT=triIncl_b, rhs=spt[:, hs],
                                                                   start=(prev is None and h == 0), stop=True, skip_group_check=True),
                               reads=[spg, cbg], writes=[cg], signal=(h == 3))
                        spprev[c] = (spt, spg)
                    if r + 1 < Lmax:
                        E1(r + 1)
                    ws = {}
                    for c in act_c:
                        ct, cg = cps[c]
                        wt, wg = wb[(gi * 64 + r) * NCH + c]
                        op("act", lambda e: e.activation(out=wt[:], in_=ct[:], func=AF.Exp, scale=-1.0), reads=[cg], writes=[wg])
                        ws[c] = (wt, wg)
                    as_ = {}
                    for c in act_c:
                        et, eg, spt, spg = cur[c]
                        wt, wg = ws[c]
                        at, ag = ab[(gi * 64 + r) * NCH + c]
                        op("dve", lambda e: e.tensor_tensor(out=at[:], in0=et[:], in1=wt[:], op=ALU.mult), reads=[eg, wg], writes=[ag])
                        as_[c] = (at, ag)
                    for c in act_c:
                        t, nmax, L = grp[c]
                        m = nmax - r
                        at, ag = as_[c]
                        ot, og_ = ops_[c]
                        for h in range(4):
                            hs = slice(h * 128, (h + 1) * 128)
                            op("pe", lambda e, hs=hs, h=h: e.matmul(ot[:, hs], lhsT=at[:, hs], rhs=VVt[:, m, hs],
                                                                   start=(r == 0 and h == 0), stop=(r == L - 1), skip_group_check=True),
                               reads=[ag, vreg[m]], writes=[og_], signal=(h == 3))
                        if r == L - 1:
                            finish(t, c)
            sch.barrier(bar_scr[:])
        lay.close()
        moe = ExitStack()
        OH0 = Buf(moe, [128, NT, 32], F32)
        OH1 = Buf(moe, [128, NT, 32], F32)
        AA = Buf(moe, [128, NT, 32], BF16)
        WW = Buf(moe, [128, NT, 2], F32)
        SL = Buf(moe, [128, NT, 2], I32)
        widx = Buf(moe, [128, CAPB], I32)
        oh0t, oh0g = OH0[0]
        oh1t, oh1g = OH1[0]
        aat, aag = AA[0]
        wwt, wwg = WW[0]
        slt, slg = SL[0]
        with ExitStack() as st:
            wob = Buf(st, [128, 8, D], BF16)
            wo_t, wo_g = wob[0]
            for c in range(8):
                dma("pool", lambda e, c=c: e.dma_start(out=wo_t[:, c, :], in_=w_out[l, c * 128:(c + 1) * 128, :]), writes=[wo_g])
            gate1 = Buf(st, [128, D], F32)
            gs2 = Buf(st, [128, D], F32)
            sh2 = Buf(st, [128, D], F32)
            dma("sp", lambda e: e.dma_start(out=gate1[0][0][:], in_=modl_d[l, 2]), writes=[gate1[0][1]])
            dma("sp", lambda e: e.dma_start(out=gs2[0][0][:], in_=modl_d[l, 4]), writes=[gs2[0][1]])
            dma("sp", lambda e: e.dma_start(out=sh2[0][0][:], in_=modl_d[l, 3]), writes=[sh2[0][1]])
            wrt = Buf(st, [128, KC, 36], F32)
            dma("sp", lambda e: e.dma_start(out=wrt[0][0][:], in_=w_r[l].rearrange("(kc p) n -> p kc n", p=128)), writes=[wrt[0][1]])
            brt = Buf(st, [128, 36], F32)
            bload("sp", brt[0][0][:], brt[0][1], b_r[l:l + 1, :])
            yTb = Buf(st, [128, 1024], BF16, n=2)
            xb = Buf(st, [128, D], F32, n=2)
            xm = Buf(st, [128, D], F32, n=2)
            junk = Buf(st, [128, D], BF16, n=1)
            ssb = Buf(st, [128, 2], F32, n=2)
            h2f = Buf(st, [128, D], F32, n=2)
            h2b = Buf(st, [128, D], BF16, n=2)
            h2T = Buf(st, [128, KC, 128], F32, n=1)
            lg = Buf(st, [128, 36], F32, n=2)
            rs = Buf(st, [128, 64], F32, n=2)
            po = [Buf(st, [128, 512], F32, n=1, ps=True) for _ in range(4)]
            tpf = Buf(st, [128, 512], F32, n=2, ps=True)
            lgp = Buf(st, [128, 512], F32, n=1, ps=True)
            xm_g = Reg()
            h2_g = Reg()

            def s1(i):
                yt, yg = yTb[i]
                dma("sp", lambda e, i=i: e.dma_start(out=yt[:], in_=yT_d[i]), writes=[yg])
                xt, xg = xb[i]
                dma("sp", lambda e, i=i: e.dma_start(out=xt[:], in_=x_src[i * 128:(i + 1) * 128, :]), writes=[xg])
                for nb in range(4):
                    pt, pg = po[nb][0]
                    for c in range(8):
                        op("pe", lambda e, c=c, nb=nb, pt=pt: e.matmul(pt[:], lhsT=yt[:, c * 128:(c + 1) * 128],
                                                                     rhs=wo_t[:, c, nb * 512:(nb + 1) * 512], start=(c == 0), stop=(c == 7)),
                           reads=[yg, wo_g], writes=[pg], signal=(c == 7))
                xmt, xmg = xm[i]
                for nb in range(4):
                    pt, pg = po[nb][0]
                    cs = slice(nb * 512, (nb + 1) * 512)
                    op("dve", lambda e, cs=cs, pt=pt: e.tensor_tensor(out=xmt[:, cs], in0=pt[:], in1=gate1[0][0][:, cs], op=ALU.mult),
                       reads=[pg, gate1[0][1]], writes=[xmg])
                op("dve", lambda e: e.tensor_tensor(out=xmt[:], in0=xmt[:], in1=xt[:], op=ALU.add), reads=[xmg, xg], writes=[xmg])
                dma("pool", lambda e, i=i: e.dma_start(out=xm_d[i * 128:(i + 1) * 128, :], in_=xmt[:]), reads=[xmg], writes=[xm_g])
                jt, jg = junk[i]
                s_t, s_g = ssb[i]
                op("act", lambda e: e.activation(out=jt[:], in_=xmt[:], func=AF.Square, accum_out=s_t[:, 0:1]),
                   reads=[xmg], writes=[jg, s_g])
                rstd_from(s_t[:, 0:1], s_t[:, 1:2], D, [s_g], s_g)
                hft, hfg = h2f[i]
                op("dve", lambda e: e.scalar_tensor_tensor(out=hft[:], in0=xmt[:], scalar=s_t[:, 1:2], in1=gs2[0][0][:],
                                                          op0=ALU.mult, op1=ALU.mult), reads=[xmg, s_g, gs2[0][1]], writes=[hfg])
                op("dve", lambda e: e.tensor_tensor(out=hft[:], in0=hft[:], in1=sh2[0][0][:], op=ALU.add),
                   reads=[hfg, sh2[0][1]], writes=[hfg])
                hbt, hbg = h2b[i]
                op("act", lambda e: e.activation(out=hbt[:], in_=hft[:], func=AF.Copy), reads=[hfg], writes=[hbg])
                dma("act", lambda e, i=i: e.dma_start(out=h2_d[i * 128:(i + 1) * 128, :], in_=hbt[:]), reads=[hbg], writes=[h2_g])

            def s2(i):
                hft, hfg = h2f[i]
                hTt, hTg = h2T[i]
                for q4 in range(4):
                    tpt, tpg = tpf[q4]
                    for k4 in range(4):
                        kc = q4 * 4 + k4
                        op("pe", lambda e, kc=kc, k4=k4, tpt=tpt: e.transpose(tpt[:, k4 * 128:(k4 + 1) * 128],
                                                                             hft[:, kc * 128:(kc + 1) * 128], ident_f),
                           reads=[hfg, cfg_], writes=[tpg], signal=(k4 == 3))
                    op("act" if q4 % 2 == 0 else "dve",
                       lambda e, q4=q4, tpt=tpt: (e.activation(out=hTt[:, q4 * 4:(q4 + 1) * 4, :].rearrange("p a b -> p (a b)"),
                                                               in_=tpt[:], func=AF.Copy) if q4 % 2 == 0 else
                                                  e.tensor_copy(out=hTt[:, q4 * 4:(q4 + 1) * 4, :].rearrange("p a b -> p (a b)"), in_=tpt[:])),
                       reads=[tpg], writes=[hTg])
                lpt, lpg = lgp[0]
                for kc in range(KC):
                    op("pe", lambda e, kc=kc: e.matmul(lpt[:, 0:36], lhsT=hTt[:, kc, :], rhs=wrt[0][0][:, kc, :],
                                                      start=(kc == 0), stop=(kc == KC - 1)),
                       reads=[hTg, wrt[0][1]], writes=[lpg], signal=(kc == KC - 1))
                lt, lgg = lg[i]
                r, rg = rs[i]
                V = lambda f, **kw: op("dve", f, **kw)
                V(lambda e: e.tensor_tensor(out=lt[:], in0=lpt[:, 0:36], in1=brt[0][0][:], op=ALU.add), reads=[lpg, brt[0][1]], writes=[lgg])
                V(lambda e: e.tensor_reduce(out=r[:, 0:1], in_=lt[:, 0:4], axis=AX.X, op=ALU.max), reads=[lgg], writes=[rg])
                V(lambda e: e.tensor_scalar(out=r[:, 4:8], in0=lt[:, 0:4], scalar1=r[:, 0:1], scalar2=None, op0=ALU.is_equal),
                  reads=[lgg, rg], writes=[rg])
                V(lambda e: e.tensor_scalar(out=r[:, 1:2], in0=r[:, 0:1], scalar1=-1.0, scalar2=None, op0=ALU.mult), reads=[rg], writes=[rg])
                op("act", lambda e: e.activation(out=r[:, 8:12], in_=lt[:, 0:4], func=AF.Exp, bias=r[:, 1:2], accum_out=r[:, 2:3]),
                   reads=[lgg, rg], writes=[rg])
                V(lambda e: e.reciprocal(out=r[:, 3:4], in_=r[:, 2:3]), reads=[rg], writes=[rg])
                V(lambda e: e.tensor_tensor(out=r[:, 16:48].rearrange("p (g j) -> p g j", g=4),
                                            in0=lt[:, 4:36].rearrange("p (g j) -> p g j", g=4),
                                            in1=r[:, 4:8].unsqueeze(2).to_broadcast([128, 4, 8]), op=ALU.mult),
                  reads=[lgg, rg], writes=[rg])
                V(lambda e: e.tensor_reduce(out=r[:, 48:56], in_=r[:, 16:48].rearrange("p (g j) -> p j g", g=4), axis=AX.X, op=ALU.add),
                  reads=[rg], writes=[rg])
                V(lambda e: e.max(out=r[:, 56:64], in_=r[:, 48:56]), reads=[rg], writes=[rg])
                V(lambda e: e.tensor_scalar(out=r[:, 16:24], in0=r[:, 48:56], scalar1=r[:, 56:57], scalar2=None, op0=ALU.is_equal),
                  reads=[rg], writes=[rg])
                V(lambda e: e.tensor_scalar(out=r[:, 24:32], in0=r[:, 48:56], scalar1=r[:, 57:58], scalar2=None, op0=ALU.is_equal),
                  reads=[rg], writes=[rg])
                V(lambda e: e.tensor_tensor(out=r[:, 8:9], in0=r[:, 57:58], in1=r[:, 56:57], op=ALU.subtract), reads=[rg], writes=[rg])
                op("act", lambda e: e.activation(out=r[:, 9:10], in_=r[:, 8:9], func=AF.Exp), reads=[rg], writes=[rg])
                V(lambda e: e.tensor_scalar(out=r[:, 9:10], in0=r[:, 9:10], scalar1=1.0, scalar2=None, op0=ALU.add), reads=[rg], writes=[rg])
                V(lambda e: e.reciprocal(out=r[:, 10:11], in_=r[:, 9:10]), reads=[rg], writes=[rg])
                V(lambda e, i=i: e.tensor_tensor(out=wwt[:, i, 0:1], in0=r[:, 10:11], in1=r[:, 3:4], op=ALU.mult), reads=[rg], writes=[wwg])
                V(lambda e, i=i: e.tensor_tensor(out=wwt[:, i, 1:2], in0=r[:, 3:4], in1=wwt[:, i, 0:1], op=ALU.subtract),
                  reads=[rg, wwg], writes=[wwg])
                for kk, (oht, ohg) in enumerate(((oh0t, oh0g), (oh1t, oh1g))):
                    V(lambda e, i=i, kk=kk, oht=oht: e.tensor_tensor(
                        out=oht[:, i, :].rearrange("p (g j) -> p g j", g=4),
                        in0=r[:, 4:8].unsqueeze(2).to_broadcast([128, 4, 8]),
                        in1=r[:, 16 + 8 * kk:24 + 8 * kk].unsqueeze(1).to_broadcast([128, 4, 8]), op=ALU.mult),
                      reads=[rg], writes=[ohg])
                V(lambda e, i=i: e.tensor_tensor(out=aat[:, i, :], in0=oh0t[:, i, :], in1=oh1t[:, i, :], op=ALU.add),
                  reads=[oh0g, oh1g], writes=[aag])

            s1(0)
            for i in range(NT):
                if i + 1 < NT:
                    s1(i + 1)
                s2(i)
            sch.barrier(bar_scr[:])
        with ExitStack() as st:
            NE = NT * 32
            Tt = Buf(st, [128, NT, 32], F32)
            Rt = Buf(st, [128, NT, 32], F32)
            Pt = Buf(st, [128, NT, 32], F32)
            cn = Buf(st, [128, 4, 32], F32)
            tmp = Buf(st, [128, NT, 32], F32)
            slf = Buf(st, [128, NT, 2], F32)
            posf = Buf(st, [128, NT, 4], F32)
            posi = Buf(st, [128, NT, 2], I32)
            qint = Buf(st, [128, 32], I32)
            cmp_ = Buf(st, [128, CAPB, 32], F32)
            bef = Buf(st, [128, CAPB], F32)
            skipf = Buf(st, [128, CAPB], F32)
            tok = Buf(st, [128, NT], F32)
            toki = Buf(st, [128, NT], I32)
            fill = Buf(st, [128, CAPB], I32)
            pq = Buf(st, [128, 512], F32, n=2, ps=True)
            T_, Tg = Tt[0]
            R_, Rg = Rt[0]
            P_, Pg = Pt[0]
            c_, cg_ = cn[0]
            V = lambda f, **kw: op("dve", f, **kw)
            aflat = aat[:].rearrange("p t e -> p (t e)")
            for (dst, dg, lhs) in ((T_, Tg, ones_b), (R_, Rg, triLow_b)):
                dflat = dst[:].rearrange("p t e -> p (t e)")
                for c0 in range(0, NE, 512):
                    c1 = min(NE, c0 + 512)
                    pt, pg = pq[c0 // 512]
                    op("pe", lambda e, c0=c0, c1=c1, pt=pt, lhs=lhs: e.matmul(pt[:, 0:c1 - c0], lhsT=lhs, rhs=aflat[:, c0:c1], start=True, stop=True),
                       reads=[aag, cbg], writes=[pg])
                    V(lambda e, c0=c0, c1=c1, pt=pt, dflat=dflat: e.tensor_copy(out=dflat[:, c0:c1], in_=pt[:, 0:c1 - c0]), reads=[pg], writes=[dg])
            V(lambda e: e.memset(P_[:, 0, :], 0.0), writes=[Pg])
            for i in range(1, NT):
                V(lambda e, i=i: e.tensor_tensor(out=P_[:, i, :], in0=P_[:, i - 1, :], in1=T_[:, i - 1, :], op=ALU.add), reads=[Pg, Tg], writes=[Pg])
            V(lambda e: e.tensor_tensor(out=c_[:, 0, :], in0=P_[:, NT - 1, :], in1=T_[:, NT - 1, :], op=ALU.add), reads=[Pg, Tg], writes=[cg_])
            qi, qig = qint[0]
            V(lambda e: e.tensor_scalar(out=c_[:, 1, :], in0=c_[:, 0, :], scalar1=127.0 - 63.5, scalar2=1.0 / 128.0, op0=ALU.add, op1=ALU.mult),
              reads=[cg_], writes=[cg_])
            V(lambda e: e.tensor_copy(out=qi[:, 0:32], in_=c_[:, 1, :]), reads=[cg_], writes=[qig])
            V(lambda e: e.tensor_copy(out=c_[:, 1, :], in_=qi[:, 0:32]), reads=[qig], writes=[cg_])
            V(lambda e: e.tensor_scalar(out=c_[:, 1, :], in0=c_[:, 1, :], scalar1=128.0, scalar2=None, op0=ALU.mult), reads=[cg_], writes=[cg_])
            V(lambda e: e.tensor_copy(out=c_[:, 2, 0:1], in_=c_[:, 1, 0:1]), reads=[cg_], writes=[cg_])
            for ex in range(1, 32):
                V(lambda e, ex=ex: e.tensor_tensor(out=c_[:, 2, ex:ex + 1], in0=c_[:, 2, ex - 1:ex], in1=c_[:, 1, ex:ex + 1], op=ALU.add),
                  reads=[cg_], writes=[cg_])
            V(lambda e: e.tensor_tensor(out=c_[:, 3, :], in0=c_[:, 2, :], in1=c_[:, 1, :], op=ALU.subtract), reads=[cg_], writes=[cg_])
            V(lambda e: e.tensor_tensor(out=P_[:], in0=P_[:], in1=R_[:], op=ALU.add), reads=[Pg, Rg], writes=[Pg])
            V(lambda e: e.tensor_tensor(out=P_[:], in0=P_[:], in1=c_[:, 3, :].unsqueeze(1).to_broadcast([128, NT, 32]), op=ALU.add),
              reads=[Pg, cg_], writes=[Pg])
            tm, tmg = tmp[0]
            sf, sfg = slf[0]
            for kk, (oht, ohg) in enumerate(((oh0t, oh0g), (oh1t, oh1g))):
                V(lambda e, oht=oht: e.tensor_tensor(out=tm[:], in0=oht[:], in1=P_[:], op=ALU.mult), reads=[ohg, Pg], writes=[tmg])
                V(lambda e, kk=kk: e.tensor_reduce(out=sf[:, :, kk], in_=tm[:], axis=AX.X, op=ALU.add), reads=[tmg], writes=[sfg])
            V(lambda e: e.tensor_copy(out=slt[:], in_=sf[:]), reads=[sfg], writes=[slg])
            pf, pfg = posf[0]
            pi_, pig = posi[0]
            V(lambda e: e.tensor_scalar(out=pf[:, :, 2:4], in0=sf[:], scalar1=-63.5, scalar2=1.0 / 128.0, op0=ALU.add, op1=ALU.mult), reads=[sfg], writes=[pfg])
            V(lambda e: e.tensor_copy(out=pi_[:], in_=pf[:, :, 2:4]), reads=[pfg], writes=[pig])
            V(lambda e: e.tensor_copy(out=pf[:, :, 2:4], in_=pi_[:]), reads=[pig], writes=[pfg])
            V(lambda e: e.scalar_tensor_tensor(out=pf[:, :, 0:2], in0=pf[:, :, 2:4], scalar=-128.0, in1=sf[:], op0=ALU.mult, op1=ALU.add),
              reads=[pfg, sfg], writes=[pfg])
            V(lambda e: e.scalar_tensor_tensor(out=pf[:, :, 0:2], in0=pf[:, :, 0:2], scalar=float(CAPB), in1=pf[:, :, 2:4], op0=ALU.mult, op1=ALU.add),
              reads=[pfg], writes=[pfg])
            V(lambda e: e.tensor_copy(out=pi_[:], in_=pf[:, :, 0:2]), reads=[pfg], writes=[pig])
            cm, cmg = cmp_[0]
            be, beg = bef[0]
            V(lambda e: e.tensor_scalar(out=be[:], in0=iota_b[:, 0:CAPB], scalar1=128.0, scalar2=None, op0=ALU.mult), reads=[cfg_], writes=[beg])
            V(lambda e: e.tensor_tensor(out=cm[:], in0=c_[:, 2, :].unsqueeze(1).to_broadcast([128, CAPB, 32]),
                                        in1=be[:].unsqueeze(2).to_broadcast([128, CAPB, 32]), op=ALU.is_le), reads=[cg_, beg], writes=[cmg])
            V(lambda e: e.tensor_reduce(out=be[:], in_=cm[:], axis=AX.X, op=ALU.add), reads=[cmg], writes=[beg])
            V(lambda e: e.tensor_scalar(out=be[:], in0=be[:], scalar1=31.0, scalar2=None, op0=ALU.min), reads=[beg], writes=[beg])
            sk, skg = skipf[0]
            V(lambda e: e.memset(sk[:], 0.0), writes=[skg])
            V(lambda e: e.tensor_tensor(out=sk[:, 2:CAPB], in0=be[:, 2:CAPB], in1=be[:, 0:CAPB - 2], op=ALU.is_equal), reads=[beg], writes=[skg])
            V(lambda e: e.tensor_scalar(out=be[:], in0=be[:], scalar1=128.0, scalar2=iota_p, op0=ALU.mult, op1=ALU.add), reads=[beg, cfg_], writes=[beg])
            V(lambda e: e.scalar_tensor_tensor(out=be[:], in0=sk[:], scalar=8192.0, in1=be[:], op0=ALU.mult, op1=ALU.add), reads=[skg, beg], writes=[beg])
            wi, wig = widx[0]
            V(lambda e: e.tensor_copy(out=wi[:], in_=be[:]), reads=[beg], writes=[wig])
            tk, tkg = tok[0]
            tki, tkig = toki[0]
            V(lambda e: e.tensor_scalar(out=tk[:], in0=iota_b[:, 0:NT], scalar1=128.0, scalar2=iota_p, op0=ALU.mult, op1=ALU.add),
              reads=[cfg_], writes=[tkg])
            V(lambda e: e.tensor_copy(out=tki[:], in_=tk[:]), reads=[tkg], writes=[tkig])
            fl, flg = fill[0]
            V(lambda e: e.memset(fl[:], NTOK), writes=[flg])
            stok_g = Reg()
            dma("sp", lambda e: e.dma_start(out=stok_d.rearrange("(p b) o -> p (b o)", b=CAPB), in_=fl[:]), reads=[flg], writes=[stok_g])
            for i in range(NT):
                for kk in range(2):
                    dma("pool", lambda e, i=i, kk=kk: e.indirect_dma_start(
                        out=stok_d, out_offset=bass.IndirectOffsetOnAxis(ap=pi_[:, i, kk:kk + 1], axis=0),
                        in_=tki[:, i:i + 1], in_offset=None), reads=[pig, tkig, stok_g], writes=[Reg()])
            sch.barrier(bar_scr[:])
        with ExitStack() as st:
            wi, wig = widx[0]
            sidx = Buf(st, [128, CAPB], I32)
            si, sig = sidx[0]
            dma("sp", lambda e: e.dma_start(out=si[:], in_=stok_d.rearrange("(p b) o -> p (b o)", b=CAPB)), writes=[sig])
            xs = Buf(st, [128, D], BF16, n=2)
            wgu = Buf(st, [128, 8192], BF16, n=2)
            wdn = Buf(st, [128, 4096], BF16, n=2)
            xsT = Buf(st, [128, KC, 128], BF16, n=2)
            sg_ = Buf(st, [128, 256], F32, n=2)
            act_ = Buf(st, [128, 256], BF16, n=2)
            actT = Buf(st, [128, 2, 128], BF16, n=2)
            yo = Buf(st, [128, D], BF16, n=2)
            tpb = Buf(st, [128, 1024], BF16, n=2, ps=True)
            hid = Buf(st, [128, 512], F32, n=2, ps=True)
            yop = Buf(st, [128, 512], F32, n=4, ps=True)
            ys_g = Reg()

            def stA(b):
                xst, xsg = xs[b]
                dma("pool", lambda e, b=b: e.indirect_dma_start(
                    out=xst[:], out_offset=None, in_=h2_d, in_offset=bass.IndirectOffsetOnAxis(ap=si[:, b:b + 1], axis=0)),
                    reads=[sig], writes=[xsg])
                wgt, wgg = wgu[b]
                wdt, wdg = wdn[b]
                dma("pool", lambda e, b=b: e.indirect_dma_start(
                    out=wgt[:], out_offset=None, in_=wgu_d, in_offset=bass.IndirectOffsetOnAxis(ap=wi[:, b:b + 1], axis=0),
                    bounds_check=bc_val, oob_is_err=False),
                    reads=[wig, wcast_g], writes=[wgg])
                dma("pool", lambda e, b=b: e.indirect_dma_start(
                    out=wdt[:], out_offset=None, in_=wd_d, in_offset=bass.IndirectOffsetOnAxis(ap=wi[:, b:b + 1], axis=0),
                    bounds_check=bc_val, oob_is_err=False),
                    reads=[wig, wcast_g], writes=[wdg])
                xTt, xTg = xsT[b]
                xv = xst[:].rearrange("p (q kc) -> p kc q", kc=KC)
                for half in range(2):
                    tpt, tpg = tpb[half]
                    for k8 in range(8):
                        kc = half * 8 + k8
                        op("pe", lambda e, kc=kc, k8=k8, tpt=tpt: e.transpose(tpt[:, k8 * 128:(k8 + 1) * 128], xv[:, kc, :], ident_b),
                           reads=[xsg, cbg], writes=[tpg], signal=(k8 == 7))
                    op("act" if half == 0 else "dve",
                       lambda e, half=half, tpt=tpt: (e.activation(out=xTt[:, half * 8:(half + 1) * 8, :].rearrange("p a b -> p (a b)"),
                                                                   in_=tpt[:], func=AF.Copy) if half == 0 else
                                                      e.tensor_copy(out=xTt[:, half * 8:(half + 1) * 8, :].rearrange("p a b -> p (a b)"), in_=tpt[:])),
                       reads=[tpg], writes=[xTg])
                ht, hg = hid[b]
                for gu_ in range(2):
                    for kc in range(KC):
                        op("pe", lambda e, kc=kc, gu_=gu_: e.matmul(ht[:, gu_ * 256:(gu_ + 1) * 256], lhsT=xTt[:, kc, :],
                                                                   rhs=wgt[:, gu_ * 4096 + kc * 256:gu_ * 4096 + (kc + 1) * 256],
                                                                   start=(kc == 0), stop=(kc == KC - 1)),
                           reads=[xTg, wgg], writes=[hg], signal=(kc == KC - 1 and gu_ == 1))

            def stB(b):
                ht, hg = hid[b]
                wdt, wdg = wdn[b]
                sgt, sgg = sg_[b]
                op("act", lambda e: e.activation(out=sgt[:], in_=ht[:, 0:256], func=AF.Silu), reads=[hg], writes=[sgg])
                att, atg = act_[b]
                op("dve", lambda e: e.tensor_tensor(out=att[:], in0=sgt[:], in1=ht[:, 256:512], op=ALU.mult), reads=[sgg, hg], writes=[atg])
                aTt, aTg = actT[b]
                tpt, tpg = tpb[0]
                av = att[:].rearrange("p (q c) -> p c q", c=2)
                for c in range(2):
                    op("pe", lambda e, c=c: e.transpose(tpt[:, c * 128:(c + 1) * 128], av[:, c, :], ident_b),
                       reads=[atg, cbg], writes=[tpg], signal=(c == 1))
                op("act", lambda e: e.activation(out=aTt[:].rearrange("p a b -> p (a b)"), in_=tpt[:, 0:256], func=AF.Copy), reads=[tpg], writes=[aTg])
                yot, yog = yo[b]
                for nb in range(4):
                    ypt, ypg = yop[nb]
                    for c in range(2):
                        op("pe", lambda e, c=c, nb=nb, ypt=ypt: e.matmul(ypt[:], lhsT=aTt[:, c, :],
                                                                       rhs=wdt[:, c * 2048 + nb * 512:c * 2048 + (nb + 1) * 512],
                                                                       start=(c == 0), stop=(c == 1)),
                           reads=[aTg, wdg], writes=[ypg], signal=(c == 1))
                    op("act" if nb % 2 == 1 else "dve",
                       lambda e, nb=nb, ypt=ypt: (e.activation(out=yot[:, nb * 512:(nb + 1) * 512], in_=ypt[:], func=AF.Copy) if nb % 2 == 1
                                                  else e.tensor_copy(out=yot[:, nb * 512:(nb + 1) * 512], in_=ypt[:])),
                       reads=[ypg], writes=[yog])
                dma("act", lambda e, b=b: e.dma_start(out=ys_d[b * 128:(b + 1) * 128, :], in_=yot[:]), reads=[yog], writes=[ys_g])

            stA(0)
            for b in range(CAPB):
                if b + 1 < CAPB:
                    stA(b + 1)
                stB(b)
            sch.barrier(bar_scr[:])
        with ExitStack() as st:
            gate2 = Buf(st, [128, D], F32)
            dma("sp", lambda e: e.dma_start(out=gate2[0][0][:], in_=modl_d[l, 5]), writes=[gate2[0][1]])
            p0 = Buf(st, [128, D], BF16, n=2)
            p1 = Buf(st, [128, D], BF16, n=2)
            xmb = Buf(st, [128, D], F32, n=2)
            mm = Buf(st, [128, D], F32, n=2)
            out_g = Reg()
            for i in range(NT):
                a0, a0g = p0[i]
                a1, a1g = p1[i]
                dma("pool", lambda e, i=i: e.indirect_dma_start(
                    out=a0[:], out_offset=None, in_=ys_d, in_offset=bass.IndirectOffsetOnAxis(ap=slt[:, i, 0:1], axis=0)),
                    reads=[slg], writes=[a0g])
                dma("pool", lambda e, i=i: e.indirect_dma_start(
                    out=a1[:], out_offset=None, in_=ys_d, in_offset=bass.IndirectOffsetOnAxis(ap=slt[:, i, 1:2], axis=0)),
                    reads=[slg], writes=[a1g])
                xt, xg = xmb[i]
                dma("sp", lambda e, i=i: e.dma_start(out=xt[:], in_=xm_d[i * 128:(i + 1) * 128, :]), writes=[xg])
                mt, mg = mm[i]
                op("act", lambda e, i=i: e.activation(out=mt[:], in_=a0[:], func=AF.Copy, scale=wwt[:, i, 0:1]),
                   reads=[a0g, wwg], writes=[mg])
                op("dve", lambda e, i=i: e.scalar_tensor_tensor(out=mt[:], in0=a1[:], scalar=wwt[:, i, 1:2], in1=mt[:], op0=ALU.mult, op1=ALU.add),
                   reads=[a1g, wwg, mg], writes=[mg])
                op("dve", lambda e: e.tensor_tensor(out=mt[:], in0=mt[:], in1=gate2[0][0][:], op=ALU.mult), reads=[mg, gate2[0][1]], writes=[mg])
                op("dve", lambda e: e.tensor_tensor(out=mt[:], in0=mt[:], in1=xt[:], op=ALU.add), reads=[mg, xg], writes=[mg])
                dma("act", lambda e, i=i: e.dma_start(out=x_dst[i * 128:(i + 1) * 128, :], in_=mt[:]), reads=[mg], writes=[out_g])
            sch.barrier(bar_scr[:])
        moe.close()
    sch.final_wait()
    return nc, sch


def make_consts():
    c = np.zeros((128, 1280), np.float32)
    i = np.arange(128)
    c[:, 0:128] = np.eye(128)
    c[:, 128:256] = (i[:, None] >= i[None, :])
    c[:, 256:384] = (i[:, None] < i[None, :])
    c[:, 384:512] = (i[:, None] < i[None, :])
    c[:, 512:640] = 1.0
    c[:, 640:768] = (i[:, None] <= i[None, :])
    c[:, 768] = i
    c[:, 1024:1280] = np.arange(256)[None, :]
    return c


_CACHE = {}


def run(inputs, trace=False):
    x = np.asarray(inputs["x"], np.float32)
    B, S, _ = x.shape
    G = 1
    NCORES = B
    NBLK = S // 128
    NT = NBLK // G
    depth = inputs["w_in"].shape[0]
    key = (NT, NBLK, G, depth)
    if key not in _CACHE:
        _CACHE[key] = build(NT, NBLK, G, depth)
    nc, sch = _CACHE[key]
    f = lambda a: np.ascontiguousarray(np.asarray(a, np.float32))
    cst = make_consts()
    tri = cst[:, 384:512]
    ones = np.ones((128, 128), np.float32)
    zeros = np.zeros((128, 128), np.float32)
    shared = {
        "w_mod": f(inputs["w_mod"]), "b_mod": f(inputs["b_mod"]).reshape(1, -1), "mod_layer": f(inputs["mod_layer"]),
        "norm1_g": f(inputs["norm1_g"]), "w_in": f(inputs["w_in"]), "gm_norm_g": f(inputs["gm_norm_g"]),
        "gm_wsT": f(np.asarray(inputs["gm_ws"]).transpose(0, 1, 3, 2)), "gm_bsT": f(np.asarray(inputs["gm_bs"]).transpose(0, 2, 1)),
        "q_norm_g": f(inputs["q_norm_g"]), "k_norm_g": f(inputs["k_norm_g"]), "out_norm_g": f(inputs["out_norm_g"]),
        "w_out": f(inputs["w_out"]), "norm2_g": f(inputs["norm2_g"]),
        "w_r": f(np.concatenate([np.asarray(inputs["w_group"]), np.asarray(inputs["w_route"])], axis=-1)),
        "b_r": f(np.concatenate([np.asarray(inputs["b_group"]), np.asarray(inputs["b_route"])], axis=-1)),
        "w_gate": f(inputs["w_gate"]), "w_up": f(inputs["w_up"]), "w_down": f(inputs["w_down"]),
        "consts": cst,
    }
    in_maps = []
    owns = []
    for c in range(NCORES):
        b, r = c // G, c % G
        ob = own_blocks(G, NT, r)
        owns.append((b, ob))
        xs = np.concatenate([x[b, n * 128:(n + 1) * 128] for n in ob], axis=0)
        if G == 1:
            am = np.stack([tri, ones, tri, ones], axis=1)
        elif r == 0:
            am = np.stack([zeros, tri, tri, ones], axis=1)
        else:
            am = np.stack([tri, ones, zeros, tri], axis=1)
        m = dict(shared)
        m["x"] = np.ascontiguousarray(xs)
        m["cT"] = f(np.asarray(inputs["c"])[b].reshape(KC, 128).T)
        m["amask"] = np.ascontiguousarray(am.astype(np.float32))
        in_maps.append(m)
    res = run_bass_kernel_spmd(nc, in_maps, core_ids=list(range(NCORES)), **({"trace": True} if trace else {}))
    out = np.empty_like(x)
    for c in range(NCORES):
        b, ob = owns[c]
        o = res.results[c]["out"]
        for t, n in enumerate(ob):
            out[b, n * 128:(n + 1) * 128] = o[t * 128:(t + 1) * 128]
    return out, res


def kernel(**inputs):
    out, _ = run(inputs)
    return out
```

```python
import numpy as np
import ml_dtypes
from contextlib import ExitStack
import concourse.bass as bass
import concourse.mybir as mybir
from concourse.bass_utils import run_bass_kernel_spmd

F32 = mybir.dt.float32
BF16 = mybir.dt.bfloat16
I32 = mybir.dt.int32
AF = mybir.ActivationFunctionType
ALU = mybir.AluOpType
AX = mybir.AxisListType

D = 2048
KC = 16
DIN = 2560
NEXP = 32
EPS = 1e-6
DEPTH = 4
NMOD = 6


class Reg:
    __slots__ = ("w", "r")

    def __init__(self):
        self.w = None
        self.r = {}


class Sched:
    NDS = 12

    def __init__(self, nc):
        self.nc = nc
        self.engs = {"pe": nc.tensor, "act": nc.scalar, "dve": nc.vector, "pool": nc.gpsimd, "sp": nc.sync}
        self.sem = {k: nc.semaphore("sem_" + k).__enter__() for k in self.engs}
        self.cnt = {k: 0 for k in self.engs}
        self.waited = {k: {} for k in self.engs}
        self.dsems = {}
        self.dcnt = {}
        self.dnext = {}
        for q in ("sp", "pool", "act"):
            self.dsems[q] = [nc.semaphore("ds_%s%d" % (q, i)).__enter__() for i in range(self.NDS)]
            self.dcnt[q] = [0] * self.NDS
            self.dnext[q] = 0
        self.n_inst = 0

    def _semobj(self, key):
        if isinstance(key, str):
            return self.sem[key]
        return self.dsems[key[0]][key[1]]

    def _wait(self, e, key, val):
        if val <= 0 or self.waited[e].get(key, 0) >= val:
            return
        self.engs[e].wait_ge(self._semobj(key), val)
        self.waited[e][key] = val
        self.n_inst += 1

    def _deps(self, e, reads, writes):
        deps = {}
        for r in reads:
            if r.w is not None:
                k, v = r.w
                deps[k] = max(deps.get(k, 0), v)
        for w in writes:
            if w.w is not None:
                k, v = w.w
                deps[k] = max(deps.get(k, 0), v)
            for k, v in w.r.items():
                deps[k] = max(deps.get(k, 0), v)
        for k, v in deps.items():
            if e == "pe" and k == "pe":
                continue
            self._wait(e, k, v)

    def _mark(self, ev, reads, writes):
        k, v = ev
        for r in reads:
            r.r[k] = max(r.r.get(k, 0), v)
        for w in writes:
            w.w = ev
            w.r = {}

    def op(self, e, fn, reads=(), writes=(), signal=True):
        self._deps(e, reads, writes)
        inst = fn(self.engs[e])
        self.n_inst += 1
        if signal:
            self.cnt[e] += 1
            inst.then_inc(self.sem[e], 1)
            ev = (e, self.cnt[e])
        else:
            ev = (e, self.cnt[e] + 1)
        self._mark(ev, reads, writes)

    def dma(self, q, fn, reads=(), writes=()):
        self._deps(q, reads, writes)
        i = self.dnext[q]
        self.dnext[q] = (i + 1) % self.NDS
        key = (q, i)
        self._wait(q, key, self.dcnt[q][i])
        inst = fn(self.engs[q])
        self.n_inst += 1
        self.dcnt[q][i] += 16
        inst.then_inc(self.dsems[q][i], 16)
        self._mark((key, self.dcnt[q][i]), reads, writes)

    def barrier(self, scratch_ap):
        for k in self.engs:
            if k != "pool":
                self._wait("pool", k, self.cnt[k])
        for q in self.dsems:
            if q == "wc":
                continue
            for i in range(len(self.dsems[q])):
                self._wait("pool", (q, i), self.dcnt[q][i])
        inst = self.nc.gpsimd.memset(scratch_ap, 0.0)
        self.cnt["pool"] += 1
        inst.then_inc(self.sem["pool"], 1)
        self.n_inst += 1
        for k in self.engs:
            if k != "pool":
                self._wait(k, "pool", self.cnt["pool"])

    def final_wait(self):
        for q in self.dsems:
            for i in range(len(self.dsems[q])):
                self._wait("sp", (q, i), self.dcnt[q][i])


def own_blocks(G, NT, rank):
    if G == 1:
        return list(range(NT))
    out = []
    for t in range(NT):
        j = t // 2
        if t % 2 == 0:
            out.append(4 * j + (0 if rank == 0 else 1))
        else:
            out.append(4 * j + (3 if rank == 0 else 2))
    return out


def blk_src(G, n):
    if G == 1:
        return (0, n)
    j, c = n // 4, n % 4
    return {0: (0, 2 * j), 1: (1, 2 * j), 2: (1, 2 * j + 1), 3: (0, 2 * j + 1)}[c]


def build(NT, NBLK, G, depth=DEPTH):
    S = NBLK * 128
    NTOK = NT * 128
    CAPB = (2 * NTOK) // 128 + NEXP
    CAP = CAPB * 128
    nc = bass.Bass("TRN2", target_bir_lowering=False)
    dt_in = lambda n, s, d=F32: nc.dram_tensor(n, s, d, kind="ExternalInput").ap()
    dt_sc = lambda n, s, d=F32: nc.dram_tensor(n, s, d, kind="Internal").ap()
    x_in = dt_in("x", [NTOK, D])
    cT_in = dt_in("cT", [128, KC])
    w_mod = dt_in("w_mod", [D, NMOD * D])
    b_mod = dt_in("b_mod", [1, NMOD * D])
    mod_layer = dt_in("mod_layer", [depth, NMOD * D])
    norm1_g = dt_in("norm1_g", [depth, D])
    w_in = dt_in("w_in", [depth, D, DIN])
    gm_norm_g = dt_in("gm_norm_g", [depth, 512])
    gm_wsT = dt_in("gm_wsT", [depth, 4, 128, 128])
    gm_bsT = dt_in("gm_bsT", [depth, 128, 4])
    q_norm_g = dt_in("q_norm_g", [depth, 128])
    k_norm_g = dt_in("k_norm_g", [depth, 128])
    out_norm_g = dt_in("out_norm_g", [depth, 1024])
    w_out = dt_in("w_out", [depth, 1024, D])
    norm2_g = dt_in("norm2_g", [depth, D])
    w_r = dt_in("w_r", [depth, D, 36])
    b_r = dt_in("b_r", [depth, 36])
    w_gate = dt_in("w_gate", [depth, NEXP, D, 256])
    w_up = dt_in("w_up", [depth, NEXP, D, 256])
    w_down = dt_in("w_down", [depth, NEXP, 256, D])
    consts = dt_in("consts", [128, 1280])
    amask_in = dt_in("amask", [128, 4, 128])
    out_d = nc.dram_tensor("out", [NTOK, D], F32, kind="ExternalOutput").ap()

    xa_d = dt_sc("xa_d", [NTOK, D])
    xm_d = dt_sc("xm_d", [NTOK, D])
    modl_d = dt_sc("modl_d", [depth, NMOD, 128, D])
    kT_d = dt_sc("kT_d", [NT, 128, 512], BF16)
    v_d = dt_sc("v_d", [NT, 128, 512], BF16)
    if G > 1:
        kTall_d = dt_sc("kTall_d", [G * NT, 128, 512], BF16)
        vall_d = dt_sc("vall_d", [G * NT, 128, 512], BF16)
    else:
        kTall_d, vall_d = kT_d, v_d
    qT_d = dt_sc("qT_d", [NT, 128, 512], BF16)
    yT_d = dt_sc("yT_d", [NT, 128, 1024], BF16)
    h2_d = dt_sc("h2_d", [NTOK + 1, D], BF16)
    wgu_d = dt_sc("wgu_d", [NEXP * 128, 8192], BF16)
    wd_d = dt_sc("wd_d", [NEXP * 128, 4096], BF16)
    stok_d = dt_sc("stok_d", [CAP, 1], I32)
    ys_d = dt_sc("ys_d", [CAP, D], BF16)

    sch = Sched(nc)
    op, dma = sch.op, sch.dma
    uid = [0]

    def alloc(st, shape, dt, ps=False):
        uid[0] += 1
        f = nc.psum_tensor if ps else nc.sbuf_tensor
        return st.enter_context(f("t%d" % uid[0], shape, dt))

    class Buf:
        def __init__(self, st, shape, dt, n=1, ps=False):
            self.t = [alloc(st, shape, dt, ps) for _ in range(n)]
            self.g = [Reg() for _ in range(n)]
            self.n = n

        def __getitem__(self, i):
            return self.t[i % self.n], self.g[i % self.n]

    top = ExitStack()
    cst_f = Buf(top, [128, 1280], F32)
    cst_b = Buf(top, [128, 1280], BF16)
    bar_scr = alloc(top, [128, 1], F32)
    cf, cfg_ = cst_f[0]
    cb, cbg = cst_b[0]
    dma("sp", lambda e: e.dma_start(out=cf[:], in_=consts), writes=[cfg_])
    op("dve", lambda e: e.tensor_copy(out=cb[:], in_=cf[:]), reads=[cfg_], writes=[cbg])
    ident_f = cf[:, 0:128]
    ident_b = cb[:, 0:128]
    triIncl_b = cb[:, 128:256]
    triLow_b = cb[:, 256:384]
    ones_b = cb[:, 512:640]
    gmMaskT_f = cf[:, 640:768]
    iota_p = cf[:, 768:769]
    iota_b = cf[:, 1024:1280]
    amask = Buf(top, [128, 4, 128], F32)
    am, amg = amask[0]
    dma("sp", lambda e: e.dma_start(out=am[:], in_=amask_in), writes=[amg])
    zrow = Buf(top, [1, D], BF16)
    zr, zrg = zrow[0]
    op("pool", lambda e: e.memset(zr[:], 0.0), writes=[zrg])
    h2z_g = Reg()
    dma("sp", lambda e: e.dma_start(out=h2_d[NTOK:NTOK + 1, :], in_=zr[:]), reads=[zrg], writes=[h2z_g])

    def rstd_from(ssq_ap, out_ap, n, greads, gwrite):
        op("dve", lambda e: e.tensor_scalar(out=out_ap, in0=ssq_ap, scalar1=1.0 / n, scalar2=EPS, op0=ALU.mult, op1=ALU.add),
           reads=greads, writes=[gwrite])
        op("act", lambda e: e.activation(out=out_ap, in_=out_ap, func=AF.Ln), reads=[gwrite], writes=[gwrite])
        op("act", lambda e: e.activation(out=out_ap, in_=out_ap, func=AF.Exp, scale=-0.5), reads=[gwrite], writes=[gwrite])

    with ExitStack() as st:
        cTt = Buf(st, [128, KC], F32)
        sc_b = Buf(st, [128, KC, 128], F32)
        wm = Buf(st, [128, KC, 512], F32, n=2)
        msh = Buf(st, [128, NMOD * D], F32)
        pm = Buf(st, [128, 512], F32, n=2, ps=True)
        bm = Buf(st, [128, 2048], F32, n=2)
        g_t = Buf(st, [128, 2048], F32, n=2)
        o_t = Buf(st, [128, 2048], F32, n=2)
        c_t, c_g = cTt[0]
        s_t, s_g = sc_b[0]
        m_t, m_g = msh[0]
        dma("sp", lambda e: e.dma_start(out=c_t[:], in_=cT_in), writes=[c_g])
        op("act", lambda e: e.activation(out=c_t[:], in_=c_t[:], func=AF.Silu), reads=[c_g], writes=[c_g])
        for kc in range(KC):
            op("dve", lambda e, kc=kc: e.tensor_copy(out=s_t[:, kc, :], in_=c_t[:, kc:kc + 1].to_broadcast([128, 128])),
               reads=[c_g], writes=[s_g])
        NG = NMOD * D // 512
        for n in range(NG):
            w_t, w_g = wm[n]
            dma("sp", lambda e, n=n, w_t=w_t: e.dma_start(
                out=w_t[:], in_=w_mod[:, n * 512:(n + 1) * 512].rearrange("(kc p) n -> p kc n", p=128)), writes=[w_g])
            p_t, p_g = pm[n]
            for kc in range(KC):
                op("pe", lambda e, kc=kc, p_t=p_t, w_t=w_t: e.matmul(p_t[:], lhsT=s_t[:, kc, :], rhs=w_t[:, kc, :],
                                                                   start=(kc == 0), stop=(kc == KC - 1)),
                   reads=[s_g, w_g], writes=[p_g], signal=(kc == KC - 1))
            op("act", lambda e, n=n, p_t=p_t: e.activation(out=m_t[:, n * 512:(n + 1) * 512], in_=p_t[:], func=AF.Copy),
               reads=[p_g], writes=[m_g])
        for j in range(NMOD):
            b_t, b_g = bm[j]
            dma("sp", lambda e, j=j, b_t=b_t: e.dma_start(out=b_t[:], in_=b_mod[0:1, j * D:(j + 1) * D].partition_broadcast(128)),
                writes=[b_g])
            op("dve", lambda e, j=j, b_t=b_t: e.tensor_tensor(out=m_t[:, j * D:(j + 1) * D], in0=m_t[:, j * D:(j + 1) * D],
                                                             in1=b_t[:], op=ALU.add), reads=[b_g, m_g], writes=[m_g])
        modl_g = Reg()
        k = 0
        for l in range(depth):
            for j in range(NMOD):
                b_t, b_g = bm[k]
                o_tt, o_g = o_t[k]
                dma("sp", lambda e, l=l, j=j, b_t=b_t: e.dma_start(
                    out=b_t[:], in_=mod_layer[l:l + 1, j * D:(j + 1) * D].partition_broadcast(128)), writes=[b_g])
                if j in (1, 4):
                    gg_t, gg_g = g_t[k]
                    gsrc = norm1_g if j == 1 else norm2_g
                    dma("sp", lambda e, l=l, gg_t=gg_t, gsrc=gsrc: e.dma_start(
                        out=gg_t[:], in_=gsrc[l:l + 1, :].partition_broadcast(128)), writes=[gg_g])
                    op("dve", lambda e, j=j, b_t=b_t, o_tt=o_tt: e.scalar_tensor_tensor(
                        out=o_tt[:], in0=m_t[:, j * D:(j + 1) * D], scalar=1.0, in1=b_t[:], op0=ALU.add, op1=ALU.add),
                       reads=[m_g, b_g], writes=[o_g])
                    op("dve", lambda e, o_tt=o_tt, gg_t=gg_t: e.tensor_tensor(out=o_tt[:], in0=o_tt[:], in1=gg_t[:], op=ALU.mult),
                       reads=[o_g, gg_g], writes=[o_g])
                else:
                    op("dve", lambda e, j=j, b_t=b_t, o_tt=o_tt: e.tensor_tensor(
                        out=o_tt[:], in0=m_t[:, j * D:(j + 1) * D], in1=b_t[:], op=ALU.add), reads=[m_g, b_g], writes=[o_g])
                dma("sp", lambda e, l=l, j=j, o_tt=o_tt: e.dma_start(out=modl_d[l, j], in_=o_tt[:]), reads=[o_g], writes=[modl_g])
                k += 1
        sch.barrier(bar_scr[:])

    bc_reg = nc.gpsimd.register("bc_reg").__enter__()
    nc.gpsimd.reg_mov(bc_reg, NEXP * 128 - 1)
    bc_val = nc.gpsimd.snap(bc_reg)
    wcast_g = Reg()
    wc_sem = nc.semaphore("wcast_sem").__enter__()
    sch.dsems["wc"] = [wc_sem]
    sch.dcnt["wc"] = [0]

    def cast_expert_weights(l, ex):
        for (o, i_) in ((wgu_d[ex * 128:(ex + 1) * 128, 0:4096], w_gate[l, ex].rearrange("(p kc) f -> p (kc f)", kc=KC)),
                        (wgu_d[ex * 128:(ex + 1) * 128, 4096:8192], w_up[l, ex].rearrange("(p kc) f -> p (kc f)", kc=KC)),
                        (wd_d[ex * 128:(ex + 1) * 128, :], w_down[l, ex].rearrange("(p c) n -> p (c n)", c=2))):
            inst = nc.gpsimd.dma_start(out=o, in_=i_)
            sch.dcnt["wc"][0] += 16
            inst.then_inc(wc_sem, 16)
            sch.n_inst += 1
        wcast_g.w = (("wc", 0), sch.dcnt["wc"][0])

    def bload(q, t, g, src_row):
        dma(q, lambda e: e.dma_start(out=t, in_=src_row.partition_broadcast(128)), writes=[g])

    for l in range(depth):
        x_src = x_in if l == 0 else xa_d
        x_dst = out_d if l == depth - 1 else xa_d
        lay = ExitStack()
        with ExitStack() as st:
            winb = Buf(st, [128, KC, DIN], BF16)
            wi_t, wi_g = winb[0]
            for kc in range(KC):
                dma("pool", lambda e, kc=kc: e.dma_start(out=wi_t[:, kc, :], in_=w_in[l, kc * 128:(kc + 1) * 128, :]), writes=[wi_g])
            gs1 = Buf(st, [128, D], F32)
            sh1 = Buf(st, [128, D], F32)
            dma("sp", lambda e: e.dma_start(out=gs1[0][0][:], in_=modl_d[l, 1]), writes=[gs1[0][1]])
            dma("sp", lambda e: e.dma_start(out=sh1[0][0][:], in_=modl_d[l, 0]), writes=[sh1[0][1]])
            wmT_f = Buf(st, [128, 4, 128], F32)
            wmT = Buf(st, [128, 4, 128], BF16)
            dma("sp", lambda e: e.dma_start(out=wmT_f[0][0][:], in_=gm_wsT[l].rearrange("g p t -> p g t")), writes=[wmT_f[0][1]])
            op("dve", lambda e: e.tensor_tensor(out=wmT[0][0][:], in0=wmT_f[0][0][:],
                                               in1=gmMaskT_f.unsqueeze(1).to_broadcast([128, 4, 128]), op=ALU.mult),
               reads=[wmT_f[0][1], cfg_], writes=[wmT[0][1]])
            bsT = Buf(st, [128, 4], F32)
            dma("sp", lambda e: e.dma_start(out=bsT[0][0][:], in_=gm_bsT[l]), writes=[bsT[0][1]])
            gmn = Buf(st, [128, 512], F32)
            bload("sp", gmn[0][0][:], gmn[0][1], gm_norm_g[l:l + 1, :])
            oga = Buf(st, [128, 512], F32)
            bload("sp", oga[0][0][:], oga[0][1], out_norm_g[l:l + 1, 0:512])
            gq = Buf(st, [128, 128], F32)
            gk = Buf(st, [128, 128], F32)
            bload("sp", gq[0][0][:], gq[0][1], q_norm_g[l:l + 1, :])
            bload("sp", gk[0][0][:], gk[0][1], k_norm_g[l:l + 1, :])
            op("dve", lambda e: e.tensor_scalar(out=gq[0][0][:], in0=gq[0][0][:], scalar1=128.0 ** -0.5, scalar2=None, op0=ALU.mult),
               reads=[gq[0][1]], writes=[gq[0][1]])
            xb = Buf(st, [128, D], F32, n=2)
            ssb = Buf(st, [128, 2], F32, n=2)
            hf = Buf(st, [128, D], F32, n=1)
            hb = Buf(st, [128, D], BF16, n=2)
            hT = Buf(st, [128, KC, 128], BF16, n=2)
            gu = Buf(st, [128, 512], F32, n=2)
            gv = Buf(st, [128, 512], F32, n=2)
            sq = Buf(st, [128, 512], F32, n=2)
            sqq = Buf(st, [128, 1024], F32, n=1)
            sm = Buf(st, [128, 16], F32, n=2)
            sm2 = Buf(st, [128, 16], F32, n=2)
            vn = Buf(st, [128, 512], BF16, n=2)
            t1 = Buf(st, [128, 512], F32, n=1)
            ynb = Buf(st, [128, 512], BF16, n=2)
            qn = Buf(st, [128, 1024], BF16, n=2)
            qf = Buf(st, [128, 1024], F32, n=1)
            yst = Buf(st, [128, 512], BF16, n=2)
            kst = Buf(st, [128, 512], BF16, n=2)
            qst = Buf(st, [128, 512], BF16, n=2)
            vst = Buf(st, [128, 512], BF16, n=2)
            tp = Buf(st, [128, 1024], BF16, n=2, ps=True)
            pp = [Buf(st, [128, 512], F32, n=1, ps=True) for _ in range(5)]
            sgp = Buf(st, [128, 512], F32, n=1, ps=True)
            yTa_g = Reg()
            kv_g = Reg()
            G4 = lambda ap: ap.rearrange("p (g k) -> p g k", g=4)

            def front_a(i):
                for ex in range(i * NEXP // NT, (i + 1) * NEXP // NT):
                    cast_expert_weights(l, ex)
                xt, xg = xb[i]
                dma("sp", lambda e: e.dma_start(out=xt[:], in_=x_src[i * 128:(i + 1) * 128, :]), writes=[xg])
                s_t, s_g = ssb[i]
                hft, hfg = hf[i]
                hbt, hbg = hb[i]
                op("act", lambda e: e.activation(out=hbt[:], in_=xt[:], func=AF.Square, accum_out=s_t[:, 0:1]),
                   reads=[xg], writes=[hbg, s_g])
                rstd_from(s_t[:, 0:1], s_t[:, 1:2], D, [s_g], s_g)
                op("dve", lambda e: e.scalar_tensor_tensor(out=hft[:], in0=xt[:], scalar=s_t[:, 1:2], in1=gs1[0][0][:],
                                                          op0=ALU.mult, op1=ALU.mult), reads=[xg, s_g, gs1[0][1]], writes=[hfg])
                op("pool", lambda e: e.tensor_tensor(out=hbt[:], in0=hft[:], in1=sh1[0][0][:], op=ALU.add),
                   reads=[hfg, sh1[0][1]], writes=[hbg])

            def front_b(i):
                hbt, hbg = hb[i]
                hTt, hTg = hT[i]
                for half in range(2):
                    tpt, tpg = tp[half]
                    for k8 in range(8):
                        kc = half * 8 + k8
                        op("pe", lambda e, kc=kc, k8=k8: e.transpose(tpt[:, k8 * 128:(k8 + 1) * 128],
                                                                    hbt[:, kc * 128:(kc + 1) * 128], ident_b),
                           reads=[hbg, cbg], writes=[tpg], signal=(k8 == 7))
                    dst = hTt[:, half * 8:(half + 1) * 8, :].rearrange("p a b -> p (a b)")
                    if half == 0:
                        op("act", lambda e: e.activation(out=dst, in_=tpt[:], func=AF.Copy), reads=[tpg], writes=[hTg])
                    else:
                        op("dve", lambda e: e.tensor_copy(out=dst, in_=tpt[:]), reads=[tpg], writes=[hTg])

            def mid(i, nbs=range(5)):
                hTt, hTg = hT[i]
                for nb in nbs:
                    ppt, ppg = pp[nb][0]
                    for kc in range(KC):
                        op("pe", lambda e, kc=kc: e.matmul(ppt[:], lhsT=hTt[:, kc, :], rhs=wi_t[:, kc, nb * 512:(nb + 1) * 512],
                                                          start=(kc == 0), stop=(kc == KC - 1)),
                           reads=[hTg, wi_g], writes=[ppg], signal=(kc == KC - 1))

            def back1(i):
                gut, gug = gu[i]
                gvt, gvg = gv[i]
                op("act", lambda e: e.activation(out=gut[:], in_=pp[0][0][0][:], func=AF.Gelu), reads=[pp[0][0][1]], writes=[gug])
                op("act", lambda e: e.activation(out=gvt[:], in_=pp[1][0][0][:], func=AF.Gelu), reads=[pp[1][0][1]], writes=[gvg])
                sqqt, sqqg = sqq[i]
                sm2t, sm2g = sm2[i]
                for which in range(2):
                    ppt, ppg = pp[2 + which][0]
                    op("act", lambda e: e.activation(out=sqqt[:, which * 512:(which + 1) * 512], in_=ppt[:], func=AF.Square),
                       reads=[ppg], writes=[sqqg])
                vstt, vstg = vst[i]
                op("act", lambda e: e.activation(out=vstt[:], in_=pp[4][0][0][:], func=AF.Copy), reads=[pp[4][0][1]], writes=[vstg])
                dma("act", lambda e: e.dma_start(out=v_d[i], in_=vstt[:]), reads=[vstg], writes=[kv_g])
                op("dve", lambda e: e.tensor_reduce(out=sm2t[:, 0:8], in_=sqqt[:].rearrange("p (g k) -> p g k", g=8), axis=AX.X, op=ALU.add),
                   reads=[sqqg], writes=[sm2g])
                rstd_from(sm2t[:, 0:8], sm2t[:, 8:16], 128, [sm2g], sm2g)
                qft, qfg = qf[i]
                qnt, qng = qn[i]
                for which in range(2):
                    ppt, ppg = pp[2 + which][0]
                    c0 = which * 512
                    op("dve", lambda e: e.tensor_tensor(out=G4(qft[:, c0:c0 + 512]), in0=G4(ppt[:]),
                                                       in1=sm2t[:, 8 + which * 4:12 + which * 4].unsqueeze(2).to_broadcast([128, 4, 128]), op=ALU.mult),
                       reads=[ppg, sm2g], writes=[qfg])
                    gsel = gq if which == 0 else gk
                    op("dve", lambda e: e.tensor_tensor(out=G4(qnt[:, c0:c0 + 512]), in0=G4(qft[:, c0:c0 + 512]),
                                                       in1=gsel[0][0][:].unsqueeze(1).to_broadcast([128, 4, 128]), op=ALU.mult),
                       reads=[qfg, gsel[0][1]], writes=[qng])

            def back1v(i):
                gvt, gvg = gv[i]
                sqt, sqg = sq[i]
                smt, smg = sm[i]
                op("pool", lambda e: e.tensor_tensor(out=sqt[:], in0=gvt[:], in1=gvt[:], op=ALU.mult), reads=[gvg], writes=[sqg])
                op("dve", lambda e: e.tensor_reduce(out=smt[:, 0:4], in_=G4(sqt[:]), axis=AX.X, op=ALU.add), reads=[sqg], writes=[smg])
                rstd_from(smt[:, 0:4], smt[:, 4:8], 128, [smg], smg)
                vnt, vng = vn[i]
                op("dve", lambda e: e.tensor_tensor(out=G4(vnt[:]), in0=G4(gvt[:]),
                                                   in1=smt[:, 4:8].unsqueeze(2).to_broadcast([128, 4, 128]), op=ALU.mult),
                   reads=[gvg, smg], writes=[vng])

            def sgmm(i):
                vnt, vng = vn[i]
                sgt, sgg = sgp[0]
                for g in range(4):
                    op("pe", lambda e, g=g: e.matmul(sgt[:, g * 128:(g + 1) * 128], lhsT=wmT[0][0][:, g, :],
                                                    rhs=vnt[:, g * 128:(g + 1) * 128], start=True, stop=True),
                       reads=[wmT[0][1], vng], writes=[sgg], signal=(g == 3))

            def back2(i):
                gut, gug = gu[i]
                sqt, sqg = sq[i]
                smt, smg = sm[i]
                sgt, sgg = sgp[0]
                t1t, t1g = t1[i]
                op("dve", lambda e: e.tensor_tensor(out=t1t[:], in0=sgt[:], in1=gmn[0][0][:], op=ALU.mult),
                   reads=[sgg, gmn[0][1]], writes=[t1g])
                op("dve", lambda e: e.tensor_tensor(out=G4(t1t[:]), in0=G4(t1t[:]),
                                                   in1=bsT[0][0][:].unsqueeze(2).to_broadcast([128, 4, 128]), op=ALU.add),
                   reads=[t1g, bsT[0][1]], writes=[t1g])
                op("dve", lambda e: e.tensor_tensor(out=t1t[:], in0=t1t[:], in1=gut[:], op=ALU.mult), reads=[t1g, gug], writes=[t1g])
                op("pool", lambda e: e.tensor_tensor(out=sqt[:], in0=t1t[:], in1=t1t[:], op=ALU.mult), reads=[t1g], writes=[sqg])
                op("dve", lambda e: e.tensor_reduce(out=smt[:, 8:12], in_=G4(sqt[:]), axis=AX.X, op=ALU.add), reads=[sqg], writes=[smg])
                rstd_from(smt[:, 8:12], smt[:, 12:16], 128, [smg], smg)
                op("dve", lambda e: e.tensor_tensor(out=G4(t1t[:]), in0=G4(t1t[:]),
                                                   in1=smt[:, 12:16].unsqueeze(2).to_broadcast([128, 4, 128]), op=ALU.mult),
                   reads=[t1g, smg], writes=[t1g])
                ynt, yng = ynb[i]
                op("dve", lambda e: e.tensor_tensor(out=ynt[:], in0=t1t[:], in1=oga[0][0][:], op=ALU.mult),
                   reads=[t1g, oga[0][1]], writes=[yng])
                tpt, tpg = tp[0]
                for g in range(4):
                    op("pe", lambda e, g=g: e.transpose(tpt[:, g * 128:(g + 1) * 128], ynt[:, g * 128:(g + 1) * 128], ident_b),
                       reads=[yng, cbg], writes=[tpg], signal=(g == 3))
                ystt, ystg = yst[i]
                op("act", lambda e: e.activation(out=ystt[:], in_=tpt[:, 0:512], func=AF.Copy), reads=[tpg], writes=[ystg])
                dma("act", lambda e: e.dma_start(out=yT_d[i, :, 0:512], in_=ystt[:]), reads=[ystg], writes=[yTa_g])

            def late_qk(i):
                qnt, qng = qn[i]
                tpt, tpg = tp[1]
                for k8 in range(8):
                    op("pe", lambda e, k8=k8: e.transpose(tpt[:, k8 * 128:(k8 + 1) * 128], qnt[:, k8 * 128:(k8 + 1) * 128], ident_b),
                       reads=[qng, cbg], writes=[tpg], signal=(k8 == 7))
                qstt, qstg = qst[i]
                op("act", lambda e: e.activation(out=qstt[:], in_=tpt[:, 0:512], func=AF.Copy), reads=[tpg], writes=[qstg])
                dma("act", lambda e: e.dma_start(out=qT_d[i], in_=qstt[:]), reads=[qstg], writes=[kv_g])
                kstt, kstg = kst[i]
                op("act", lambda e: e.activation(out=kstt[:], in_=tpt[:, 512:1024], func=AF.Copy), reads=[tpg], writes=[kstg])
                dma("act", lambda e: e.dma_start(out=kT_d[i], in_=kstt[:]), reads=[kstg], writes=[kv_g])

            front_a(0)
            front_b(0)
            mid(0)
            if NT > 1:
                front_a(1)
            for i in range(NT):
                if i + 1 < NT:
                    front_b(i + 1)
                back1(i)
                back1v(i)
                if i + 1 < NT:
                    mid(i + 1, range(0, 2))
                sgmm(i)
                if i + 1 < NT:
                    mid(i + 1, range(2, 5))
                if i + 2 < NT:
                    front_a(i + 2)
                back2(i)
                late_qk(i)
            sch.barrier(bar_scr[:])
        if G > 1:
            groups = [[g * G + r for r in range(G)] for g in range(8 // G)]
            sch._deps("pool", [], [])
            for src, dst in ((kT_d, kTall_d), (v_d, vall_d)):
                dma("pool", lambda e, src=src, dst=dst: e.collective_compute(
                    "AllGather", ALU.bypass, replica_groups=groups,
                    ins=[src.rearrange("t p c -> (t p) c")], outs=[dst.rearrange("t p c -> (t p) c")]), writes=[Reg()])
            sch.barrier(bar_scr[:])
        with ExitStack() as st:
            NCH = 2
            KT = Buf(st, [128, 4, S], BF16)
            VV = Buf(st, [128, NBLK, 512], BF16)
            KTt, _ = KT[0]
            VVt, _ = VV[0]
            kreg = [Reg() for _ in range(NBLK)]
            vreg = [Reg() for _ in range(NBLK)]
            for n in range(NBLK):
                r, t = blk_src(G, n)
                row = r * NT + t
                dma("sp", lambda e, n=n, row=row: e.dma_start(out=KTt[:, :, n * 128:(n + 1) * 128],
                                                             in_=kTall_d[row].rearrange("p (h t) -> p h t", h=4)), writes=[kreg[n]])
                dma("sp", lambda e, n=n, row=row: e.dma_start(out=VVt[:, n, :], in_=vall_d[row]), writes=[vreg[n]])
            ogb = Buf(st, [128, 512], F32)
            bload("sp", ogb[0][0][:], ogb[0][1], out_norm_g[l:l + 1, 512:1024])
            eb = Buf(st, [128, 512], F32, n=2 * NCH)
            spb = Buf(st, [128, 512], BF16, n=3 * NCH)
            wb = Buf(st, [128, 512], F32, n=NCH + 1)
            ab = Buf(st, [128, 512], BF16, n=2 * NCH)
            qtb = Buf(st, [128, 4, 128], BF16, n=2 * NCH)
            zps = Buf(st, [128, 512], F32, n=NCH, ps=True)
            tpo = Buf(st, [128, 512], F32, n=1, ps=True)
            cps = Buf(st, [128, 512], F32, n=NCH, ps=True)
            ops_ = Buf(st, [128, 512], F32, n=NCH, ps=True)
            osq = Buf(st, [128, 512], F32, n=2)
            osm = Buf(st, [128, 8], F32, n=2)
            of = Buf(st, [128, 512], F32, n=2)
            ost = Buf(st, [128, 512], BF16, n=2)
            yTb_g = Reg()
            tiles = [(i, i, i + 1) for i in range(NT)]
            zctr = [0]
            fin_ctr = [0]

            def finish(t, c):
                ot, og_ = ops_[c]
                k = fin_ctr[0]
                fin_ctr[0] += 1
                sqt, sqg = osq[k]
                smt, smg = osm[k]
                op("act", lambda e: e.activation(out=sqt[:], in_=ot[:], func=AF.Square), reads=[og_], writes=[sqg])
                op("dve", lambda e: e.tensor_reduce(out=smt[:, 0:4], in_=sqt[:].rearrange("p (g k) -> p g k", g=4), axis=AX.X, op=ALU.add),
                   reads=[sqg], writes=[smg])
                rstd_from(smt[:, 0:4], smt[:, 4:8], 128, [smg], smg)
                oft, ofg = of[k]
                op("dve", lambda e: e.tensor_tensor(out=oft[:].rearrange("p (g k) -> p g k", g=4),
                                                   in0=ot[:].rearrange("p (g k) -> p g k", g=4),
                                                   in1=smt[:, 4:8].unsqueeze(2).to_broadcast([128, 4, 128]), op=ALU.mult),
                   reads=[og_, smg], writes=[ofg])
                op("dve", lambda e: e.tensor_tensor(out=oft[:], in0=oft[:], in1=ogb[0][0][:], op=ALU.mult),
                   reads=[ofg, ogb[0][1]], writes=[ofg])
                tpt, tpg = tpo[0]
                for g in range(4):
                    op("pe", lambda e, g=g: e.transpose(tpt[:, g * 128:(g + 1) * 128], oft[:, g * 128:(g + 1) * 128], ident_f),
                       reads=[ofg, cfg_], writes=[tpg], signal=(g == 3))
                ostt, ostg = ost[k]
                op("act", lambda e: e.activation(out=ostt[:], in_=tpt[:], func=AF.Copy), reads=[tpg], writes=[ostg])
                dma("act", lambda e: e.dma_start(out=yT_d[t, :, 512:1024], in_=ostt[:]), reads=[ostg], writes=[yTb_g])

            gctr = [0]
            for g0 in range(0, NT, NCH):
                grp = tiles[g0:g0 + NCH]
                Lmax = max(x[2] for x in grp)
                nch = len(grp)
                gi = gctr[0]
                gctr[0] += 1
                qts = []
                for c, (t, nmax, L) in enumerate(grp):
                    QTt, qg_ = qtb[gi * NCH + c]
                    dma("sp", lambda e, t=t, QTt=QTt: e.dma_start(out=QTt[:], in_=qT_d[t].rearrange("p (h t) -> p h t", h=4)), writes=[qg_])
                    qts.append((QTt, qg_))
                zb = {}
                spprev = {}

                def E1(r):
                    for c, (t, nmax, L) in enumerate(grp):
                        if r >= L:
                            continue
                        m = nmax - r
                        zt, zg = zps[c]
                        QTt, qg_ = qts[c]
                        for h in range(4):
                            op("pe", lambda e, h=h: e.matmul(zt[:, h * 128:(h + 1) * 128], lhsT=KTt[:, h, m * 128:(m + 1) * 128],
                                                            rhs=QTt[:, h, :], start=True, stop=True),
                               reads=[kreg[m], qg_], writes=[zg], signal=(h == 3))
                        zb[(r, c)] = (zt, zg)

                cur = {}
                E1(0)
                for r in range(Lmax):
                    act_c = [c for c, (t, nmax, L) in enumerate(grp) if r < L]
                    for c in act_c:
                        t, nmax, L = grp[c]
                        zt, zg = zb.pop((r, c))
                        idx = (gi * 64 + r) * NCH + c
                        et, eg = eb[idx]
                        spt, spg = spb[idx]
                        op("act", lambda e: e.activation(out=et[:], in_=zt[:], func=AF.Exp), reads=[zg], writes=[eg])
                        op("act", lambda e: e.activation(out=spt[:], in_=et[:], func=AF.Ln, bias=1.0), reads=[eg], writes=[spg])
                        if r == 0:
                            mk = am[:, 0, :]
                            op("pool", lambda e: e.tensor_tensor(out=spt[:].rearrange("p (h t) -> p h t", h=4),
                                                                in0=spt[:].rearrange("p (h t) -> p h t", h=4),
                                                                in1=mk.unsqueeze(1).to_broadcast([128, 4, 128]), op=ALU.mult),
                               reads=[spg, amg], writes=[spg])
                            op("pool", lambda e: e.tensor_tensor(out=et[:].rearrange("p (h t) -> p h t", h=4),
                                                                in0=et[:].rearrange("p (h t) -> p h t", h=4),
                                                                in1=mk.unsqueeze(1).to_broadcast([128, 4, 128]), op=ALU.mult),
                               reads=[eg, amg], writes=[eg])
                        cur[c] = (et, eg, spt, spg)
                    for c in act_c:
                        et, eg, spt, spg = cur[c]
                        ct, cg = cps[c]
                        prev = spprev.get(c)
                        for h in range(4):
                            hs = slice(h * 128, (h + 1) * 128)
                            if prev is not None:
                                pt_, pg_ = prev
                                op("pe", lambda e, hs=hs: e.matmul(ct[:, hs], lhsT=triLow_b, rhs=pt_[:, hs], start=False, stop=False,
                                                                  skip_group_check=True),
                                   reads=[pg_, cbg], writes=[cg], signal=False)
                            op("pe", lambda e, hs=hs, h=h: e.matmul(ct[:, hs], lhsT=triIncl_b, rhs=spt[:, hs],
                                                                   start=(prev is None and h == 0), stop=True, skip_group_check=True),
                               reads=[spg, cbg], writes=[cg], signal=(h == 3))
                        spprev[c] = (spt, spg)
                    if r + 1 < Lmax:
                        E1(r + 1)
                    ws = {}
                    for c in act_c:
                        ct, cg = cps[c]
                        wt, wg = wb[(gi * 64 + r) * NCH + c]
                        op("act", lambda e: e.activation(out=wt[:], in_=ct[:], func=AF.Exp, scale=-1.0), reads=[cg], writes=[wg])
                        ws[c] = (wt, wg)
                    as_ = {}
                    for c in act_c:
                        et, eg, spt, spg = cur[c]
                        wt, wg = ws[c]
                        at, ag = ab[(gi * 64 + r) * NCH + c]
                        op("dve", lambda e: e.tensor_tensor(out=at[:], in0=et[:], in1=wt[:], op=ALU.mult), reads=[eg, wg], writes=[ag])
                        as_[c] = (at, ag)
                    for c in act_c:
                        t, nmax, L = grp[c]
                        m = nmax - r
                        at, ag = as_[c]
                        ot, og_ = ops_[c]
                        for h in range(4):
                            hs = slice(h * 128, (h + 1) * 128)
                            op("pe", lambda e, hs=hs, h=h: e.matmul(ot[:, hs], lhsT=at[:, hs], rhs=VVt[:, m, hs],
                                                                   start=(r == 0 and h == 0), stop=(r == L - 1), skip_group_check=True),
                               reads=[ag, vreg[m]], writes=[og_], signal=(h == 3))
                        if r == L - 1:
                            finish(t, c)
            sch.barrier(bar_scr[:])
        lay.close()
        moe = ExitStack()
        OH0 = Buf(moe, [128, NT, 32], F32)
        OH1 = Buf(moe, [128, NT, 32], F32)
        AA = Buf(moe, [128, NT, 32], BF16)
        WW = Buf(moe, [128, NT, 2], F32)
        SL = Buf(moe, [128, NT, 2], I32)
        widx = Buf(moe, [128, CAPB], I32)
        oh0t, oh0g = OH0[0]
        oh1t, oh1g = OH1[0]
        aat, aag = AA[0]
        wwt, wwg = WW[0]
        slt, slg = SL[0]
        with ExitStack() as st:
            wob = Buf(st, [128, 8, D], BF16)
            wo_t, wo_g = wob[0]
            for c in range(8):
                dma("pool", lambda e, c=c: e.dma_start(out=wo_t[:, c, :], in_=w_out[l, c * 128:(c + 1) * 128, :]), writes=[wo_g])
            gate1 = Buf(st, [128, D], F32)
            gs2 = Buf(st, [128, D], F32)
            sh2 = Buf(st, [128, D], F32)
            dma("sp", lambda e: e.dma_start(out=gate1[0][0][:], in_=modl_d[l, 2]), writes=[gate1[0][1]])
            dma("sp", lambda e: e.dma_start(out=gs2[0][0][:], in_=modl_d[l, 4]), writes=[gs2[0][1]])
            dma("sp", lambda e: e.dma_start(out=sh2[0][0][:], in_=modl_d[l, 3]), writes=[sh2[0][1]])
            wrt = Buf(st, [128, KC, 36], F32)
            dma("sp", lambda e: e.dma_start(out=wrt[0][0][:], in_=w_r[l].rearrange("(kc p) n -> p kc n", p=128)), writes=[wrt[0][1]])
            brt = Buf(st, [128, 36], F32)
            bload("sp", brt[0][0][:], brt[0][1], b_r[l:l + 1, :])
            yTb = Buf(st, [128, 1024], BF16, n=2)
            xb = Buf(st, [128, D], F32, n=2)
            xm = Buf(st, [128, D], F32, n=2)
            junk = Buf(st, [128, D], BF16, n=1)
            ssb = Buf(st, [128, 2], F32, n=2)
            h2f = Buf(st, [128, D], F32, n=2)
            h2b = Buf(st, [128, D], BF16, n=2)
            h2T = Buf(st, [128, KC, 128], F32, n=1)
            lg = Buf(st, [128, 36], F32, n=2)
            rs = Buf(st, [128, 64], F32, n=2)
            po = [Buf(st, [128, 512], F32, n=1, ps=True) for _ in range(4)]
            tpf = Buf(st, [128, 512], F32, n=2, ps=True)
            lgp = Buf(st, [128, 512], F32, n=1, ps=True)
            xm_g = Reg()
            h2_g = Reg()

            def s1(i):
                yt, yg = yTb[i]
                dma("sp", lambda e, i=i: e.dma_start(out=yt[:], in_=yT_d[i]), writes=[yg])
                xt, xg = xb[i]
                dma("sp", lambda e, i=i: e.dma_start(out=xt[:], in_=x_src[i * 128:(i + 1) * 128, :]), writes=[xg])
                for nb in range(4):
                    pt, pg = po[nb][0]
                    for c in range(8):
                        op("pe", lambda e, c=c, nb=nb, pt=pt: e.matmul(pt[:], lhsT=yt[:, c * 128:(c + 1) * 128],
                                                                     rhs=wo_t[:, c, nb * 512:(nb + 1) * 512], start=(c == 0), stop=(c == 7)),
                           reads=[yg, wo_g], writes=[pg], signal=(c == 7))
                xmt, xmg = xm[i]
                for nb in range(4):
                    pt, pg = po[nb][0]
                    cs = slice(nb * 512, (nb + 1) * 512)
                    op("dve", lambda e, cs=cs, pt=pt: e.tensor_tensor(out=xmt[:, cs], in0=pt[:], in1=gate1[0][0][:, cs], op=ALU.mult),
                       reads=[pg, gate1[0][1]], writes=[xmg])
                op("dve", lambda e: e.tensor_tensor(out=xmt[:], in0=xmt[:], in1=xt[:], op=ALU.add), reads=[xmg, xg], writes=[xmg])
                dma("pool", lambda e, i=i: e.dma_start(out=xm_d[i * 128:(i + 1) * 128, :], in_=xmt[:]), reads=[xmg], writes=[xm_g])
                jt, jg = junk[i]
                s_t, s_g = ssb[i]
                op("act", lambda e: e.activation(out=jt[:], in_=xmt[:], func=AF.Square, accum_out=s_t[:, 0:1]),
                   reads=[xmg], writes=[jg, s_g])
                rstd_from(s_t[:, 0:1], s_t[:, 1:2], D, [s_g], s_g)
                hft, hfg = h2f[i]
                op("dve", lambda e: e.scalar_tensor_tensor(out=hft[:], in0=xmt[:], scalar=s_t[:, 1:2], in1=gs2[0][0][:],
                                                          op0=ALU.mult, op1=ALU.mult), reads=[xmg, s_g, gs2[0][1]], writes=[hfg])
                op("dve", lambda e: e.tensor_tensor(out=hft[:], in0=hft[:], in1=sh2[0][0][:], op=ALU.add),
                   reads=[hfg, sh2[0][1]], writes=[hfg])
                hbt, hbg = h2b[i]
                op("act", lambda e: e.activation(out=hbt[:], in_=hft[:], func=AF.Copy), reads=[hfg], writes=[hbg])
                dma("act", lambda e, i=i: e.dma_start(out=h2_d[i * 128:(i + 1) * 128, :], in_=hbt[:]), reads=[hbg], writes=[h2_g])

            def s2(i):
                hft, hfg = h2f[i]
                hTt, hTg = h2T[i]
                for q4 in range(4):
                    tpt, tpg = tpf[q4]
                    for k4 in range(4):
                        kc = q4 * 4 + k4
                        op("pe", lambda e, kc=kc, k4=k4, tpt=tpt: e.transpose(tpt[:, k4 * 128:(k4 + 1) * 128],
                                                                             hft[:, kc * 128:(kc + 1) * 128], ident_f),
                           reads=[hfg, cfg_], writes=[tpg], signal=(k4 == 3))
                    op("act" if q4 % 2 == 0 else "dve",
                       lambda e, q4=q4, tpt=tpt: (e.activation(out=hTt[:, q4 * 4:(q4 + 1) * 4, :].rearrange("p a b -> p (a b)"),
                                                               in_=tpt[:], func=AF.Copy) if q4 % 2 == 0 else
                                                  e.tensor_copy(out=hTt[:, q4 * 4:(q4 + 1) * 4, :].rearrange("p a b -> p (a b)"), in_=tpt[:])),
                       reads=[tpg], writes=[hTg])
                lpt, lpg = lgp[0]
                for kc in range(KC):
                    op("pe", lambda e, kc=kc: e.matmul(lpt[:, 0:36], lhsT=hTt[:, kc, :], rhs=wrt[0][0][:, kc, :],
                                                      start=(kc == 0), stop=(kc == KC - 1)),
                       reads=[hTg, wrt[0][1]], writes=[lpg], signal=(kc == KC - 1))
                lt, lgg = lg[i]
                r, rg = rs[i]
                V = lambda f, **kw: op("dve", f, **kw)
                V(lambda e: e.tensor_tensor(out=lt[:], in0=lpt[:, 0:36], in1=brt[0][0][:], op=ALU.add), reads=[lpg, brt[0][1]], writes=[lgg])
                V(lambda e: e.tensor_reduce(out=r[:, 0:1], in_=lt[:, 0:4], axis=AX.X, op=ALU.max), reads=[lgg], writes=[rg])
                V(lambda e: e.tensor_scalar(out=r[:, 4:8], in0=lt[:, 0:4], scalar1=r[:, 0:1], scalar2=None, op0=ALU.is_equal),
                  reads=[lgg, rg], writes=[rg])
                V(lambda e: e.tensor_scalar(out=r[:, 1:2], in0=r[:, 0:1], scalar1=-1.0, scalar2=None, op0=ALU.mult), reads=[rg], writes=[rg])
                op("act", lambda e: e.activation(out=r[:, 8:12], in_=lt[:, 0:4], func=AF.Exp, bias=r[:, 1:2], accum_out=r[:, 2:3]),
                   reads=[lgg, rg], writes=[rg])
                V(lambda e: e.reciprocal(out=r[:, 3:4], in_=r[:, 2:3]), reads=[rg], writes=[rg])
                V(lambda e: e.tensor_tensor(out=r[:, 16:48].rearrange("p (g j) -> p g j", g=4),
                                            in0=lt[:, 4:36].rearrange("p (g j) -> p g j", g=4),
                                            in1=r[:, 4:8].unsqueeze(2).to_broadcast([128, 4, 8]), op=ALU.mult),
                  reads=[lgg, rg], writes=[rg])
                V(lambda e: e.tensor_reduce(out=r[:, 48:56], in_=r[:, 16:48].rearrange("p (g j) -> p j g", g=4), axis=AX.X, op=ALU.add),
                  reads=[rg], writes=[rg])
                V(lambda e: e.max(out=r[:, 56:64], in_=r[:, 48:56]), reads=[rg], writes=[rg])
                V(lambda e: e.tensor_scalar(out=r[:, 16:24], in0=r[:, 48:56], scalar1=r[:, 56:57], scalar2=None, op0=ALU.is_equal),
                  reads=[rg], writes=[rg])
                V(lambda e: e.tensor_scalar(out=r[:, 24:32], in0=r[:, 48:56], scalar1=r[:, 57:58], scalar2=None, op0=ALU.is_equal),
                  reads=[rg], writes=[rg])
                V(lambda e: e.tensor_tensor(out=r[:, 8:9], in0=r[:, 57:58], in1=r[:, 56:57], op=ALU.subtract), reads=[rg], writes=[rg])
                op("act", lambda e: e.activation(out=r[:, 9:10], in_=r[:, 8:9], func=AF.Exp), reads=[rg], writes=[rg])
                V(lambda e: e.tensor_scalar(out=r[:, 9:10], in0=r[:, 9:10], scalar1=1.0, scalar2=None, op0=ALU.add), reads=[rg], writes=[rg])
                V(lambda e: e.reciprocal(out=r[:, 10:11], in_=r[:, 9:10]), reads=[rg], writes=[rg])
                V(lambda e, i=i: e.tensor_tensor(out=wwt[:, i, 0:1], in0=r[:, 10:11], in1=r[:, 3:4], op=ALU.mult), reads=[rg], writes=[wwg])
                V(lambda e, i=i: e.tensor_tensor(out=wwt[:, i, 1:2], in0=r[:, 3:4], in1=wwt[:, i, 0:1], op=ALU.subtract),
                  reads=[rg, wwg], writes=[wwg])
                for kk, (oht, ohg) in enumerate(((oh0t, oh0g), (oh1t, oh1g))):
                    V(lambda e, i=i, kk=kk, oht=oht: e.tensor_tensor(
                        out=oht[:, i, :].rearrange("p (g j) -> p g j", g=4),
                        in0=r[:, 4:8].unsqueeze(2).to_broadcast([128, 4, 8]),
                        in1=r[:, 16 + 8 * kk:24 + 8 * kk].unsqueeze(1).to_broadcast([128, 4, 8]), op=ALU.mult),
                      reads=[rg], writes=[ohg])
                V(lambda e, i=i: e.tensor_tensor(out=aat[:, i, :], in0=oh0t[:, i, :], in1=oh1t[:, i, :], op=ALU.add),
                  reads=[oh0g, oh1g], writes=[aag])

            s1(0)
            for i in range(NT):
                if i + 1 < NT:
                    s1(i + 1)
                s2(i)
            sch.barrier(bar_scr[:])
        with ExitStack() as st:
            NE = NT * 32
            Tt = Buf(st, [128, NT, 32], F32)
            Rt = Buf(st, [128, NT, 32], F32)
            Pt = Buf(st, [128, NT, 32], F32)
            cn = Buf(st, [128, 4, 32], F32)
            tmp = Buf(st, [128, NT, 32], F32)
            slf = Buf(st, [128, NT, 2], F32)
            posf = Buf(st, [128, NT, 4], F32)
            posi = Buf(st, [128, NT, 2], I32)
            qint = Buf(st, [128, 32], I32)
            cmp_ = Buf(st, [128, CAPB, 32], F32)
            bef = Buf(st, [128, CAPB], F32)
            skipf = Buf(st, [128, CAPB], F32)
            tok = Buf(st, [128, NT], F32)
            toki = Buf(st, [128, NT], I32)
            fill = Buf(st, [128, CAPB], I32)
            pq = Buf(st, [128, 512], F32, n=2, ps=True)
            T_, Tg = Tt[0]
            R_, Rg = Rt[0]
            P_, Pg = Pt[0]
            c_, cg_ = cn[0]
            V = lambda f, **kw: op("dve", f, **kw)
            aflat = aat[:].rearrange("p t e -> p (t e)")
            for (dst, dg, lhs) in ((T_, Tg, ones_b), (R_, Rg, triLow_b)):
                dflat = dst[:].rearrange("p t e -> p (t e)")
                for c0 in range(0, NE, 512):
                    c1 = min(NE, c0 + 512)
                    pt, pg = pq[c0 // 512]
                    op("pe", lambda e, c0=c0, c1=c1, pt=pt, lhs=lhs: e.matmul(pt[:, 0:c1 - c0], lhsT=lhs, rhs=aflat[:, c0:c1], start=True, stop=True),
                       reads=[aag, cbg], writes=[pg])
                    V(lambda e, c0=c0, c1=c1, pt=pt, dflat=dflat: e.tensor_copy(out=dflat[:, c0:c1], in_=pt[:, 0:c1 - c0]), reads=[pg], writes=[dg])
            V(lambda e: e.memset(P_[:, 0, :], 0.0), writes=[Pg])
            for i in range(1, NT):
                V(lambda e, i=i: e.tensor_tensor(out=P_[:, i, :], in0=P_[:, i - 1, :], in1=T_[:, i - 1, :], op=ALU.add), reads=[Pg, Tg], writes=[Pg])
            V(lambda e: e.tensor_tensor(out=c_[:, 0, :], in0=P_[:, NT - 1, :], in1=T_[:, NT - 1, :], op=ALU.add), reads=[Pg, Tg], writes=[cg_])
            qi, qig = qint[0]
            V(lambda e: e.tensor_scalar(out=c_[:, 1, :], in0=c_[:, 0, :], scalar1=127.0 - 63.5, scalar2=1.0 / 128.0, op0=ALU.add, op1=ALU.mult),
              reads=[cg_], writes=[cg_])
            V(lambda e: e.tensor_copy(out=qi[:, 0:32], in_=c_[:, 1, :]), reads=[cg_], writes=[qig])
            V(lambda e: e.tensor_copy(out=c_[:, 1, :], in_=qi[:, 0:32]), reads=[qig], writes=[cg_])
            V(lambda e: e.tensor_scalar(out=c_[:, 1, :], in0=c_[:, 1, :], scalar1=128.0, scalar2=None, op0=ALU.mult), reads=[cg_], writes=[cg_])
            V(lambda e: e.tensor_copy(out=c_[:, 2, 0:1], in_=c_[:, 1, 0:1]), reads=[cg_], writes=[cg_])
            for ex in range(1, 32):
                V(lambda e, ex=ex: e.tensor_tensor(out=c_[:, 2, ex:ex + 1], in0=c_[:, 2, ex - 1:ex], in1=c_[:, 1, ex:ex + 1], op=ALU.add),
                  reads=[cg_], writes=[cg_])
            V(lambda e: e.tensor_tensor(out=c_[:, 3, :], in0=c_[:, 2, :], in1=c_[:, 1, :], op=ALU.subtract), reads=[cg_], writes=[cg_])
            V(lambda e: e.tensor_tensor(out=P_[:], in0=P_[:], in1=R_[:], op=ALU.add), reads=[Pg, Rg], writes=[Pg])
            V(lambda e: e.tensor_tensor(out=P_[:], in0=P_[:], in1=c_[:, 3, :].unsqueeze(1).to_broadcast([128, NT, 32]), op=ALU.add),
              reads=[Pg, cg_], writes=[Pg])
            tm, tmg = tmp[0]
            sf, sfg = slf[0]
            for kk, (oht, ohg) in enumerate(((oh0t, oh0g), (oh1t, oh1g))):
                V(lambda e, oht=oht: e.tensor_tensor(out=tm[:], in0=oht[:], in1=P_[:], op=ALU.mult), reads=[ohg, Pg], writes=[tmg])
                V(lambda e, kk=kk: e.tensor_reduce(out=sf[:, :, kk], in_=tm[:], axis=AX.X, op=ALU.add), reads=[tmg], writes=[sfg])
            V(lambda e: e.tensor_copy(out=slt[:], in_=sf[:]), reads=[sfg], writes=[slg])
            pf, pfg = posf[0]
            pi_, pig = posi[0]
            V(lambda e: e.tensor_scalar(out=pf[:, :, 2:4], in0=sf[:], scalar1=-63.5, scalar2=1.0 / 128.0, op0=ALU.add, op1=ALU.mult), reads=[sfg], writes=[pfg])
            V(lambda e: e.tensor_copy(out=pi_[:], in_=pf[:, :, 2:4]), reads=[pfg], writes=[pig])
            V(lambda e: e.tensor_copy(out=pf[:, :, 2:4], in_=pi_[:]), reads=[pig], writes=[pfg])
            V(lambda e: e.scalar_tensor_tensor(out=pf[:, :, 0:2], in0=pf[:, :, 2:4], scalar=-128.0, in1=sf[:], op0=ALU.mult, op1=ALU.add),
              reads=[pfg, sfg], writes=[pfg])
            V(lambda e: e.scalar_tensor_tensor(out=pf[:, :, 0:2], in0=pf[:, :, 0:2], scalar=float(CAPB), in1=pf[:, :, 2:4], op0=ALU.mult, op1=ALU.add),
              reads=[pfg], writes=[pfg])
            V(lambda e: e.tensor_copy(out=pi_[:], in_=pf[:, :, 0:2]), reads=[pfg], writes=[pig])
            cm, cmg = cmp_[0]
            be, beg = bef[0]
            V(lambda e: e.tensor_scalar(out=be[:], in0=iota_b[:, 0:CAPB], scalar1=128.0, scalar2=None, op0=ALU.mult), reads=[cfg_], writes=[beg])
            V(lambda e: e.tensor_tensor(out=cm[:], in0=c_[:, 2, :].unsqueeze(1).to_broadcast([128, CAPB, 32]),
                                        in1=be[:].unsqueeze(2).to_broadcast([128, CAPB, 32]), op=ALU.is_le), reads=[cg_, beg], writes=[cmg])
            V(lambda e: e.tensor_reduce(out=be[:], in_=cm[:], axis=AX.X, op=ALU.add), reads=[cmg], writes=[beg])
            V(lambda e: e.tensor_scalar(out=be[:], in0=be[:], scalar1=31.0, scalar2=None, op0=ALU.min), reads=[beg], writes=[beg])
            sk, skg = skipf[0]
            V(lambda e: e.memset(sk[:], 0.0), writes=[skg])
            V(lambda e: e.tensor_tensor(out=sk[:, 2:CAPB], in0=be[:, 2:CAPB], in1=be[:, 0:CAPB - 2], op=ALU.is_equal), reads=[beg], writes=[skg])
            V(lambda e: e.tensor_scalar(out=be[:], in0=be[:], scalar1=128.0, scalar2=iota_p, op0=ALU.mult, op1=ALU.add), reads=[beg, cfg_], writes=[beg])
            V(lambda e: e.scalar_tensor_tensor(out=be[:], in0=sk[:], scalar=8192.0, in1=be[:], op0=ALU.mult, op1=ALU.add), reads=[skg, beg], writes=[beg])
            wi, wig = widx[0]
            V(lambda e: e.tensor_copy(out=wi[:], in_=be[:]), reads=[beg], writes=[wig])
            tk, tkg = tok[0]
            tki, tkig = toki[0]
            V(lambda e: e.tensor_scalar(out=tk[:], in0=iota_b[:, 0:NT], scalar1=128.0, scalar2=iota_p, op0=ALU.mult, op1=ALU.add),
              reads=[cfg_], writes=[tkg])
            V(lambda e: e.tensor_copy(out=tki[:], in_=tk[:]), reads=[tkg], writes=[tkig])
            fl, flg = fill[0]
            V(lambda e: e.memset(fl[:], NTOK), writes=[flg])
            stok_g = Reg()
            dma("sp", lambda e: e.dma_start(out=stok_d.rearrange("(p b) o -> p (b o)", b=CAPB), in_=fl[:]), reads=[flg], writes=[stok_g])
            for i in range(NT):
                for kk in range(2):
                    dma("pool", lambda e, i=i, kk=kk: e.indirect_dma_start(
                        out=stok_d, out_offset=bass.IndirectOffsetOnAxis(ap=pi_[:, i, kk:kk + 1], axis=0),
                        in_=tki[:, i:i + 1], in_offset=None), reads=[pig, tkig, stok_g], writes=[Reg()])
            sch.barrier(bar_scr[:])
        with ExitStack() as st:
            wi, wig = widx[0]
            sidx = Buf(st, [128, CAPB], I32)
            si, sig = sidx[0]
            dma("sp", lambda e: e.dma_start(out=si[:], in_=stok_d.rearrange("(p b) o -> p (b o)", b=CAPB)), writes=[sig])
            xs = Buf(st, [128, D], BF16, n=2)
            wgu = Buf(st, [128, 8192], BF16, n=2)
            wdn = Buf(st, [128, 4096], BF16, n=2)
            xsT = Buf(st, [128, KC, 128], BF16, n=2)
            sg_ = Buf(st, [128, 256], F32, n=2)
            act_ = Buf(st, [128, 256], BF16, n=2)
            actT = Buf(st, [128, 2, 128], BF16, n=2)
            yo = Buf(st, [128, D], BF16, n=2)
            tpb = Buf(st, [128, 1024], BF16, n=2, ps=True)
            hid = Buf(st, [128, 512], F32, n=2, ps=True)
            yop = Buf(st, [128, 512], F32, n=4, ps=True)
            ys_g = Reg()

            def stA(b):
                xst, xsg = xs[b]
                dma("pool", lambda e, b=b: e.indirect_dma_start(
                    out=xst[:], out_offset=None, in_=h2_d, in_offset=bass.IndirectOffsetOnAxis(ap=si[:, b:b + 1], axis=0)),
                    reads=[sig], writes=[xsg])
                wgt, wgg = wgu[b]
                wdt, wdg = wdn[b]
                dma("pool", lambda e, b=b: e.indirect_dma_start(
                    out=wgt[:], out_offset=None, in_=wgu_d, in_offset=bass.IndirectOffsetOnAxis(ap=wi[:, b:b + 1], axis=0),
                    bounds_check=bc_val, oob_is_err=False),
                    reads=[wig, wcast_g], writes=[wgg])
                dma("pool", lambda e, b=b: e.indirect_dma_start(
                    out=wdt[:], out_offset=None, in_=wd_d, in_offset=bass.IndirectOffsetOnAxis(ap=wi[:, b:b + 1], axis=0),
                    bounds_check=bc_val, oob_is_err=False),
                    reads=[wig, wcast_g], writes=[wdg])
                xTt, xTg = xsT[b]
                xv = xst[:].rearrange("p (q kc) -> p kc q", kc=KC)
                for half in range(2):
                    tpt, tpg = tpb[half]
                    for k8 in range(8):
                        kc = half * 8 + k8
                        op("pe", lambda e, kc=kc, k8=k8, tpt=tpt: e.transpose(tpt[:, k8 * 128:(k8 + 1) * 128], xv[:, kc, :], ident_b),
                           reads=[xsg, cbg], writes=[tpg], signal=(k8 == 7))
                    op("act" if half == 0 else "dve",
                       lambda e, half=half, tpt=tpt: (e.activation(out=xTt[:, half * 8:(half + 1) * 8, :].rearrange("p a b -> p (a b)"),
                                                                   in_=tpt[:], func=AF.Copy) if half == 0 else
                                                      e.tensor_copy(out=xTt[:, half * 8:(half + 1) * 8, :].rearrange("p a b -> p (a b)"), in_=tpt[:])),
                       reads=[tpg], writes=[xTg])
                ht, hg = hid[b]
                for gu_ in range(2):
                    for kc in range(KC):
                        op("pe", lambda e, kc=kc, gu_=gu_: e.matmul(ht[:, gu_ * 256:(gu_ + 1) * 256], lhsT=xTt[:, kc, :],
                                                                   rhs=wgt[:, gu_ * 4096 + kc * 256:gu_ * 4096 + (kc + 1) * 256],
                                                                   start=(kc == 0), stop=(kc == KC - 1)),
                           reads=[xTg, wgg], writes=[hg], signal=(kc == KC - 1 and gu_ == 1))

            def stB(b):
                ht, hg = hid[b]
                wdt, wdg = wdn[b]
                sgt, sgg = sg_[b]
                op("act", lambda e: e.activation(out=sgt[:], in_=ht[:, 0:256], func=AF.Silu), reads=[hg], writes=[sgg])
                att, atg = act_[b]
                op("dve", lambda e: e.tensor_tensor(out=att[:], in0=sgt[:], in1=ht[:, 256:512], op=ALU.mult), reads=[sgg, hg], writes=[atg])
                aTt, aTg = actT[b]
                tpt, tpg = tpb[0]
                av = att[:].rearrange("p (q c) -> p c q", c=2)
                for c in range(2):
                    op("pe", lambda e, c=c: e.transpose(tpt[:, c * 128:(c + 1) * 128], av[:, c, :], ident_b),
                       reads=[atg, cbg], writes=[tpg], signal=(c == 1))
                op("act", lambda e: e.activation(out=aTt[:].rearrange("p a b -> p (a b)"), in_=tpt[:, 0:256], func=AF.Copy), reads=[tpg], writes=[aTg])
                yot, yog = yo[b]
                for nb in range(4):
                    ypt, ypg = yop[nb]
                    for c in range(2):
                        op("pe", lambda e, c=c, nb=nb, ypt=ypt: e.matmul(ypt[:], lhsT=aTt[:, c, :],
                                                                       rhs=wdt[:, c * 2048 + nb * 512:c * 2048 + (nb + 1) * 512],
                                                                       start=(c == 0), stop=(c == 1)),
                           reads=[aTg, wdg], writes=[ypg], signal=(c == 1))
                    op("act" if nb % 2 == 1 else "dve",
                       lambda e, nb=nb, ypt=ypt: (e.activation(out=yot[:, nb * 512:(nb + 1) * 512], in_=ypt[:], func=AF.Copy) if nb % 2 == 1
                                                  else e.tensor_copy(out=yot[:, nb * 512:(nb + 1) * 512], in_=ypt[:])),
                       reads=[ypg], writes=[yog])
                dma("act", lambda e, b=b: e.dma_start(out=ys_d[b * 128:(b + 1) * 128, :], in_=yot[:]), reads=[yog], writes=[ys_g])

            stA(0)
            for b in range(CAPB):
                if b + 1 < CAPB:
                    stA(b + 1)
                stB(b)
            sch.barrier(bar_scr[:])
        with ExitStack() as st:
            gate2 = Buf(st, [128, D], F32)
            dma("sp", lambda e: e.dma_start(out=gate2[0][0][:], in_=modl_d[l, 5]), writes=[gate2[0][1]])
            p0 = Buf(st, [128, D], BF16, n=2)
            p1 = Buf(st, [128, D], BF16, n=2)
            xmb = Buf(st, [128, D], F32, n=2)
            mm = Buf(st, [128, D], F32, n=3)
            out_g = Reg()

            def d4a(i):
                a0, a0g = p0[i]
                a1, a1g = p1[i]
                dma("pool", lambda e, i=i: e.indirect_dma_start(
                    out=a0[:], out_offset=None, in_=ys_d, in_offset=bass.IndirectOffsetOnAxis(ap=slt[:, i, 0:1], axis=0)),
                    reads=[slg], writes=[a0g])
                dma("pool", lambda e, i=i: e.indirect_dma_start(
                    out=a1[:], out_offset=None, in_=ys_d, in_offset=bass.IndirectOffsetOnAxis(ap=slt[:, i, 1:2], axis=0)),
                    reads=[slg], writes=[a1g])
                xt, xg = xmb[i]
                dma("sp", lambda e, i=i: e.dma_start(out=xt[:], in_=xm_d[i * 128:(i + 1) * 128, :]), writes=[xg])
                mt, mg = mm[i]
                op("act", lambda e, i=i: e.activation(out=mt[:], in_=a0[:], func=AF.Copy, scale=wwt[:, i, 0:1]),
                   reads=[a0g, wwg], writes=[mg])

            def d4b(i):
                a1, a1g = p1[i]
                xt, xg = xmb[i]
                mt, mg = mm[i]
                op("dve", lambda e, i=i: e.scalar_tensor_tensor(out=mt[:], in0=a1[:], scalar=wwt[:, i, 1:2], in1=mt[:], op0=ALU.mult, op1=ALU.add),
                   reads=[a1g, wwg, mg], writes=[mg])
                op("dve", lambda e: e.tensor_tensor(out=mt[:], in0=mt[:], in1=gate2[0][0][:], op=ALU.mult), reads=[mg, gate2[0][1]], writes=[mg])
                op("dve", lambda e: e.tensor_tensor(out=mt[:], in0=mt[:], in1=xt[:], op=ALU.add), reads=[mg, xg], writes=[mg])
                dma("act", lambda e, i=i: e.dma_start(out=x_dst[i * 128:(i + 1) * 128, :], in_=mt[:]), reads=[mg], writes=[out_g])

            d4a(0)
            for i in range(NT):
                if i + 1 < NT:
                    d4a(i + 1)
                d4b(i)
            sch.barrier(bar_scr[:])
        moe.close()
    sch.final_wait()
    return nc, sch


def make_consts():
    c = np.zeros((128, 1280), np.float32)
    i = np.arange(128)
    c[:, 0:128] = np.eye(128)
    c[:, 128:256] = (i[:, None] >= i[None, :])
    c[:, 256:384] = (i[:, None] < i[None, :])
    c[:, 384:512] = (i[:, None] < i[None, :])
    c[:, 512:640] = 1.0
    c[:, 640:768] = (i[:, None] <= i[None, :])
    c[:, 768] = i
    c[:, 1024:1280] = np.arange(256)[None, :]
    return c


_CACHE = {}


def run(inputs, trace=False):
    x = np.asarray(inputs["x"], np.float32)
    B, S, _ = x.shape
    G = 1
    NCORES = B
    NBLK = S // 128
    NT = NBLK // G
    depth = inputs["w_in"].shape[0]
    key = (NT, NBLK, G, depth)
    if key not in _CACHE:
        _CACHE[key] = build(NT, NBLK, G, depth)
    nc, sch = _CACHE[key]
    f = lambda a: np.ascontiguousarray(np.asarray(a, np.float32))
    cst = make_consts()
    tri = cst[:, 384:512]
    ones = np.ones((128, 128), np.float32)
    zeros = np.zeros((128, 128), np.float32)
    shared = {
        "w_mod": f(inputs["w_mod"]), "b_mod": f(inputs["b_mod"]).reshape(1, -1), "mod_layer": f(inputs["mod_layer"]),
        "norm1_g": f(inputs["norm1_g"]), "w_in": f(inputs["w_in"]), "gm_norm_g": f(inputs["gm_norm_g"]),
        "gm_wsT": f(np.asarray(inputs["gm_ws"]).transpose(0, 1, 3, 2)), "gm_bsT": f(np.asarray(inputs["gm_bs"]).transpose(0, 2, 1)),
        "q_norm_g": f(inputs["q_norm_g"]), "k_norm_g": f(inputs["k_norm_g"]), "out_norm_g": f(inputs["out_norm_g"]),
        "w_out": f(inputs["w_out"]), "norm2_g": f(inputs["norm2_g"]),
        "w_r": f(np.concatenate([np.asarray(inputs["w_group"]), np.asarray(inputs["w_route"])], axis=-1)),
        "b_r": f(np.concatenate([np.asarray(inputs["b_group"]), np.asarray(inputs["b_route"])], axis=-1)),
        "w_gate": f(inputs["w_gate"]), "w_up": f(inputs["w_up"]), "w_down": f(inputs["w_down"]),
        "consts": cst,
    }
    in_maps = []
    owns = []
    for c in range(NCORES):
        b, r = c // G, c % G
        ob = own_blocks(G, NT, r)
        owns.append((b, ob))
        xs = np.concatenate([x[b, n * 128:(n + 1) * 128] for n in ob], axis=0)
        if G == 1:
            am = np.stack([tri, ones, tri, ones], axis=1)
        elif r == 0:
            am = np.stack([zeros, tri, tri, ones], axis=1)
        else:
            am = np.stack([tri, ones, zeros, tri], axis=1)
        m = dict(shared)
        m["x"] = np.ascontiguousarray(xs)
        m["cT"] = f(np.asarray(inputs["c"])[b].reshape(KC, 128).T)
        m["amask"] = np.ascontiguousarray(am.astype(np.float32))
        in_maps.append(m)
    res = run_bass_kernel_spmd(nc, in_maps, core_ids=list(range(NCORES)), **({"trace": True} if trace else {}))
    out = np.empty_like(x)
    for c in range(NCORES):
        b, ob = owns[c]
        o = res.results[c]["out"]
        for t, n in enumerate(ob):
            out[b, n * 128:(n + 1) * 128] = o[t * 128:(t + 1) * 128]
    return out, res


def kernel(**inputs):
    out, _ = run(inputs)
    return out
```
